# Optimizing a Trainium2 kernel written in Bass

```python
import math
import jax, jax.numpy as jnp
from jax import lax
import numpy as np

D_MODEL = 1024
BATCH = 16
SEQ = 2048
DEPTH = 4

N_A_LAYERS = DEPTH // 2
N_B_LAYERS = DEPTH - N_A_LAYERS
RWKV_HEAD_DIM = 64
RWKV_HEADS = D_MODEL // RWKV_HEAD_DIM
DECAY_LORA = 64
ICLR_LORA = 64
VRES_LORA = 32
GATE_LORA = 160
RWKV_GN_EPS = 64e-5
L2_EPS = 1e-12
DA_HEAD_DIM = 64
DA_HEADS = D_MODEL // (2 * DA_HEAD_DIM)
DA_QK_WIDTH = DA_HEADS * 2 * DA_HEAD_DIM
DA_V_WIDTH = DA_HEADS * 2 * DA_HEAD_DIM
BLOCK_Q = 128
SUBLN_EPS = 1e-5
N_EXPERTS = 32
TOP_K = 4
EXPERT_FF = D_MODEL
SWIGLU_LIMIT = 7.0
SWIGLU_ALPHA = 1.702
EXPERT_BLOCK = 512
DEEPNORM_ALPHA = (2 * DEPTH) ** 0.25
DEEPNORM_BETA = (8 * DEPTH) ** -0.25
LN_EPS = 1e-5

kernel_name = "yoco_rwkv7_diffattn_moe_deepnorm"

F32 = jnp.float32


def layer_norm(x, g, b):
    xf = x.astype(F32)
    mu = jnp.mean(xf, axis=-1, keepdims=True)
    var = jnp.mean(jnp.square(xf - mu), axis=-1, keepdims=True)
    return ((xf - mu) * lax.rsqrt(var + LN_EPS) * g + b).astype(x.dtype)


def wkv7_scan(r, w, k, v, a, b):
    B_, S_, H, N = r.shape

    def step(state, inp):
        r_t, w_t, k_t, v_t, a_t, b_t = inp
        sa = jnp.einsum('bhvk,bhk->bhv', state, a_t)
        state = (state * w_t[:, :, None, :] + sa[..., None] * b_t[:, :, None, :]
                 + v_t[..., None] * k_t[:, :, None, :])
        y_t = jnp.einsum('bhvk,bhk->bhv', state, r_t)
        return state, y_t

    xs = tuple(jnp.moveaxis(t, 1, 0) for t in (r, w, k, v, a, b))
    state0 = jnp.zeros((B_, H, N, N), F32)
    _, y = lax.scan(step, state0, xs)
    return jnp.moveaxis(y, 0, 1)


def rwkv7_time_mix(x, v_first, mix, w_rkv, w_o, w0, w1, w2, a0, a1, a2, g1, g2,
                   k_k, k_a, r_k, lnx_g, lnx_b, v0=None, v1=None, v2=None):
    B_, S_, D_ = x.shape
    H, N = RWKV_HEADS, RWKV_HEAD_DIM
    xx = jnp.pad(x, ((0, 0), (1, 0), (0, 0)))[:, :-1] - x
    xm = x[:, :, None, :] + xx[:, :, None, :] * mix
    rkv = jnp.einsum('bsjd,jde->bsje', xm[:, :, :3], w_rkv)
    r, k, v = rkv[:, :, 0], rkv[:, :, 1], rkv[:, :, 2]
    xv, xw, xa, xg = xm[:, :, 2], xm[:, :, 3], xm[:, :, 4], xm[:, :, 5]
    w_log = -jax.nn.softplus(-(w0 + jnp.tanh(xw @ w1) @ w2)) - 0.5
    decay = jnp.exp(-jnp.exp(w_log.astype(F32)))
    if v0 is None:
        v_first = v
    else:
        v = v + (v_first - v) * jax.nn.sigmoid(v0 + (xv @ v1) @ v2)
    a = jax.nn.sigmoid(a0 + (xa @ a1) @ a2)
    g = jax.nn.sigmoid(xg @ g1) @ g2

    def heads(t):
        return t.astype(F32).reshape(B_, S_, H, N)

    r_h, k_h, v_h, a_h, w_h = heads(r), heads(k), heads(v), heads(a), heads(decay)
    kk = k_h * k_k.astype(F32).reshape(H, N)
    kk = kk / jnp.maximum(jnp.sqrt(jnp.sum(kk * kk, axis=-1, keepdims=True)), L2_EPS)
    k_h = k_h * (1.0 + (a_h - 1.0) * k_a.astype(F32).reshape(H, N))
    y = wkv7_scan(r_h, w_h, k_h, v_h, -kk, kk * a_h)
    mu = jnp.mean(y, axis=-1, keepdims=True)
    var = jnp.mean(jnp.square(y - mu), axis=-1, keepdims=True)
    y = ((y - mu) * lax.rsqrt(var + RWKV_GN_EPS)).reshape(B_, S_, D_) * lnx_g + lnx_b
    bonus = jnp.sum(r_h * k_h * r_k.astype(F32), axis=-1, keepdims=True) * v_h
    y = y + bonus.reshape(B_, S_, D_)
    out = (y * g).astype(x.dtype) @ w_o
    return out, v_first


def shared_kv(x, kv_w):
    B_, S_, _ = x.shape
    kv = x @ kv_w
    k = kv[..., :DA_QK_WIDTH].reshape(B_, S_, DA_HEADS, 2, DA_HEAD_DIM).transpose(0, 2, 3, 1, 4)
    v = kv[..., DA_QK_WIDTH:].reshape(B_, S_, DA_HEADS, 2 * DA_HEAD_DIM).transpose(0, 2, 1, 3)
    return k, v


def alibi_slopes(n_heads):
    return 2.0 ** (-8.0 * jnp.arange(1, n_heads + 1, dtype=F32) / n_heads)


def diff_attention(x, k_sh, v_sh, w_q, w_o, lam, subln_g, lambda_init):
    B_, S_, _ = x.shape
    scale = DA_HEAD_DIM ** -0.5
    q = (x @ w_q).reshape(B_, S_, DA_HEADS, 2, DA_HEAD_DIM).transpose(0, 2, 3, 1, 4)
    lam_f = lam.astype(F32)
    lam_full = (jnp.exp(jnp.sum(lam_f[0] * lam_f[1])) - jnp.exp(jnp.sum(lam_f[2] * lam_f[3]))
                + lambda_init)
    slopes = alibi_slopes(DA_HEADS)
    outs = []
    for i in range(S_ // BLOCK_Q):
        q0, q1 = i * BLOCK_Q, (i + 1) * BLOCK_Q
        qb = q[:, :, :, q0:q1]
        kb = k_sh[:, :, :, :q1]
        vb = v_sh[:, :, :q1]
        s = jnp.einsum('bhcqd,bhckd->bhcqk', qb, kb).astype(F32) * scale
        dist = (q0 + jnp.arange(BLOCK_Q))[:, None] - jnp.arange(q1)[None, :]
        bias = jnp.where(dist >= 0, -slopes[:, None, None] * dist.astype(F32), -jnp.inf)
        p = jax.nn.softmax(s + bias[None, :, None], axis=-1)
        attn = p[:, :, 0] - lam_full * p[:, :, 1]
        outs.append(jnp.einsum('bhqk,bhkd->bhqd', attn.astype(vb.dtype), vb))
    o = jnp.concatenate(outs, axis=2).astype(F32)
    o = o * lax.rsqrt(jnp.mean(o * o, axis=-1, keepdims=True) + SUBLN_EPS) * subln_g
    o = o * (1.0 - lambda_init)
    o = o.transpose(0, 2, 1, 3).reshape(B_, S_, DA_V_WIDTH).astype(x.dtype)
    return o @ w_o


def clamped_swiglu(h):
    gate, up = h[..., :EXPERT_FF], h[..., EXPERT_FF:]
    gate = jnp.minimum(gate, SWIGLU_LIMIT)
    up = jnp.clip(up, -SWIGLU_LIMIT, SWIGLU_LIMIT)
    return (up + 1.0) * (gate * jax.nn.sigmoid(SWIGLU_ALPHA * gate))


def moe_ffn(x, router_w, router_b, w_gu, b_gu, w_dn, b_dn):
    B_, S_, D_ = x.shape
    T = B_ * S_
    TK = T * TOP_K
    xt = x.reshape(T, D_)
    logits = (xt @ router_w + router_b).astype(F32)
    top_logit, top_idx = lax.top_k(logits, TOP_K)
    gate = jax.nn.softmax(top_logit, axis=-1)
    flat_e = top_idx.reshape(TK)
    flat_tok = jnp.arange(TK, dtype=jnp.int32) // TOP_K
    flat_gate = gate.reshape(TK)
    order = jnp.argsort(flat_e)
    e_sorted = flat_e[order]
    counts = jnp.bincount(flat_e, length=N_EXPERTS)
    padded = (counts + EXPERT_BLOCK - 1) // EXPERT_BLOCK * EXPERT_BLOCK
    pad_end = jnp.cumsum(padded)
    pad_start = pad_end - padded
    grp_start = jnp.cumsum(counts) - counts
    dest = pad_start[e_sorted] + jnp.arange(TK, dtype=jnp.int32) - grp_start[e_sorted]
    n_blocks = -(-TK // EXPERT_BLOCK) + N_EXPERTS
    P = n_blocks * EXPERT_BLOCK
    row_tok = jnp.full((P,), T, jnp.int32).at[dest].set(flat_tok[order])
    row_gate = jnp.zeros((P,), F32).at[dest].set(flat_gate[order])
    block_start = jnp.arange(n_blocks, dtype=jnp.int32) * EXPERT_BLOCK
    block_exp = jnp.minimum(jnp.searchsorted(pad_end, block_start, side='right'), N_EXPERTS - 1)
    x_pad = jnp.concatenate([xt, jnp.zeros((1, D_), xt.dtype)], axis=0)

    def expert_block(args):
        toks, e = args
        h = x_pad[toks] @ w_gu[e] + b_gu[e]
        return clamped_swiglu(h) @ w_dn[e] + b_dn[e]

    y_rows = lax.map(expert_block, (row_tok.reshape(n_blocks, EXPERT_BLOCK), block_exp))
    y_rows = y_rows.reshape(P, D_).astype(F32) * row_gate[:, None]
    out = jnp.zeros((T + 1, D_), F32).at[row_tok].add(y_rows)[:T]
    return out.reshape(B_, S_, D_).astype(x.dtype)


def setup_inputs(seed: int = 0) -> dict:
    key = jax.random.key(seed)
    ks = iter(jax.random.split(key, 48))

    def nrm(shape, scale):
        return jax.random.normal(next(ks), shape, F32) * scale

    D, NA, NB, E, FF = D_MODEL, N_A_LAYERS, N_B_LAYERS, N_EXPERTS, EXPERT_FF
    H, N = RWKV_HEADS, RWKV_HEAD_DIM
    x = nrm((BATCH, SEQ, D), 1.0)
    ln_g = 1.0 + nrm((DEPTH, 2, D), 0.05)
    ln_b = nrm((DEPTH, 2, D), 0.02)
    rwkv_mix = jax.random.uniform(next(ks), (NA, 6, D), F32)
    rkv_scale = jnp.array([1.0, 1.0, DEEPNORM_BETA], F32)[None, :, None, None] * D ** -0.5
    rwkv_w_rkv = nrm((NA, 3, D, D), 1.0) * rkv_scale
    rwkv_w_o = nrm((NA, D, D), D ** -0.5 * DEEPNORM_BETA)
    rwkv_w0 = jnp.linspace(-6.0, -1.0, D, dtype=F32)[None, :] + nrm((NA, D), 0.1)
    rwkv_w1 = nrm((NA, D, DECAY_LORA), D ** -0.5)
    rwkv_w2 = nrm((NA, DECAY_LORA, D), 0.1 * DECAY_LORA ** -0.5)
    rwkv_a0 = nrm((NA, D), 0.1)
    rwkv_a1 = nrm((NA, D, ICLR_LORA), D ** -0.5)
    rwkv_a2 = nrm((NA, ICLR_LORA, D), 0.5 * ICLR_LORA ** -0.5)
    rwkv_g1 = nrm((NA, D, GATE_LORA), D ** -0.5)
    rwkv_g2 = nrm((NA, GATE_LORA, D), GATE_LORA ** -0.5)
    rwkv_k_k = 0.85 + nrm((NA, D), 0.05)
    rwkv_k_a = 1.0 + nrm((NA, D), 0.05)
    rwkv_r_k = nrm((NA, H, N), 0.1)
    rwkv_lnx_g = 1.0 + nrm((NA, D), 0.05)
    rwkv_lnx_b = nrm((NA, D), 0.02)
    rwkv_v0 = 1.0 + nrm((NA - 1, D), 0.1)
    rwkv_v1 = nrm((NA - 1, D, VRES_LORA), D ** -0.5)
    rwkv_v2 = nrm((NA - 1, VRES_LORA, D), 0.5 * VRES_LORA ** -0.5)
    kv_scale = jnp.concatenate([jnp.ones((DA_QK_WIDTH,), F32),
                                jnp.full((DA_V_WIDTH,), DEEPNORM_BETA, F32)]) * D ** -0.5
    kv_w = nrm((D, DA_QK_WIDTH + DA_V_WIDTH), 1.0) * kv_scale
    da_w_q = nrm((NB, D, DA_QK_WIDTH), D ** -0.5)
    da_w_o = nrm((NB, DA_V_WIDTH, D), DA_V_WIDTH ** -0.5 * DEEPNORM_BETA)
    da_lambda = nrm((NB, 4, DA_HEAD_DIM), 0.1)
    da_subln_g = 1.0 + nrm((NB, 2 * DA_HEAD_DIM), 0.05)
    moe_router_w = nrm((DEPTH, D, E), D ** -0.5)
    moe_router_b = nrm((DEPTH, E), 0.01)
    moe_w_gu = nrm((DEPTH, E, D, 2 * FF), D ** -0.5)
    moe_b_gu = nrm((DEPTH, E, 2 * FF), 0.02)
    moe_w_dn = nrm((DEPTH, E, FF, D), FF ** -0.5 * DEEPNORM_BETA)
    moe_b_dn = nrm((DEPTH, E, D), 0.02)
    return {"x": x, "ln_g": ln_g, "ln_b": ln_b,
            "rwkv_mix": rwkv_mix, "rwkv_w_rkv": rwkv_w_rkv, "rwkv_w_o": rwkv_w_o,
            "rwkv_w0": rwkv_w0, "rwkv_w1": rwkv_w1, "rwkv_w2": rwkv_w2,
            "rwkv_a0": rwkv_a0, "rwkv_a1": rwkv_a1, "rwkv_a2": rwkv_a2,
            "rwkv_g1": rwkv_g1, "rwkv_g2": rwkv_g2, "rwkv_k_k": rwkv_k_k, "rwkv_k_a": rwkv_k_a,
            "rwkv_r_k": rwkv_r_k, "rwkv_lnx_g": rwkv_lnx_g, "rwkv_lnx_b": rwkv_lnx_b,
            "rwkv_v0": rwkv_v0, "rwkv_v1": rwkv_v1, "rwkv_v2": rwkv_v2,
            "kv_w": kv_w, "da_w_q": da_w_q, "da_w_o": da_w_o, "da_lambda": da_lambda,
            "da_subln_g": da_subln_g,
            "moe_router_w": moe_router_w, "moe_router_b": moe_router_b,
            "moe_w_gu": moe_w_gu, "moe_b_gu": moe_b_gu, "moe_w_dn": moe_w_dn, "moe_b_dn": moe_b_dn}


def reference(x, ln_g, ln_b, rwkv_mix, rwkv_w_rkv, rwkv_w_o, rwkv_w0, rwkv_w1, rwkv_w2,
              rwkv_a0, rwkv_a1, rwkv_a2, rwkv_g1, rwkv_g2, rwkv_k_k, rwkv_k_a, rwkv_r_k,
              rwkv_lnx_g, rwkv_lnx_b, rwkv_v0, rwkv_v1, rwkv_v2, kv_w, da_w_q, da_w_o,
              da_lambda, da_subln_g, moe_router_w, moe_router_b, moe_w_gu, moe_b_gu,
              moe_w_dn, moe_b_dn):
    v_first = None
    k_sh = v_sh = None
    for l in range(DEPTH):
        if l < N_A_LAYERS:
            vres = (None, None, None) if l == 0 else (rwkv_v0[l - 1], rwkv_v1[l - 1], rwkv_v2[l - 1])
            mix_out, v_first = rwkv7_time_mix(
                x, v_first, rwkv_mix[l], rwkv_w_rkv[l], rwkv_w_o[l], rwkv_w0[l], rwkv_w1[l],
                rwkv_w2[l], rwkv_a0[l], rwkv_a1[l], rwkv_a2[l], rwkv_g1[l], rwkv_g2[l],
                rwkv_k_k[l], rwkv_k_a[l], rwkv_r_k[l], rwkv_lnx_g[l], rwkv_lnx_b[l], *vres)
        else:
            if l == N_A_LAYERS:
                k_sh, v_sh = shared_kv(x, kv_w)
            j = l - N_A_LAYERS
            lambda_init = 0.8 - 0.6 * math.exp(-0.3 * l)
            mix_out = diff_attention(x, k_sh, v_sh, da_w_q[j], da_w_o[j], da_lambda[j],
                                     da_subln_g[j], lambda_init)
        x = layer_norm(DEEPNORM_ALPHA * x + mix_out, ln_g[l, 0], ln_b[l, 0])
        ffn_out = moe_ffn(x, moe_router_w[l], moe_router_b[l], moe_w_gu[l], moe_b_gu[l],
                          moe_w_dn[l], moe_b_dn[l])
        x = layer_norm(DEEPNORM_ALPHA * x + ffn_out, ln_g[l, 1], ln_b[l, 1])
    return x
```

```python
import contextlib
import numpy as np
import concourse.bass as bass
import concourse.mybir as mybir

F32 = mybir.dt.float32
BF16 = mybir.dt.bfloat16
I32 = mybir.dt.int32
ALU = mybir.AluOpType
AF = mybir.ActivationFunctionType
AX = mybir.AxisListType

N_DMA_SEMS = 40


class Res:
    __slots__ = ("name", "last_w", "readers")

    def __init__(self, name=""):
        self.name = name
        self.last_w = None
        self.readers = []


class Op:
    __slots__ = ("eng", "fn", "deps", "marked", "is_dma", "sem", "val", "idx")

    def __init__(self, eng, fn, is_dma):
        self.eng = eng
        self.fn = fn
        self.deps = []
        self.marked = False
        self.is_dma = is_dma
        self.sem = None
        self.val = 0


ENGS = ("pe", "dve", "act", "pool", "sp")


class Prog:
    def __init__(self, nc):
        self.nc = nc
        self.base = contextlib.ExitStack()
        self.csem = {e: self.base.enter_context(nc.semaphore(f"s_{e}")) for e in ENGS}
        self.dsem = [self.base.enter_context(nc.semaphore(f"d_{i}")) for i in range(N_DMA_SEMS)]
        self.ccount = {e: 0 for e in ENGS}
        self.dma_rr = 0
        self.dma_last = [None] * N_DMA_SEMS
        self.dma_cnt = [0] * N_DMA_SEMS
        self.nres = 0
        self.stack = None
        self.total = {e: 0 for e in ENGS}
        self._reset()

    def _reset(self):
        self.ops = {e: [] for e in ENGS}

    @contextlib.contextmanager
    def scope(self, final=False):
        self.stack = contextlib.ExitStack()
        try:
            yield self
            self.flush(final)
        finally:
            self.stack.close()
            self.stack = None

    def sb(self, name, shape, dtype=F32):
        self.nres += 1
        return self.stack.enter_context(self.nc.sbuf_tensor(f"{name}_{self.nres}", list(shape), dtype))

    def ps(self, name, shape, dtype=F32):
        self.nres += 1
        return self.stack.enter_context(self.nc.psum_tensor(f"{name}_{self.nres}", list(shape), dtype))

    def res(self, name=""):
        self.nres += 1
        return Res(name or f"r{self.nres}")

    def _add(self, eng, fn, reads, writes, is_dma):
        op = Op(eng, fn, is_dma)
        deps = []
        for r in reads:
            if r.last_w is not None:
                deps.append(r.last_w)
        for w in writes:
            if w.last_w is not None:
                deps.append(w.last_w)
            deps.extend(w.readers)
        seen = set()
        for d in deps:
            if id(d) in seen or d is op:
                continue
            seen.add(id(d))
            if (not d.is_dma) and (not is_dma) and d.eng == "pe" and eng == "pe":
                continue
            op.deps.append(d)
            d.marked = True
        if is_dma:
            k = self.dma_rr
            self.dma_rr = (self.dma_rr + 1) % N_DMA_SEMS
            prev = self.dma_last[k]
            if prev is not None and all(prev is not x for x in op.deps):
                op.deps.append(prev)
            self.dma_last[k] = op
            self.dma_cnt[k] += 16
            op.sem = k
            op.val = self.dma_cnt[k]
            op.marked = True
        for r in reads:
            r.readers.append(op)
        for w in writes:
            w.last_w = op
            w.readers = []
        self.ops[eng].append(op)
        return op

    def op(self, eng, fn, reads=(), writes=()):
        return self._add(eng, fn, _rs(reads), _rs(writes), False)

    def dma(self, eng, out, in_, reads=(), writes=(), **kw):
        return self._add(eng, lambda e: e.dma_start(out=out, in_=in_, **kw),
                         _rs(reads), _rs(writes), True)

    def mm(self, out, lhsT, rhs, start=True, stop=True, reads=(), writes=()):
        return self.op("pe", lambda e: e.matmul(out, lhsT, rhs, start=start, stop=stop), reads, writes)

    def tr(self, out, in_, ident, reads=(), writes=()):
        return self.op("pe", lambda e: e.transpose(out, in_, ident), reads, writes)

    def flush(self, final=False):
        nc = self.nc
        csem, dsem = self.csem, self.dsem
        lastc = []
        for e in ENGS:
            comp = [o for o in self.ops[e] if not o.is_dma and o.fn is not None]
            if comp:
                comp[-1].marked = True
                lastc.append(comp[-1])
        lastd = [o for o in self.dma_last if o is not None]
        for e in ENGS:
            b = Op(e, None, False)
            b.deps = list(lastc) + list(lastd)
            self.ops[e].append(b)
        for e in ENGS:
            c = self.ccount[e]
            for op in self.ops[e]:
                if op.is_dma or op.fn is None:
                    continue
                if op.marked:
                    c += 1
                    op.val = c
            self.ccount[e] = c

        def semof(d):
            return (("d", d.sem), dsem[d.sem]) if d.is_dma else (("c", d.eng), csem[d.eng])

        def replay(ename):
            def body(e):
                known = {}
                for op in self.ops[ename]:
                    for d in op.deps:
                        key, sem = semof(d)
                        if known.get(key, 0) >= d.val:
                            continue
                        e.wait_ge(sem, d.val)
                        known[key] = d.val
                    if op.fn is None:
                        continue
                    ins = op.fn(e)
                    if op.is_dma:
                        ins.then_inc(dsem[op.sem], 16)
                    elif op.marked:
                        ins.then_inc(csem[ename], 1)
            return body

        with nc.Block() as block:
            block.sync(replay("sp"))
            block.scalar(replay("act"))
            block.vector(replay("dve"))
            block.gpsimd(replay("pool"))
            block.tensor(replay("pe"))
        for e in ENGS:
            self.total[e] += len(self.ops[e])
        self._reset()

    def finish(self):
        self.base.close()


class Tl:
    def __init__(self, P, name, shape, dtype=F32, psum=False):
        self.t = (P.ps if psum else P.sb)(name, shape, dtype)
        self.r = P.res(name)
        self.shape = shape

    def __getitem__(self, k):
        return self.t[k]


def _rs(xs):
    return [x.r if isinstance(x, Tl) else x for x in xs]


D_MODEL = 1024
DEPTH = 4
NA = 2
H_R = 16
EXPM05 = float(np.exp(-0.5))
ALPHA = (2 * DEPTH) ** 0.25
LN_EPS = 1e-5
GN_EPS = 64e-5


class Ctx:
    pass


def setup_common(P):
    C = Ctx()
    C.P = P
    C.ident = Tl(P, "ident", [128, 128])
    P.op("pool", lambda e: e.memset(C.ident[:], 0.0), writes=[C.ident])
    P.op("pool", lambda e: e.affine_select(C.ident[:], C.ident[:], [[-1, 128]], ALU.not_equal, 1.0,
                                           base=0, channel_multiplier=1), reads=[C.ident], writes=[C.ident])
    C.banks = [Tl(P, f"bank{i}", [128, 512], F32, psum=True) for i in range(8)]
    C.bi = 0
    C.rr = 0
    return C


def bank(C):
    b = C.banks[C.bi]
    C.bi = (C.bi + 1) % 8
    return b


def ev_eng(C):
    C.rr ^= 1
    return "act" if C.rr else "dve"


def copy_op(P, eng, out, in_, reads, writes):
    if eng == "act":
        P.op("act", lambda e: e.activation(out, in_, AF.Copy), reads, writes)
    else:
        P.op(eng, lambda e: e.tensor_copy(out, in_), reads, writes)


def tt(P, eng, out, in0, in1, op, reads, writes):
    P.op(eng, lambda e: e.tensor_tensor(out, in0, in1, op), reads, writes)


def stt(P, eng, out, in0, scalar, in1, op0, op1, reads, writes):
    P.op(eng, lambda e: e.scalar_tensor_tensor(out, in0, scalar, in1, op0, op1), reads, writes)


def rsqrt_op(P, t, lo, hi, bias, use_max):
    sl = t[:, lo:hi]
    op0 = ALU.max if use_max else ALU.add
    P.op("dve", lambda e: e.tensor_scalar(sl, sl, bias, None, op0), [t], [t])
    P.op("act", lambda e: e.activation(sl, sl, AF.Sqrt), [t], [t])
    P.op("dve", lambda e: e.reciprocal(sl, sl), [t], [t])


def load_bcast(P, name, src_1d, n, eng="sp"):
    t = Tl(P, name, [128, n])
    P.dma(eng, t[:], src_1d.partition_broadcast(128), writes=[t])
    return t


def layer_norm_tile(C, z, out, g_t, b_t, st):
    P = C.P
    junk = C.junk
    P.op("act", lambda e: e.activation(junk[:], z[:], AF.Copy, accum_out=st[:, 0:1]), [z], [junk, st])
    P.op("act", lambda e: e.activation(junk[:], z[:], AF.Square, accum_out=st[:, 1:2]), [z], [junk, st])
    P.op("dve", lambda e: e.tensor_scalar(st[:, 2:3], st[:, 0:1], 1.0 / 1024, None, ALU.mult), [st], [st])
    P.op("dve", lambda e: e.tensor_tensor(st[:, 3:4], st[:, 2:3], st[:, 2:3], ALU.mult), [st], [st])
    P.op("dve", lambda e: e.scalar_tensor_tensor(st[:, 4:5], st[:, 1:2], 1.0 / 1024, st[:, 3:4], ALU.mult, ALU.subtract), [st], [st])
    copy_op(P, "dve", st[:, 5:6], st[:, 4:5], [st], [st])
    rsqrt_op(P, st, 5, 6, LN_EPS, False)
    P.op("dve", lambda e: e.scalar_tensor_tensor(st[:, 6:7], st[:, 2:3], -1.0, st[:, 5:6], ALU.mult, ALU.mult), [st], [st])
    P.op("act", lambda e: e.activation(out[:], z[:], AF.Identity, bias=st[:, 6:7], scale=st[:, 5:6]), [z, st], [out])
    tt(P, "dve", out[:], out[:], g_t[:], ALU.mult, [out, g_t], [out])
    tt(P, "pool", out[:], out[:], b_t[:], ALU.add, [out, b_t], [out])


def load_w_bf16(C, dst, src2d, K, N, stage):
    P = C.P
    kcs = K // 128
    per = max(1, stage.shape[1] // N)
    for k0 in range(0, kcs, per):
        k1 = min(kcs, k0 + per)
        sv = stage[:, 0:(k1 - k0) * N].rearrange("p (k n) -> p k n", n=N)
        P.dma(C.dq(), sv, src2d[k0 * 128:k1 * 128, :].rearrange("(k p) n -> p k n", p=128), writes=[stage])
        copy_op(P, ev_eng(C), dst[:, k0:k1, :], sv, [stage], [C.wres])


def dq_factory(C):
    qs = ["sp", "act", "pool"]
    C.dqi = 0

    def dq():
        C.dqi = (C.dqi + 1) % 2
        return qs[C.dqi]
    C.dq = dq


def rwkv_pass1(P, D, l, S, x_in, scr):
    NCH = S // 64
    with P.scope():
        C = setup_common(P)
        dq_factory(C)
        C.wres = P.res("weights")
        C.junk = Tl(P, "junk", [128, 1024])
        stage = Tl(P, "wstage", [128, 2048])
        wr = Tl(P, "wrkv", [128, 3 * 8, 1024], BF16)
        for j in range(3):
            load_w_bf16(C, wr.t[:, j * 8:(j + 1) * 8, :], D["rwkv_w_rkv"][l, j], 1024, 1024, stage)
        w1 = Tl(P, "w1", [128, 8, 64], BF16)
        load_w_bf16(C, w1.t, D["rwkv_w1"][l], 1024, 64, stage)
        a1 = Tl(P, "a1", [128, 8, 64], BF16)
        load_w_bf16(C, a1.t, D["rwkv_a1"][l], 1024, 64, stage)
        g1 = Tl(P, "g1", [128, 8, 160], BF16)
        load_w_bf16(C, g1.t, D["rwkv_g1"][l], 1024, 160, stage)
        if l > 0:
            v1 = Tl(P, "v1", [128, 8, 32], BF16)
            load_w_bf16(C, v1.t, D["rwkv_v1"][l - 1], 1024, 32, stage)
            v2 = Tl(P, "v2", [65, 1024])
            P.op("dve", lambda e: e.memset(v2[:], 0.0), writes=[C.wres])
            P.dma("sp", v2[0:32, :], D["rwkv_v2"][l - 1], writes=[C.wres])
            P.dma("sp", v2[64:65, :], D["rwkv_v0"][l - 1:l, :], writes=[C.wres])
        w2 = Tl(P, "w2", [65, 1024])
        P.dma("sp", w2[0:64, :], D["rwkv_w2"][l], writes=[C.wres])
        P.dma("sp", w2[64:65, :], D["rwkv_w0"][l:l + 1, :], writes=[C.wres])
        a2 = Tl(P, "a2", [65, 1024])
        P.dma("act", a2[0:64, :], D["rwkv_a2"][l], writes=[C.wres])
        P.dma("act", a2[64:65, :], D["rwkv_a0"][l:l + 1, :], writes=[C.wres])
        g2a = Tl(P, "g2a", [128, 1024])
        g2b = Tl(P, "g2b", [32, 1024])
        P.dma("sp", g2a[:], D["rwkv_g2"][l, 0:128, :], writes=[C.wres])
        P.dma("sp", g2b[:], D["rwkv_g2"][l, 128:160, :], writes=[C.wres])
        mixT = Tl(P, "mixT", [128, 6, 8])
        P.dma("sp", mixT[:], D["rwkv_mix"][l].rearrange("j (k p) -> p j k", p=128), writes=[C.wres],
              allow_slow_non_contiguous=True)
        kk_t = load_bcast(P, "k_k", D["rwkv_k_k"][l], 1024, "act")
        ka_t = load_bcast(P, "k_a", D["rwkv_k_a"][l], 1024, "sp")
        rk_t = load_bcast(P, "r_k", D["rwkv_r_k"][l].rearrange("h n -> (h n)"), 1024, "sp")
        WR = [C.wres, kk_t, ka_t, rk_t]

        xt = Tl(P, "xt", [128, 1024]); xp = Tl(P, "xp", [128, 1024])
        xT = Tl(P, "xT", [128, 8, 128]); xxT = Tl(P, "xxT", [128, 8, 128])
        tmpA = Tl(P, "tmpA", [128, 8, 128]); tmpB = Tl(P, "tmpB", [128, 8, 128])
        xm = [Tl(P, f"xm{j}", [128, 8, 128], BF16) for j in range(6)]
        r_t = Tl(P, "r", [128, 1024]); k_t = Tl(P, "k", [128, 1024]); v_t = Tl(P, "v", [128, 1024])
        sg_t = Tl(P, "sg", [128, 1024]); a_t = Tl(P, "a", [128, 1024]); g_t = Tl(P, "g", [128, 1024])
        kn_t = Tl(P, "kn", [128, 1024]); kp_t = Tl(P, "kp", [128, 1024]); t1 = Tl(P, "t1", [128, 1024])
        t2 = Tl(P, "t2", [128, 1024]); vf_t = Tl(P, "vf", [128, 1024])
        l1w = Tl(P, "l1w", [65, 128]); l1a = Tl(P, "l1a", [65, 128]); l1v = Tl(P, "l1v", [65, 128])
        l1ga = Tl(P, "l1ga", [128, 128]); l1gb = Tl(P, "l1gb", [32, 128])
        s16 = Tl(P, "s16", [128, 16]); s16b = Tl(P, "s16b", [128, 16])
        P.op("dve", lambda e: e.memset(l1w[:], 1.0), writes=[l1w])
        P.op("dve", lambda e: e.memset(l1a[:], 1.0), writes=[l1a])
        P.op("dve", lambda e: e.memset(l1v[:], 0.0), writes=[l1v])
        P.op("dve", lambda e: e.memset(l1v[64:65, :], 1.0), [l1v], [l1v])

        def v3(t):
            return t[:].rearrange("p (h k) -> p h k", k=64)

        for c in range(NCH):
            for b in range(2):
                P.dma("sp", xt[b * 64:(b + 1) * 64, :], x_in[b, c * 64:(c + 1) * 64, :], writes=[xt])
            if c == 0:
                P.op("pool", lambda e: e.memset(xp[:], 0.0), writes=[xp])
                for b in range(2):
                    P.dma("act", xp[b * 64 + 1:(b + 1) * 64, :], x_in[b, 0:63, :], writes=[xp])
            else:
                for b in range(2):
                    P.dma("act", xp[b * 64:(b + 1) * 64, :], x_in[b, c * 64 - 1:c * 64 + 63, :], writes=[xp])
            if l > 0:
                P.dma("sp", vf_t[:], scr["vf"][c], writes=[vf_t])
            bA, bB = bank(C), bank(C)
            for kc in range(8):
                bk = bA if kc < 4 else bB
                P.tr(bk[:, (kc % 4) * 128:(kc % 4 + 1) * 128], xt[:, kc * 128:(kc + 1) * 128], C.ident[:], [xt, C.ident], [bk])
            copy_op(P, "act", xT[:, 0:4, :], bA[:].rearrange("p (k n) -> p k n", n=128), [bA], [xT])
            copy_op(P, "dve", xT[:, 4:8, :], bB[:].rearrange("p (k n) -> p k n", n=128), [bB], [xT])
            bA, bB = bank(C), bank(C)
            for kc in range(8):
                bk = bA if kc < 4 else bB
                P.tr(bk[:, (kc % 4) * 128:(kc % 4 + 1) * 128], xp[:, kc * 128:(kc + 1) * 128], C.ident[:], [xp, C.ident], [bk])
            tt(P, "dve", xxT[:, 0:4, :], bA[:].rearrange("p (k n) -> p k n", n=128), xT[:, 0:4, :], ALU.subtract, [bA, xT], [xxT])
            tt(P, "dve", xxT[:, 4:8, :], bB[:].rearrange("p (k n) -> p k n", n=128), xT[:, 4:8, :], ALU.subtract, [bB, xT], [xxT])
            for j in range(6):
                eng = "dve" if j % 2 == 0 else "pool"
                tmp = tmpA if j % 2 == 0 else tmpB
                mb = mixT[:, j, :].unsqueeze(2).broadcast_to([128, 8, 128])
                tt(P, eng, tmp[:], xxT[:], mb, ALU.mult, [xxT, C.wres], [tmp])
                tt(P, eng, xm[j][:], tmp[:], xT[:], ALU.add, [tmp, xT], [xm[j]])
            for j, dst in enumerate((r_t, k_t, v_t)):
                for hf in range(2):
                    bk = bank(C)
                    for kc in range(8):
                        P.mm(bk[:], xm[j][:, kc, :], wr[:, j * 8 + kc, hf * 512:(hf + 1) * 512], start=(kc == 0), stop=(kc == 7),
                             reads=[xm[j], C.wres], writes=[bk])
                    copy_op(P, ev_eng(C), dst[:, hf * 512:(hf + 1) * 512], bk[:], [bk], [dst])
            bk = bank(C)
            for kc in range(8):
                P.mm(bk[0:64, 0:128], w1[:, kc, :], xm[3][:, kc, :], start=(kc == 0), stop=(kc == 7), reads=[xm[3], C.wres], writes=[bk])
            for kc in range(8):
                P.mm(bk[0:64, 128:256], a1[:, kc, :], xm[4][:, kc, :], start=(kc == 0), stop=(kc == 7), reads=[xm[4], C.wres], writes=[bk])
            if l > 0:
                for kc in range(8):
                    P.mm(bk[0:32, 256:384], v1[:, kc, :], xm[2][:, kc, :], start=(kc == 0), stop=(kc == 7), reads=[xm[2], C.wres], writes=[bk])
            P.op("act", lambda e, bk=bk: e.activation(l1w[0:64, :], bk[0:64, 0:128], AF.Tanh), [bk], [l1w])
            copy_op(P, "act", l1a[0:64, :], bk[0:64, 128:256], [bk], [l1a])
            if l > 0:
                copy_op(P, "act", l1v[0:32, :], bk[0:32, 256:384], [bk], [l1v])
            bk2 = bank(C)
            for kc in range(8):
                P.mm(bk2[:, 0:128], g1[:, kc, 0:128], xm[5][:, kc, :], start=(kc == 0), stop=(kc == 7), reads=[xm[5], C.wres], writes=[bk2])
            for kc in range(8):
                P.mm(bk2[0:32, 128:256], g1[:, kc, 128:160], xm[5][:, kc, :], start=(kc == 0), stop=(kc == 7), reads=[xm[5], C.wres], writes=[bk2])
            P.op("act", lambda e, bk2=bk2: e.activation(l1ga[:], bk2[:, 0:128], AF.Sigmoid), [bk2], [l1ga])
            P.op("act", lambda e, bk2=bk2: e.activation(l1gb[:], bk2[0:32, 128:256], AF.Sigmoid), [bk2], [l1gb])
            for hf in range(2):
                cs = slice(hf * 512, (hf + 1) * 512)
                bk = bank(C)
                P.mm(bk[:], l1w[:], w2[:, cs], reads=[l1w, C.wres], writes=[bk])
                P.op("act", lambda e, bk=bk, cs=cs: e.activation(sg_t[:, cs], bk[:], AF.Sigmoid), [bk], [sg_t])
                bk = bank(C)
                P.mm(bk[:], l1a[:], a2[:, cs], reads=[l1a, C.wres], writes=[bk])
                P.op("act", lambda e, bk=bk, cs=cs: e.activation(a_t[:, cs], bk[:], AF.Sigmoid), [bk], [a_t])
                bk = bank(C)
                P.mm(bk[:], l1ga[:], g2a[:, cs], start=True, stop=False, reads=[l1ga, C.wres], writes=[bk])
                P.mm(bk[:], l1gb[:], g2b[:, cs], start=False, stop=True, reads=[l1gb, C.wres], writes=[bk])
                copy_op(P, "dve", g_t[:, cs], bk[:], [bk], [g_t])
                if l > 0:
                    bk = bank(C)
                    P.mm(bk[:], l1v[:], v2[:, cs], reads=[l1v, C.wres], writes=[bk])
                    P.op("act", lambda e, bk=bk, cs=cs: e.activation(t1[:, cs], bk[:], AF.Sigmoid), [bk], [t1])
            if l > 0:
                tt(P, "dve", t2[:], vf_t[:], v_t[:], ALU.subtract, [vf_t, v_t], [t2])
                tt(P, "dve", t2[:], t2[:], t1[:], ALU.mult, [t2, t1], [t2])
                tt(P, "dve", v_t[:], v_t[:], t2[:], ALU.add, [v_t, t2], [v_t])
            else:
                P.dma("sp", scr["vf"][c], v_t[:], reads=[v_t])
            tt(P, "pool", kn_t[:], k_t[:], kk_t[:], ALU.mult, [k_t, kk_t], [kn_t])
            tt(P, "pool", t2[:], kn_t[:], kn_t[:], ALU.mult, [kn_t], [t2])
            P.op("dve", lambda e: e.tensor_reduce(s16[:], v3(t2), AX.X, ALU.add), [t2], [s16])
            rsqrt_op(P, s16, 0, 16, 1e-24, True)
            tt(P, "dve", v3(kn_t), v3(kn_t), s16[:].unsqueeze(2).broadcast_to([128, 16, 64]), ALU.mult, [kn_t, s16], [kn_t])
            stt(P, "dve", t2[:], a_t[:], -1.0, ka_t[:], ALU.add, ALU.mult, [a_t, ka_t], [t2])
            stt(P, "dve", kp_t[:], t2[:], 1.0, k_t[:], ALU.add, ALU.mult, [t2, k_t], [kp_t])
            tt(P, "pool", a_t[:], kn_t[:], a_t[:], ALU.mult, [kn_t, a_t], [a_t])
            tt(P, "dve", t2[:], r_t[:], kp_t[:], ALU.mult, [r_t, kp_t], [t2])
            tt(P, "pool", t2[:], t2[:], rk_t[:], ALU.mult, [t2, rk_t], [t2])
            P.op("dve", lambda e: e.tensor_reduce(s16b[:], v3(t2), AX.X, ALU.add), [t2], [s16b])
            tt(P, "dve", v3(t2), v3(v_t), s16b[:].unsqueeze(2).broadcast_to([128, 16, 64]), ALU.mult, [v_t, s16b], [t2])
            for nm, tl in (("r", r_t), ("kp", kp_t), ("v", v_t), ("sg", sg_t), ("kn", kn_t), ("kka", a_t), ("bonus", t2), ("g", g_t)):
                P.dma(C.dq(), scr[nm][c], tl[:], reads=[tl])


def rwkv_pass2(P, D, l, S, x_in, x_out, scr):
    NCH = S // 64
    import os as _os
    CUT = float(_os.environ.get('CUT', '99'))
    with P.scope():
        C = setup_common(P)
        dq_factory(C)
        C.wres = P.res("weights")
        stage = Tl(P, "wstage", [128, 1024])
        wo = Tl(P, "wo", [128, 8, 1024], BF16)
        load_w_bf16(C, wo.t, D["rwkv_w_o"][l], 1024, 1024, stage)
        lnxg = load_bcast(P, "lnxg", D["rwkv_lnx_g"][l], 1024, "sp")
        lnxb = load_bcast(P, "lnxb", D["rwkv_lnx_b"][l], 1024, "act")
        lng = load_bcast(P, "lng", D["ln_g"][l, 0], 1024, "sp")
        lnb = load_bcast(P, "lnb", D["ln_b"][l, 0], 1024, "sp")
        iu = Tl(P, "iu", [128, 128]); su = Tl(P, "su", [128, 128]); sl = Tl(P, "sl", [128, 128])
        triI = Tl(P, "triI", [128, 128]); triS = Tl(P, "triS", [128, 128]); bsel = Tl(P, "bsel", [128, 32])
        for m, cmpop, pat, cm in ((iu, ALU.is_ge, 1, -1), (su, ALU.is_gt, 1, -1), (sl, ALU.is_gt, -1, 1)):
            P.op("pool", lambda e, m=m: e.memset(m[:], 1.0), writes=[m])
            P.op("pool", lambda e, m=m, cmpop=cmpop, pat=pat, cm=cm: e.affine_select(
                m[:], m[:], [[pat, 128]], cmpop, 0.0, base=0, channel_multiplier=cm), [m], [m])
        P.op("pool", lambda e: e.memset(iu[0:64, 64:128], 0.0), [iu], [iu])
        P.op("pool", lambda e: e.memset(su[0:64, 64:128], 0.0), [su], [su])
        P.op("pool", lambda e: e.memset(sl[64:128, 0:64], 0.0), [sl], [sl])
        P.op("dve", lambda e: e.tensor_scalar(triI[:], iu[:], -EXPM05, None, ALU.mult), [iu], [triI])
        P.op("dve", lambda e: e.tensor_scalar(triS[:], su[:], -EXPM05, None, ALU.mult), [su], [triS])
        P.op("pool", lambda e: e.memset(bsel[:], 0.0), writes=[bsel])
        P.op("pool", lambda e: e.memset(bsel[0:64, 0:16], -EXPM05), [bsel], [bsel])
        P.op("pool", lambda e: e.memset(bsel[64:128, 16:32], -EXPM05), [bsel], [bsel])
        ST = Tl(P, "ST", [64, 16, 128])
        P.op("dve", lambda e: e.memset(ST[:], 0.0), writes=[ST])
        Vblk = Tl(P, "Vblk", [128, 16, 128]); Ublk = Tl(P, "Ublk", [128, 16, 128])
        P.op("pool", lambda e: e.memset(Vblk[:], 0.0), writes=[Vblk])
        P.op("pool", lambda e: e.memset(Ublk[:], 0.0), writes=[Ublk])
        r_t = Tl(P, "r", [128, 1024]); kp_t = Tl(P, "kp", [128, 1024]); v_t = Tl(P, "v", [128, 1024])
        sg_t = Tl(P, "sg", [128, 1024]); kn_t = Tl(P, "kn", [128, 1024]); kka_t = Tl(P, "kka", [128, 1024])
        Ep = Tl(P, "Ep", [128, 1024]); Em = Tl(P, "Em", [128, 1024]); Epp = Tl(P, "Epp", [128, 1024])
        bon_t = Ep; g_t = Epp; xt = sg_t
        C.junk = Em
        Y = Tl(P, "Y", [128, 1024]); Rh = Tl(P, "Rh", [128, 1024]); t2 = Tl(P, "t2", [128, 1024])
        oT = Tl(P, "oT", [128, 8, 128], BF16)
        gamT = Tl(P, "gamT", [64, 16, 2])
        st = Tl(P, "st", [128, 8]); s16 = Tl(P, "s16", [128, 16]); s16b = Tl(P, "s16b", [128, 16])
        G = []
        for gi in range(4):
            g = Ctx()
            g.qT = Tl(P, f"qT{gi}", [64, 16, 128])
            g.Q = Tl(P, f"Q{gi}", [128, 4, 128]); g.QT = Tl(P, f"QT{gi}", [128, 4, 128])
            g.Aak = Tl(P, f"Aak{gi}", [128, 4, 128]); g.Arb = Tl(P, f"Arb{gi}", [128, 4, 128]); g.Ark = Tl(P, f"Ark{gi}", [128, 4, 128])
            g.X = Tl(P, f"X{gi}", [128, 4, 128])
            g.WT = g.qT
            G.append(g)

        def v3(t):
            return t[:].rearrange("p (h k) -> p h k", k=64)

        def b4(bk):
            return bk[:].rearrange("p (h n) -> p h n", n=128)

        for c in range(NCH):
            for nm, tl in (("r", r_t), ("kp", kp_t), ("v", v_t), ("sg", sg_t), ("kn", kn_t), ("kka", kka_t)):
                P.dma(C.dq(), tl[:], scr[nm][c], writes=[tl])
            for hf in range(2):
                cs = slice(hf * 512, (hf + 1) * 512)
                bk = bank(C)
                P.mm(bk[:], triI[:], sg_t[:, cs], reads=[triI, sg_t], writes=[bk])
                P.op("act", lambda e, bk=bk, cs=cs: e.activation(Ep[:, cs], bk[:], AF.Exp), [bk], [Ep])
                P.op("act", lambda e, bk=bk, cs=cs: e.activation(Em[:, cs], bk[:], AF.Exp, scale=-1.0), [bk], [Em])
                bk = bank(C)
                P.mm(bk[:], triS[:], sg_t[:, cs], reads=[triS, sg_t], writes=[bk])
                P.op("act", lambda e, bk=bk, cs=cs: e.activation(Epp[:, cs], bk[:], AF.Exp), [bk], [Epp])
            bk = bank(C)
            for h in range(16):
                P.mm(bk[0:64, h * 32:(h + 1) * 32], sg_t[:, h * 64:(h + 1) * 64], bsel[:], reads=[sg_t, bsel], writes=[bk])
            P.op("act", lambda e, bk=bk: e.activation(gamT[:], bk[0:64, :].rearrange("p (h b r) -> p h b r", b=2, r=16)[:, :, :, 0], AF.Exp), [bk], [gamT])
            stt(P, "dve", kn_t[:], kn_t[:], -1.0, Epp[:], ALU.mult, ALU.mult, [kn_t, Epp], [kn_t])
            tt(P, "pool", kka_t[:], kka_t[:], Em[:], ALU.mult, [kka_t, Em], [kka_t])
            tt(P, "dve", kp_t[:], kp_t[:], Em[:], ALU.mult, [kp_t, Em], [kp_t])
            tt(P, "pool", r_t[:], r_t[:], Ep[:], ALU.mult, [r_t, Ep], [r_t])
            copy_op(P, "pool", Vblk[0:64, :, 0:64], v3(v_t)[0:64], [v_t], [Vblk])
            copy_op(P, "pool", Vblk[64:128, :, 64:128], v3(v_t)[64:128], [v_t], [Vblk])
            quant = (kn_t, kka_t, kp_t, r_t)
            P.dma("sp", bon_t[:], scr["bonus"][c], writes=[bon_t])
            P.dma("act", g_t[:], scr["g"][c], writes=[g_t])
            for b in range(2):
                P.dma("sp", xt[b * 64:(b + 1) * 64, :], x_in[b, c * 64:(c + 1) * 64, :], writes=[xt])
            if CUT <= 1:
                continue
            for gi, g in enumerate(G):
                for q in range(4):
                    bk = bank(C)
                    for h4 in range(4):
                        h = gi * 4 + h4
                        P.tr(bk[0:64, h4 * 128:(h4 + 1) * 128], quant[q][:, h * 64:(h + 1) * 64], C.ident[:], [quant[q], C.ident], [bk])
                    copy_op(P, ev_eng(C), g.qT[:, q * 4:(q + 1) * 4, :], b4(bk)[0:64], [bk], [g.qT])
            if CUT <= 2:
                continue
            for gi, g in enumerate(G):
                for (dst, lq, rq, msk) in ((g.QT, 1, 0, su), (g.Q, 0, 1, sl), (g.Aak, 2, 0, su), (g.Arb, 1, 3, iu), (g.Ark, 2, 3, iu)):
                    bk = bank(C)
                    for h4 in range(4):
                        P.mm(bk[:, h4 * 128:(h4 + 1) * 128], g.qT[:, lq * 4 + h4, :], g.qT[:, rq * 4 + h4, :], reads=[g.qT], writes=[bk])
                    tt(P, "dve", dst[:], b4(bk), msk[:].unsqueeze(1).broadcast_to([128, 4, 128]), ALU.mult, [bk, msk], [dst])
            if CUT <= 3:
                continue
            for gi, g in enumerate(G):
                bk = bank(C)
                for h4 in range(4):
                    h = gi * 4 + h4
                    P.mm(bk[:, h4 * 128 + 64:(h4 + 1) * 128], g.Aak[:, h4, :], v_t[:, h * 64:(h + 1) * 64], reads=[g.Aak, v_t], writes=[bk])
                copy_op(P, ev_eng(C), g.X[:, :, 64:128], b4(bk)[:, :, 64:128], [bk], [g.X])
                copy_op(P, "pool", g.X[:, :, 0:64], v3(kn_t)[:, gi * 4:(gi + 1) * 4, :], [kn_t], [g.X])
            if CUT <= 4:
                continue
            for j in range(6):
                for gi, g in enumerate(G):
                    bk = bank(C)
                    for h4 in range(4):
                        P.mm(bk[:, h4 * 128:(h4 + 1) * 128], g.QT[:, h4, :], g.X[:, h4, :], reads=[g.QT, g.X], writes=[bk])
                    if j < 5:
                        bq = bank(C); bqt = bank(C)
                        for h4 in range(4):
                            P.mm(bq[:, h4 * 128:(h4 + 1) * 128], g.QT[:, h4, :], g.Q[:, h4, :], reads=[g.QT, g.Q], writes=[bq])
                        for h4 in range(4):
                            P.mm(bqt[:, h4 * 128:(h4 + 1) * 128], g.Q[:, h4, :], g.QT[:, h4, :], reads=[g.QT, g.Q], writes=[bqt])
                    tt(P, "dve", g.X[:], b4(bk), g.X[:], ALU.add, [bk, g.X], [g.X])
                    if j < 5:
                        copy_op(P, "act", g.Q[:], b4(bq), [bq], [g.Q])
                        copy_op(P, ev_eng(C), g.QT[:], b4(bqt), [bqt], [g.QT])
            if CUT <= 5:
                continue
            for gi, g in enumerate(G):
                bk = bank(C)
                for h4 in range(4):
                    h = gi * 4 + h4
                    EV = _os.environ.get("EV", "0")
                    if EV in ("0", "1"):
                        P.mm(bk[:, h4 * 128:h4 * 128 + 64], g.Arb[:, h4, :], g.X[:, h4, 0:64], reads=[g.Arb, g.X], writes=[bk])
                    if EV in ("0", "2"):
                        P.mm(bk[:, h4 * 128 + 64:(h4 + 1) * 128], g.Arb[:, h4, :], g.X[:, h4, 64:128], start=True, stop=False, reads=[g.Arb, g.X], writes=[bk])
                        P.mm(bk[:, h4 * 128 + 64:(h4 + 1) * 128], g.Ark[:, h4, :], v_t[:, h * 64:(h + 1) * 64], start=False, stop=True, reads=[g.Ark, v_t], writes=[bk])
                    if EV == "3":
                        P.mm(bk[:, h4 * 128:(h4 + 1) * 128], g.Arb[:, h4, :], g.X[:, h4, :], reads=[g.Arb, g.X], writes=[bk])
                hs = slice(gi * 4, (gi + 1) * 4)
                EVC = _os.environ.get("EVC", "0")
                if EVC != "1":
                    tt(P, "dve", v3(Rh)[:, hs, :], b4(bk)[:, :, 0:64], v3(r_t)[:, hs, :], ALU.add, [bk, r_t], [Rh])
                if EVC != "2":
                    copy_op(P, "dve", v3(Y)[:, hs, :], b4(bk)[:, :, 64:128], [bk], [Y])
            if CUT <= 5.5:
                continue
            for gi, g in enumerate(G):
                bk = bank(C); bk2 = bank(C)
                for h4 in range(4):
                    h = gi * 4 + h4
                    P.tr(bk[0:64, h4 * 128:(h4 + 1) * 128], g.X[:, h4, 0:64], C.ident[:], [g.X, C.ident], [bk])
                    P.tr(bk2[0:64, h4 * 128:(h4 + 1) * 128], Rh[:, h * 64:(h + 1) * 64], C.ident[:], [Rh, C.ident], [bk2])
                copy_op(P, "act", g.WT[:, 0:4, :], b4(bk)[0:64], [bk], [g.WT])
                copy_op(P, "dve", g.WT[:, 4:8, :], b4(bk2)[0:64], [bk2], [g.WT])
            if CUT <= 6:
                continue
            for gi, g in enumerate(G):
                hs = slice(gi * 4, (gi + 1) * 4)
                bu = bank(C); by = bank(C)
                for h4 in range(4):
                    h = gi * 4 + h4
                    P.mm(bu[:, h4 * 128:(h4 + 1) * 128], g.WT[:, h4, :], ST[:, h, :], reads=[g.WT, ST], writes=[bu])
                for h4 in range(4):
                    h = gi * 4 + h4
                    P.mm(by[:, h4 * 128:(h4 + 1) * 128], g.WT[:, 4 + h4, :], ST[:, h, :], reads=[g.WT, ST], writes=[by])
                for b in range(2):
                    ps_ = slice(b * 64, (b + 1) * 64)
                    tt(P, "dve", Ublk[ps_, hs, b * 64:(b + 1) * 64], b4(bu)[ps_, :, b * 64:(b + 1) * 64], g.X[ps_, :, 64:128], ALU.add,
                       [bu, g.X], [Ublk])
                    tt(P, "dve", v3(Y)[ps_, hs, :], b4(by)[ps_, :, b * 64:(b + 1) * 64], v3(Y)[ps_, hs, :], ALU.add, [by, Y], [Y])
                bs = bank(C)
                for h4 in range(4):
                    h = gi * 4 + h4
                    P.mm(bs[0:64, h4 * 128:(h4 + 1) * 128], kka_t[:, h * 64:(h + 1) * 64], Ublk[:, h, :], start=True, stop=False,
                         reads=[kka_t, Ublk], writes=[bs])
                    P.mm(bs[0:64, h4 * 128:(h4 + 1) * 128], kp_t[:, h * 64:(h + 1) * 64], Vblk[:, h, :], start=False, stop=True,
                         reads=[kp_t, Vblk], writes=[bs])
                tt(P, "dve", ST[:, hs, :], b4(bs)[0:64], ST[:, hs, :], ALU.add, [bs, ST], [ST])
                tt(P, "dve", ST[:, hs, :].rearrange("p h (b v) -> p h b v", b=2),
                   ST[:, hs, :].rearrange("p h (b v) -> p h b v", b=2),
                   gamT[:, hs, :].unsqueeze(3).broadcast_to([64, 4, 2, 64]), ALU.mult, [ST, gamT], [ST])
            if CUT <= 7:
                continue
            P.op("dve", lambda e: e.tensor_reduce(s16[:], v3(Y), AX.X, ALU.add), [Y], [s16])
            tt(P, "pool", t2[:], Y[:], Y[:], ALU.mult, [Y], [t2])
            P.op("dve", lambda e: e.tensor_reduce(s16b[:], v3(t2), AX.X, ALU.add), [t2], [s16b])
            P.op("dve", lambda e: e.tensor_scalar(s16[:], s16[:], 1.0 / 64, None, ALU.mult), [s16], [s16])
            tt(P, "pool", t2[:, 0:16], s16[:], s16[:], ALU.mult, [s16], [t2])
            stt(P, "dve", s16b[:], s16b[:], 1.0 / 64, t2[:, 0:16], ALU.mult, ALU.subtract, [s16b, t2], [s16b])
            rsqrt_op(P, s16b, 0, 16, GN_EPS, False)
            tt(P, "dve", v3(Y), v3(Y), s16[:].unsqueeze(2).broadcast_to([128, 16, 64]), ALU.subtract, [Y, s16], [Y])
            tt(P, "dve", v3(Y), v3(Y), s16b[:].unsqueeze(2).broadcast_to([128, 16, 64]), ALU.mult, [Y, s16b], [Y])
            tt(P, "pool", Y[:], Y[:], lnxg[:], ALU.mult, [Y, lnxg], [Y])
            tt(P, "pool", Y[:], Y[:], lnxb[:], ALU.add, [Y, lnxb], [Y])
            tt(P, "dve", Y[:], Y[:], bon_t[:], ALU.add, [Y, bon_t], [Y])
            tt(P, "dve", Y[:], Y[:], g_t[:], ALU.mult, [Y, g_t], [Y])
            if CUT <= 8:
                continue
            bA, bB = bank(C), bank(C)
            for kc in range(8):
                bk = bA if kc < 4 else bB
                P.tr(bk[:, (kc % 4) * 128:(kc % 4 + 1) * 128], Y[:, kc * 128:(kc + 1) * 128], C.ident[:], [Y, C.ident], [bk])
            copy_op(P, "act", oT[:, 0:4, :], b4(bA), [bA], [oT])
            copy_op(P, "dve", oT[:, 4:8, :], b4(bB), [bB], [oT])
            for hf in range(2):
                cs = slice(hf * 512, (hf + 1) * 512)
                bk = bank(C)
                for kc in range(8):
                    P.mm(bk[:], oT[:, kc, :], wo[:, kc, cs], start=(kc == 0), stop=(kc == 7), reads=[oT, C.wres], writes=[bk])
                copy_op(P, "act", t2[:, cs], bk[:], [bk], [t2])
                stt(P, "dve", t2[:, cs], xt[:, cs], ALPHA, t2[:, cs], ALU.mult, ALU.add, [xt, t2], [t2])
            layer_norm_tile(C, t2, Rh, lng, lnb, st)
            for b in range(2):
                P.dma("act", x_out[b, c * 64:(c + 1) * 64, :], Rh[b * 64:(b + 1) * 64, :], reads=[Rh])


SW_LIMIT = 7.0
SW_ALPHA = 1.702


def moe_stage(P, D, l, S, x_in, x_out, NE=32):
    T = 2 * S
    TQ = min(1024, T)
    NQ = T // TQ
    NT = TQ // 128
    NB = TQ // 512
    xin = x_in.rearrange("b s d -> (b s) d")
    xout = x_out.rearrange("b s d -> (b s) d")
    wgu_d = D["moe_w_gu"][l]
    wdn_d = D["moe_w_dn"][l]
    with P.scope():
        C = setup_common(P)
        dq_factory(C)
        C.wres = P.res("weights")
        wr = Tl(P, "wr", [128, 8, 32])
        P.dma("sp", wr[:], D["moe_router_w"][l].rearrange("(k p) e -> p k e", p=128), writes=[C.wres])
        br = load_bcast(P, "br", D["moe_router_b"][l], 32, "act")
        bdn = Tl(P, "bdn", [32, 1024])
        P.dma("sp", bdn[:], D["moe_b_dn"][l], writes=[C.wres])
        xT32f = Tl(P, "xT32", [128, 1024])
        C.junk = xT32f
        bgu_raw = xT32f
        bguT = Tl(P, "bguT", [128, 16, 32])
        for half in range(2):
            P.dma("act", bgu_raw[0:32, :], D["moe_b_gu"][l][:, half * 1024:(half + 1) * 1024], writes=[bgu_raw])
            for cb in range(2):
                bk = bank(C)
                for c4 in range(4):
                    cc = cb * 4 + c4
                    P.tr(bk[:, c4 * 32:(c4 + 1) * 32], bgu_raw[0:32, cc * 128:(cc + 1) * 128], C.ident[0:32, 0:32], [bgu_raw, C.ident], [bk])
                copy_op(P, "dve", bguT[:, half * 8 + cb * 4:half * 8 + (cb + 1) * 4, :], bk[:, 0:128].rearrange("p (c e) -> p c e", e=32), [bk], [bguT])
        lng = load_bcast(P, "lng", D["ln_g"][l, 1], 1024, "sp")
        lnb = load_bcast(P, "lnb", D["ln_b"][l, 1], 1024, "act")
        xT = Tl(P, "xT", [128, 8, TQ], BF16)
        acc = Tl(P, "acc", [128, NT, 1024])
        G = Tl(P, "G", [128, NT, 32])
        Wgu = [Tl(P, f"Wgu{i}", [128, 8, 2048], BF16) for i in range(2)]
        Wdn = [Tl(P, f"Wdn{i}", [128, 8, 1024], BF16) for i in range(2)]
        actT = [Tl(P, f"actT{i}", [128, 8, 512], BF16) for i in range(2)]
        xt = Tl(P, "xt", [128, 1024]); z = Tl(P, "z", [128, 1024])
        xT32 = xT32f
        x3 = xT32f[:].rearrange("p (k n) -> p k n", n=128)
        lg = Tl(P, "lg", [128, 32]); t8 = Tl(P, "t8", [128, 8]); msk = Tl(P, "msk", [128, 32]); st = Tl(P, "st", [128, 8])
        GT = Tl(P, "GT", [32, 128])
        tmp = [[Tl(P, f"sw{i}_{j}", [128, 512]) for j in range(3)] for i in range(2)]

        def load_expert(e, buf):
            for k0 in range(0, 8, 2):
                P.dma("pool", Wgu[buf][:, k0:k0 + 2, :], wgu_d[e, k0 * 128:(k0 + 2) * 128, :].rearrange("(k p) n -> p k n", p=128), writes=[Wgu[buf]])
            for k0 in range(0, 8, 4):
                P.dma("pool", Wdn[buf][:, k0:k0 + 4, :], wdn_d[e, k0 * 128:(k0 + 4) * 128, :].rearrange("(k p) n -> p k n", p=128), writes=[Wdn[buf]])

        seq = [(q, e) for q in range(NQ) for e in range(NE)]
        load_expert(0, 0)
        si = 0
        for q in range(NQ):
            t0 = q * TQ
            for t in range(NT):
                rows = slice(t0 + t * 128, t0 + (t + 1) * 128)
                P.dma("sp", xt[:], xin[rows, :], writes=[xt])
                bA, bB = bank(C), bank(C)
                for kc in range(8):
                    bk = bA if kc < 4 else bB
                    P.tr(bk[:, (kc % 4) * 128:(kc % 4 + 1) * 128], xt[:, kc * 128:(kc + 1) * 128], C.ident[:], [xt, C.ident], [bk])
                copy_op(P, "act", x3[:, 0:4, :], bA[:].rearrange("p (k n) -> p k n", n=128), [bA], [xT32])
                copy_op(P, "dve", x3[:, 4:8, :], bB[:].rearrange("p (k n) -> p k n", n=128), [bB], [xT32])
                copy_op(P, "pool", xT[:, :, t * 128:(t + 1) * 128], x3, [xT32], [xT])
                bk = bank(C)
                for kc in range(8):
                    P.mm(bk[:, 0:32], x3[:, kc, :], wr[:, kc, :], start=(kc == 0), stop=(kc == 7), reads=[xT32, C.wres], writes=[bk])
                tt(P, "dve", lg[:], bk[:, 0:32], br[:], ALU.add, [bk, br], [lg])
                P.op("dve", lambda e: e.max(t8[:], lg[:]), [lg], [t8])
                P.op("dve", lambda e: e.tensor_scalar(msk[:], lg[:], t8[:, 3:4], None, ALU.is_ge), [lg, t8], [msk])
                P.op("dve", lambda e: e.tensor_scalar(st[:, 0:1], t8[:, 0:1], -1.0, None, ALU.mult), [t8], [st])
                P.op("act", lambda e: e.activation(lg[:], lg[:], AF.Exp, bias=st[:, 0:1]), [lg, st], [lg])
                tt(P, "dve", lg[:], lg[:], msk[:], ALU.mult, [lg, msk], [lg])
                P.op("dve", lambda e: e.tensor_reduce(st[:, 1:2], lg[:], AX.X, ALU.add), [lg], [st])
                P.op("dve", lambda e: e.reciprocal(st[:, 2:3], st[:, 1:2]), [st], [st])
                P.op("dve", lambda e, t=t: e.tensor_scalar(G[:, t, :], lg[:], st[:, 2:3], None, ALU.mult), [lg, st], [G])
            P.op("pool", lambda e: e.memset(acc[:], 0.0), writes=[acc])
            for e in range(NE):
                buf = si % 2
                if si + 1 < len(seq):
                    load_expert(seq[si + 1][1], (si + 1) % 2)
                si += 1
                for blk in range(NB):
                    ts = slice(blk * 512, (blk + 1) * 512)
                    aT = actT[blk % 2]
                    for fc in range(8):
                        tp = tmp[fc % 2]
                        bg, bu = bank(C), bank(C)
                        for kc in range(8):
                            P.mm(bg[:], Wgu[buf][:, kc, fc * 128:(fc + 1) * 128], xT[:, kc, ts], start=(kc == 0), stop=(kc == 7),
                                 reads=[Wgu[buf], xT], writes=[bg])
                        for kc in range(8):
                            P.mm(bu[:], Wgu[buf][:, kc, 1024 + fc * 128:1024 + (fc + 1) * 128], xT[:, kc, ts], start=(kc == 0), stop=(kc == 7),
                                 reads=[Wgu[buf], xT], writes=[bu])
                        gp, sg, u1 = tp
                        tq = gp
                        P.op("dve", lambda en, bg=bg, gp=gp, fc=fc, e=e: en.tensor_scalar(gp[:], bg[:], bguT[:, fc, e:e + 1], SW_LIMIT, ALU.add, ALU.min),
                             [bg, bguT], [gp])
                        P.op("act", lambda en, gp=gp, sg=sg: en.activation(sg[:], gp[:], AF.Sigmoid, scale=SW_ALPHA), [gp], [sg])
                        P.op("dve", lambda en, bu=bu, u1=u1, fc=fc, e=e: en.tensor_scalar(u1[:], bu[:], bguT[:, 8 + fc, e:e + 1], SW_LIMIT, ALU.add, ALU.min),
                             [bu, bguT], [u1])
                        P.op("pool", lambda en, u1=u1: en.tensor_scalar(u1[:], u1[:], -SW_LIMIT, 1.0, ALU.max, ALU.add), [u1], [u1])
                        tt(P, "pool", tq[:], gp[:], sg[:], ALU.mult, [gp, sg], [tq])
                        tt(P, "dve", aT[:, fc, :], tq[:], u1[:], ALU.mult, [tq, u1], [aT])
                    for t4 in range(4):
                        tile_i = blk * 4 + t4
                        for hf in range(2):
                            cs = slice(hf * 512, (hf + 1) * 512)
                            bk = bank(C)
                            for fc in range(8):
                                P.mm(bk[:], aT[:, fc, t4 * 128:(t4 + 1) * 128], Wdn[buf][:, fc, cs], start=(fc == 0), stop=(fc == 7),
                                     reads=[aT, Wdn[buf]], writes=[bk])
                            stt(P, "dve", acc[:, tile_i, cs], bk[:], G[:, tile_i, e:e + 1], acc[:, tile_i, cs], ALU.mult, ALU.add, [bk, G, acc], [acc])
            for t in range(NT):
                rows = slice(t0 + t * 128, t0 + (t + 1) * 128)
                bk = bank(C)
                P.tr(bk[0:32, 0:128], G[:, t, :], C.ident[:], [G, C.ident], [bk])
                copy_op(P, "act", GT[:], bk[0:32, 0:128], [bk], [GT])
                P.dma("sp", xt[:], xin[rows, :], writes=[xt])
                for hf in range(2):
                    cs = slice(hf * 512, (hf + 1) * 512)
                    bk = bank(C)
                    P.mm(bk[:], GT[:], bdn[:, cs], reads=[GT, C.wres], writes=[bk])
                    tt(P, "dve", z[:, cs], bk[:], acc[:, t, cs], ALU.add, [bk, acc], [z])
                stt(P, "dve", z[:], xt[:], ALPHA, z[:], ALU.mult, ALU.add, [xt, z], [z])
                layer_norm_tile(C, z, xt, lng, lnb, st)
                P.dma("act", xout[rows, :], xt[:], reads=[xt])


NEG = -1.0e30
SUBLN_EPS = 1e-5


def xT_block(C, xin, r0, nt, xt, xTb):
    P = C.P
    for t in range(nt):
        P.dma("sp", xt[:], xin[r0 + t * 128:r0 + (t + 1) * 128, :], writes=[xt])
        bA, bB = bank(C), bank(C)
        for kc in range(8):
            bk = bA if kc < 4 else bB
            P.tr(bk[:, (kc % 4) * 128:(kc % 4 + 1) * 128], xt[:, kc * 128:(kc + 1) * 128], C.ident[:], [xt, C.ident], [bk])
        copy_op(P, "act", xTb[:, 0:4, t * 128:(t + 1) * 128], bA[:].rearrange("p (k n) -> p k n", n=128), [bA], [xTb])
        copy_op(P, "dve", xTb[:, 4:8, t * 128:(t + 1) * 128], bB[:].rearrange("p (k n) -> p k n", n=128), [bB], [xTb])


def proj_stage(P, D, S, x_in, w_T_dram, T_scr, w_tok_dram=None, tok_scr=None):
    T = 2 * S
    NBLK = T // 512
    xin = x_in.rearrange("b s d -> (b s) d")
    with P.scope():
        C = setup_common(P)
        dq_factory(C)
        C.wres = P.res("weights")
        wT = Tl(P, "wT", [128, 8, 1024], BF16)
        for k0 in range(0, 8, 2):
            P.dma("pool", wT[:, k0:k0 + 2, :], w_T_dram[k0 * 128:(k0 + 2) * 128, :].rearrange("(k p) n -> p k n", p=128), writes=[C.wres])
        if w_tok_dram is not None:
            wK = Tl(P, "wK", [128, 8, 1024], BF16)
            for k0 in range(0, 8, 2):
                P.dma("pool", wK[:, k0:k0 + 2, :], w_tok_dram[k0 * 128:(k0 + 2) * 128, :].rearrange("(k p) n -> p k n", p=128), writes=[C.wres])
        xt = Tl(P, "xt", [128, 1024])
        xTb = [Tl(P, f"xTb{i}", [128, 8, 512], BF16) for i in range(2)]
        oT = [Tl(P, f"oT{i}", [64, 512]) for i in range(4)]
        ot = [Tl(P, f"ot{i}", [128, 1024]) for i in range(2)]
        for blk in range(NBLK):
            xb = xTb[blk % 2]
            xT_block(C, xin, blk * 512, 4, xt, xb)
            for g in range(16):
                bk = bank(C)
                for kc in range(8):
                    P.mm(bk[0:64, :], wT[:, kc, g * 64:(g + 1) * 64], xb[:, kc, :], start=(kc == 0), stop=(kc == 7), reads=[C.wres, xb], writes=[bk])
                o = oT[g % 4]
                copy_op(P, ev_eng(C), o[:], bk[0:64, :], [bk], [o])
                P.dma(C.dq(), T_scr[g, :, blk * 512:(blk + 1) * 512], o[:], reads=[o])
            if w_tok_dram is not None:
                for t4 in range(4):
                    o = ot[t4 % 2]
                    for hf in range(2):
                        cs = slice(hf * 512, (hf + 1) * 512)
                        bk = bank(C)
                        for kc in range(8):
                            P.mm(bk[:], xb[:, kc, t4 * 128:(t4 + 1) * 128], wK[:, kc, cs], start=(kc == 0), stop=(kc == 7), reads=[C.wres, xb], writes=[bk])
                        copy_op(P, ev_eng(C), o[:, cs], bk[:], [bk], [o])
                    P.dma(C.dq(), tok_scr[blk * 512 + t4 * 128:blk * 512 + (t4 + 1) * 128, :], o[:], reads=[o])


def attn_stage(P, D, j, S, qT_scr, kT_scr, v_scr, o_scr):
    import math
    l = NA + j
    lam_init = 0.8 - 0.6 * math.exp(-0.3 * l)
    NQB = S // 128
    with P.scope():
        C = setup_common(P)
        dq_factory(C)
        lamt = load_bcast(P, "lamt", D["da_lambda"][j].rearrange("a d -> (a d)"), 256, "sp")
        lsc = Tl(P, "lsc", [128, 8]); ljunk = Tl(P, "ljunk", [128, 64])
        tt(P, "dve", ljunk[:], lamt[:, 0:64], lamt[:, 64:128], ALU.mult, [lamt], [ljunk])
        P.op("dve", lambda e: e.tensor_reduce(lsc[:, 0:1], ljunk[:], AX.X, ALU.add), [ljunk], [lsc])
        tt(P, "dve", ljunk[:], lamt[:, 128:192], lamt[:, 192:256], ALU.mult, [lamt, ljunk], [ljunk])
        P.op("dve", lambda e: e.tensor_reduce(lsc[:, 1:2], ljunk[:], AX.X, ALU.add), [ljunk], [lsc])
        P.op("act", lambda e: e.activation(lsc[:, 2:4], lsc[:, 0:2], AF.Exp), [lsc], [lsc])
        tt(P, "dve", lsc[:, 4:5], lsc[:, 3:4], lsc[:, 2:3], ALU.subtract, [lsc], [lsc])
        P.op("dve", lambda e: e.tensor_scalar(lsc[:, 5:6], lsc[:, 4:5], -lam_init, None, ALU.add), [lsc], [lsc])
        gsc = load_bcast(P, "gsc", D["da_subln_g"][j], 128, "act")
        P.op("dve", lambda e: e.tensor_scalar(gsc[:], gsc[:], 1.0 - lam_init, None, ALU.mult), [gsc], [gsc])
        D0i = Tl(P, "D0i", [128, S], I32)
        D0f = Tl(P, "D0f", [128, S])
        P.op("pool", lambda e: e.iota(D0i[:], [[-1, S]], base=S - 128, channel_multiplier=1), writes=[D0i])
        copy_op(P, "dve", D0f[:], D0i[:], [D0i], [D0f])
        Bh = Tl(P, "Bh", [128, S])
        kT = [Tl(P, f"kT{i}", [64, 2, S]) for i in range(2)]
        qT = [Tl(P, f"qT{i}", [64, 2, S]) for i in range(2)]
        vv = [Tl(P, f"vv{i}", [128, NQB, 128]) for i in range(2)]
        tmp = [Tl(P, f"tmp{i}", [128, S]) for i in range(2)]
        attnT = Tl(P, "attnT", [128, NQB, 128])
        sc = Tl(P, "sc", [128, 16])
        osb = Tl(P, "osb", [128, 128]); oo = [Tl(P, f"oo{i}", [128, 128]) for i in range(2)]; ojunk = Tl(P, "ojunk", [128, 128])
        n_ = 0
        for b in range(2):
            for h in range(8):
                st_ = n_ % 2
                n_ += 1
                for c in range(2):
                    P.dma("sp", kT[st_][:, c, :], kT_scr[h * 2 + c, :, b * S:(b + 1) * S], writes=[kT[st_]])
                    P.dma("act", qT[st_][:, c, :], qT_scr[h * 2 + c, :, b * S:(b + 1) * S], writes=[qT[st_]])
                P.dma("sp", vv[st_][:], v_scr[b * S:(b + 1) * S, h * 128:(h + 1) * 128].rearrange("(t p) d -> p t d", p=128), writes=[vv[st_]])
                slope = 2.0 ** (-(h + 1))
                P.op("act", lambda e, slope=slope: e.activation(Bh[:], D0f[:], AF.Copy, scale=-slope), [D0f], [Bh])
                P.op("pool", lambda e: e.affine_select(Bh[:], Bh[:], [[-1, S]], ALU.is_ge, NEG, base=S - 128, channel_multiplier=1), [Bh], [Bh])
                for i in range(NQB):
                    nk = (i + 1) * 128
                    for c in range(2):
                        tc_ = tmp[c]
                        for k0 in range(0, nk, 512):
                            kw = min(512, nk - k0)
                            bk = bank(C)
                            P.mm(bk[:, 0:kw], qT[st_][:, c, i * 128:(i + 1) * 128], kT[st_][:, c, k0:k0 + kw], reads=[qT[st_], kT[st_]], writes=[bk])
                            stt(P, "dve", tc_[:, k0:k0 + kw], bk[:, 0:kw], 0.125, Bh[:, S - nk + k0:S - nk + k0 + kw], ALU.mult, ALU.add, [bk, Bh], [tc_])
                        P.op("dve", lambda e, tc_=tc_, nk=nk, c=c: e.tensor_reduce(sc[:, c:c + 1], tc_[:, 0:nk], AX.X, ALU.max), [tc_], [sc])
                        P.op("dve", lambda e, c=c: e.tensor_scalar(sc[:, 2 + c:3 + c], sc[:, c:c + 1], -1.0, None, ALU.mult), [sc], [sc])
                        P.op("act", lambda e, tc_=tc_, nk=nk, c=c: e.activation(tc_[:, 0:nk], tc_[:, 0:nk], AF.Exp, bias=sc[:, 2 + c:3 + c],
                                                                             accum_out=sc[:, 4 + c:5 + c]), [tc_, sc], [tc_, sc])
                    P.op("dve", lambda e: e.reciprocal(sc[:, 6:8], sc[:, 4:6]), [sc], [sc])
                    tt(P, "dve", sc[:, 8:9], sc[:, 7:8], lsc[:, 5:6], ALU.mult, [sc, lsc], [sc])
                    P.op("dve", lambda e, nk=nk: e.tensor_scalar(tmp[1][:, 0:nk], tmp[1][:, 0:nk], sc[:, 8:9], None, ALU.mult), [tmp[1], sc], [tmp[1]])
                    stt(P, "dve", tmp[0][:, 0:nk], tmp[0][:, 0:nk], sc[:, 6:7], tmp[1][:, 0:nk], ALU.mult, ALU.add, [tmp[0], tmp[1], sc], [tmp[0]])
                    for k0 in range(0, i + 1, 4):
                        k1 = min(i + 1, k0 + 4)
                        bk = bank(C)
                        for kt in range(k0, k1):
                            P.tr(bk[:, (kt - k0) * 128:(kt - k0 + 1) * 128], tmp[0][:, kt * 128:(kt + 1) * 128], C.ident[:], [tmp[0], C.ident], [bk])
                        copy_op(P, ev_eng(C), attnT[:, k0:k1, :], bk[:, 0:(k1 - k0) * 128].rearrange("p (k n) -> p k n", n=128), [bk], [attnT])
                    bk = bank(C)
                    for kt in range(i + 1):
                        P.mm(bk[:, 0:128], attnT[:, kt, :], vv[st_][:, kt, :], start=(kt == 0), stop=(kt == i), reads=[attnT, vv[st_]], writes=[bk])
                    copy_op(P, "dve", osb[:], bk[:, 0:128], [bk], [osb])
                    P.op("act", lambda e: e.activation(ojunk[:], osb[:], AF.Square, accum_out=sc[:, 9:10]), [osb], [ojunk, sc])
                    P.op("dve", lambda e: e.tensor_scalar(sc[:, 10:11], sc[:, 9:10], 1.0 / 128, SUBLN_EPS, ALU.mult, ALU.add), [sc], [sc])
                    P.op("act", lambda e: e.activation(sc[:, 10:11], sc[:, 10:11], AF.Sqrt), [sc], [sc])
                    P.op("dve", lambda e: e.reciprocal(sc[:, 10:11], sc[:, 10:11]), [sc], [sc])
                    o = oo[i % 2]
                    stt(P, "dve", o[:], osb[:], sc[:, 10:11], gsc[:], ALU.mult, ALU.mult, [osb, sc, gsc], [o])
                    P.dma(C.dq(), o_scr[b * S + i * 128:b * S + (i + 1) * 128, h * 128:(h + 1) * 128], o[:], reads=[o])


def outproj_ln_stage(P, D, S, o_scr, w_dram, x_in, x_out, lng_ap, lnb_ap):
    T = 2 * S
    xin = x_in.rearrange("b s d -> (b s) d")
    xout = x_out.rearrange("b s d -> (b s) d")
    with P.scope():
        C = setup_common(P)
        dq_factory(C)
        C.wres = P.res("weights")
        C.junk = Tl(P, "junk", [128, 1024])
        wo = Tl(P, "wo", [128, 8, 1024], BF16)
        for k0 in range(0, 8, 2):
            P.dma("pool", wo[:, k0:k0 + 2, :], w_dram[k0 * 128:(k0 + 2) * 128, :].rearrange("(k p) n -> p k n", p=128), writes=[C.wres])
        lng = load_bcast(P, "lng", lng_ap, 1024, "sp")
        lnb = load_bcast(P, "lnb", lnb_ap, 1024, "act")
        ot = Tl(P, "ot", [128, 1024]); oTb = Tl(P, "oTb", [128, 8, 128], BF16)
        xt = [Tl(P, f"xt{i}", [128, 1024]) for i in range(2)]; z = Tl(P, "z", [128, 1024]); st = Tl(P, "st", [128, 8])
        res = [Tl(P, f"res{i}", [128, 1024]) for i in range(2)]
        for t in range(T // 128):
            rows = slice(t * 128, (t + 1) * 128)
            xx = xt[t % 2]
            P.dma("act", xx[:], xin[rows, :], writes=[xx])
            xT_block(C, o_scr, t * 128, 1, ot, oTb)
            for hf in range(2):
                cs = slice(hf * 512, (hf + 1) * 512)
                bk = bank(C)
                for kc in range(8):
                    P.mm(bk[:], oTb[:, kc, :], wo[:, kc, cs], start=(kc == 0), stop=(kc == 7), reads=[oTb, C.wres], writes=[bk])
                copy_op(P, "act", z[:, cs], bk[:], [bk], [z])
            stt(P, "dve", z[:], xx[:], ALPHA, z[:], ALU.mult, ALU.add, [xx, z], [z])
            r = res[t % 2]
            layer_norm_tile(C, z, r, lng, lnb, st)
            P.dma("sp", xout[rows, :], r[:], reads=[r])


INPUT_SHAPES = {
    "ln_g": [4, 2, 1024], "ln_b": [4, 2, 1024], "rwkv_mix": [2, 6, 1024], "rwkv_w_rkv": [2, 3, 1024, 1024],
    "rwkv_w_o": [2, 1024, 1024], "rwkv_w0": [2, 1024], "rwkv_w1": [2, 1024, 64], "rwkv_w2": [2, 64, 1024],
    "rwkv_a0": [2, 1024], "rwkv_a1": [2, 1024, 64], "rwkv_a2": [2, 64, 1024], "rwkv_g1": [2, 1024, 160],
    "rwkv_g2": [2, 160, 1024], "rwkv_k_k": [2, 1024], "rwkv_k_a": [2, 1024], "rwkv_r_k": [2, 16, 64],
    "rwkv_lnx_g": [2, 1024], "rwkv_lnx_b": [2, 1024], "rwkv_v0": [1, 1024], "rwkv_v1": [1, 1024, 32],
    "rwkv_v2": [1, 32, 1024], "kv_w": [1024, 2048], "da_w_q": [2, 1024, 1024], "da_w_o": [2, 1024, 1024],
    "da_lambda": [2, 4, 64], "da_subln_g": [2, 128], "moe_router_w": [4, 1024, 32], "moe_router_b": [4, 32],
    "moe_w_gu": [4, 32, 1024, 2048], "moe_b_gu": [4, 32, 2048], "moe_w_dn": [4, 32, 1024, 1024], "moe_b_dn": [4, 32, 1024],
}
RWKV_KEYS = [k for k in INPUT_SHAPES if k.startswith("rwkv_")] + ["ln_g", "ln_b"]
SCR_NAMES = ("r", "kp", "v", "sg", "kn", "kka", "bonus", "g", "vf")


def build_program(S, plan, keys, shapes=None, ne=32):
    nc = bass.Bass("TRN2", target_bir_lowering=False)
    shapes = shapes or {}
    D = {k: nc.dram_tensor(k, shapes.get(k, INPUT_SHAPES[k]), F32, kind="ExternalInput").ap() for k in keys}
    x = nc.dram_tensor("x", [2, S, 1024], F32, kind="ExternalInput").ap()
    out = nc.dram_tensor("out", [2, S, 1024], F32, kind="ExternalOutput").ap()
    xa = nc.dram_tensor("xa", [2, S, 1024], F32).ap()
    xb = nc.dram_tensor("xb", [2, S, 1024], F32).ap()
    NCH = S // 64
    scr = {n: nc.dram_tensor("scr_" + n, [NCH, 128, 1024], F32).ap() for n in SCR_NAMES}
    ascr = {"kT": nc.dram_tensor("scr_kT", [16, 64, 2 * S], F32).ap(), "qT": nc.dram_tensor("scr_qT", [16, 64, 2 * S], F32).ap(),
            "v": nc.dram_tensor("scr_vsh", [2 * S, 1024], F32).ap(), "o": nc.dram_tensor("scr_o", [2 * S, 1024], F32).ap()}
    P = Prog(nc)
    bufs = {"x": x, "out": out, "xa": xa, "xb": xb}
    for stg in plan:
        kind = stg[0]
        if kind == "rwkv":
            _, l, src, dst = stg
            import os as _os
            if _os.environ.get("ONLY") != "2":
                rwkv_pass1(P, D, l, S, bufs[src], scr)
            if _os.environ.get("ONLY") != "1":
                rwkv_pass2(P, D, l, S, bufs[src], bufs[dst], scr)
        elif kind == "kvproj":
            _, src_ = stg
            proj_stage(P, D, S, bufs[src_], D["kv_w"][:, 0:1024], ascr["kT"], D["kv_w"][:, 1024:2048], ascr["v"])
        elif kind == "attn":
            _, j, src_, dst = stg
            proj_stage(P, D, S, bufs[src_], D["da_w_q"][j], ascr["qT"])
            attn_stage(P, D, j, S, ascr["qT"], ascr["kT"], ascr["v"], ascr["o"])
            outproj_ln_stage(P, D, S, ascr["o"], D["da_w_o"][j], bufs[src_], bufs[dst], D["ln_g"][NA + j, 0], D["ln_b"][NA + j, 0])
        elif kind == "moe":
            _, l, src_, dst = stg
            moe_stage(P, D, l, S, bufs[src_], bufs[dst], NE=ne)
        else:
            raise ValueError(kind)
    P.finish()
    return nc, P


FULL_PLAN = [("rwkv", 0, "x", "xa"), ("moe", 0, "xa", "xb"), ("rwkv", 1, "xb", "xa"), ("moe", 1, "xa", "xb"),
             ("kvproj", "xb"), ("attn", 0, "xb", "xa"), ("moe", 2, "xa", "xb"), ("attn", 1, "xb", "xa"), ("moe", 3, "xa", "out")]


def kernel(**inputs):
    from concourse.bass_utils import run_bass_kernel_spmd
    n = 8
    S = 2048
    x = np.ascontiguousarray(np.asarray(inputs["x"], dtype=np.float32))
    keys = list(INPUT_SHAPES.keys())
    nc, P = build_program(S, FULL_PLAN, keys)
    shared = {k: np.ascontiguousarray(np.asarray(inputs[k], dtype=np.float32)) for k in keys}
    in_maps = []
    for c in range(n):
        m = dict(shared)
        m["x"] = x[2 * c:2 * c + 2]
        in_maps.append(m)
    res = run_bass_kernel_spmd(nc, in_maps, core_ids=list(range(n)))
    return np.concatenate([r["out"] for r in res.results], axis=0).astype(np.float32)
```

```python
import contextlib
import numpy as np
import concourse.bass as bass
import concourse.mybir as mybir

F32 = mybir.dt.float32
BF16 = mybir.dt.bfloat16
I32 = mybir.dt.int32
ALU = mybir.AluOpType
AF = mybir.ActivationFunctionType
AX = mybir.AxisListType

N_DMA_SEMS = 40


class Res:
    __slots__ = ("name", "last_w", "readers")

    def __init__(self, name=""):
        self.name = name
        self.last_w = None
        self.readers = []


class Op:
    __slots__ = ("eng", "fn", "deps", "marked", "is_dma", "sem", "val", "idx")

    def __init__(self, eng, fn, is_dma):
        self.eng = eng
        self.fn = fn
        self.deps = []
        self.marked = False
        self.is_dma = is_dma
        self.sem = None
        self.val = 0


ENGS = ("pe", "dve", "act", "pool", "sp")


class Prog:
    def __init__(self, nc):
        self.nc = nc
        self.base = contextlib.ExitStack()
        self.csem = {e: self.base.enter_context(nc.semaphore(f"s_{e}")) for e in ENGS}
        self.dsem = [self.base.enter_context(nc.semaphore(f"d_{i}")) for i in range(N_DMA_SEMS)]
        self.ccount = {e: 0 for e in ENGS}
        self.dma_rr = 0
        self.dma_last = [None] * N_DMA_SEMS
        self.dma_cnt = [0] * N_DMA_SEMS
        self.nres = 0
        self.stack = None
        self.total = {e: 0 for e in ENGS}
        self._reset()

    def _reset(self):
        self.ops = {e: [] for e in ENGS}

    @contextlib.contextmanager
    def scope(self, final=False):
        self.stack = contextlib.ExitStack()
        try:
            yield self
            self.flush(final)
        finally:
            self.stack.close()
            self.stack = None

    def sb(self, name, shape, dtype=F32):
        self.nres += 1
        return self.stack.enter_context(self.nc.sbuf_tensor(f"{name}_{self.nres}", list(shape), dtype))

    def ps(self, name, shape, dtype=F32):
        self.nres += 1
        return self.stack.enter_context(self.nc.psum_tensor(f"{name}_{self.nres}", list(shape), dtype))

    def res(self, name=""):
        self.nres += 1
        return Res(name or f"r{self.nres}")

    def _add(self, eng, fn, reads, writes, is_dma):
        op = Op(eng, fn, is_dma)
        deps = []
        for r in reads:
            if r.last_w is not None:
                deps.append(r.last_w)
        for w in writes:
            if w.last_w is not None:
                deps.append(w.last_w)
            deps.extend(w.readers)
        seen = set()
        for d in deps:
            if id(d) in seen or d is op:
                continue
            seen.add(id(d))
            if (not d.is_dma) and (not is_dma) and d.eng == "pe" and eng == "pe":
                continue
            op.deps.append(d)
            d.marked = True
        if is_dma:
            k = self.dma_rr
            self.dma_rr = (self.dma_rr + 1) % N_DMA_SEMS
            prev = self.dma_last[k]
            if prev is not None and all(prev is not x for x in op.deps):
                op.deps.append(prev)
            self.dma_last[k] = op
            self.dma_cnt[k] += 16
            op.sem = k
            op.val = self.dma_cnt[k]
            op.marked = True
        for r in reads:
            if not is_dma:
                r.readers = [o for o in r.readers if o.is_dma or o.eng != eng]
            r.readers.append(op)
        for w in writes:
            w.last_w = op
            w.readers = []
        self.ops[eng].append(op)
        return op

    def op(self, eng, fn, reads=(), writes=()):
        return self._add(eng, fn, _rs(reads), _rs(writes), False)

    def dma(self, eng, out, in_, reads=(), writes=(), **kw):
        return self._add(eng, lambda e: e.dma_start(out=out, in_=in_, **kw),
                         _rs(reads), _rs(writes), True)

    def mm(self, out, lhsT, rhs, start=True, stop=True, reads=(), writes=()):
        return self.op("pe", lambda e: e.matmul(out, lhsT, rhs, start=start, stop=stop), reads, writes)

    def tr(self, out, in_, ident, reads=(), writes=()):
        return self.op("pe", lambda e: e.transpose(out, in_, ident), reads, writes)

    def flush(self, final=False):
        nc = self.nc
        csem, dsem = self.csem, self.dsem
        lastc = []
        for e in ENGS:
            comp = [o for o in self.ops[e] if not o.is_dma and o.fn is not None]
            if comp:
                comp[-1].marked = True
                lastc.append(comp[-1])
        lastd = [o for o in self.dma_last if o is not None]
        for e in ENGS:
            b = Op(e, None, False)
            b.deps = list(lastc) + list(lastd)
            self.ops[e].append(b)
        for e in ENGS:
            c = self.ccount[e]
            for op in self.ops[e]:
                if op.is_dma or op.fn is None:
                    continue
                if op.marked:
                    c += 1
                    op.val = c
            self.ccount[e] = c

        def semof(d):
            return (("d", d.sem), dsem[d.sem]) if d.is_dma else (("c", d.eng), csem[d.eng])

        def replay(ename):
            def body(e):
                known = {}
                for op in self.ops[ename]:
                    for d in op.deps:
                        key, sem = semof(d)
                        if known.get(key, 0) >= d.val:
                            continue
                        e.wait_ge(sem, d.val)
                        known[key] = d.val
                    if op.fn is None:
                        continue
                    ins = op.fn(e)
                    if op.is_dma:
                        ins.then_inc(dsem[op.sem], 16)
                    elif op.marked:
                        ins.then_inc(csem[ename], 1)
            return body

        with nc.Block() as block:
            block.sync(replay("sp"))
            block.scalar(replay("act"))
            block.vector(replay("dve"))
            block.gpsimd(replay("pool"))
            block.tensor(replay("pe"))
        for e in ENGS:
            self.total[e] += len(self.ops[e])
        self._reset()

    def finish(self):
        self.base.close()


class Tl:
    def __init__(self, P, name, shape, dtype=F32, psum=False):
        self.t = (P.ps if psum else P.sb)(name, shape, dtype)
        self.r = P.res(name)
        self.shape = shape

    def __getitem__(self, k):
        return self.t[k]


def _rs(xs):
    return [x.r if isinstance(x, Tl) else x for x in xs]


D_MODEL = 1024
DEPTH = 4
NA = 2
H_R = 16
EXPM05 = float(np.exp(-0.5))
ALPHA = (2 * DEPTH) ** 0.25
LN_EPS = 1e-5
GN_EPS = 64e-5


class Ctx:
    pass


def setup_common(P):
    C = Ctx()
    C.P = P
    C.ident = Tl(P, "ident", [128, 128])
    P.op("pool", lambda e: e.memset(C.ident[:], 0.0), writes=[C.ident])
    P.op("pool", lambda e: e.affine_select(C.ident[:], C.ident[:], [[-1, 128]], ALU.not_equal, 1.0,
                                           base=0, channel_multiplier=1), reads=[C.ident], writes=[C.ident])
    C.banks = [Tl(P, f"bank{i}", [128, 512], F32, psum=True) for i in range(8)]
    C.bi = 0
    C.rr = 0
    return C


def bank(C):
    b = C.banks[C.bi]
    C.bi = (C.bi + 1) % 8
    return b


def ev_eng(C):
    C.rr ^= 1
    return "act" if C.rr else "dve"


def copy_op(P, eng, out, in_, reads, writes):
    if eng == "act":
        P.op("act", lambda e: e.activation(out, in_, AF.Copy), reads, writes)
    else:
        P.op(eng, lambda e: e.tensor_copy(out, in_), reads, writes)


def tt(P, eng, out, in0, in1, op, reads, writes):
    P.op(eng, lambda e: e.tensor_tensor(out, in0, in1, op), reads, writes)


def stt(P, eng, out, in0, scalar, in1, op0, op1, reads, writes):
    P.op(eng, lambda e: e.scalar_tensor_tensor(out, in0, scalar, in1, op0, op1), reads, writes)


def rsqrt_op(P, t, lo, hi, bias, use_max):
    sl = t[:, lo:hi]
    op0 = ALU.max if use_max else ALU.add
    P.op("dve", lambda e: e.tensor_scalar(sl, sl, bias, None, op0), [t], [t])
    P.op("act", lambda e: e.activation(sl, sl, AF.Sqrt), [t], [t])
    P.op("dve", lambda e: e.reciprocal(sl, sl), [t], [t])


def load_bcast(P, name, src_1d, n, eng="sp"):
    t = Tl(P, name, [128, n])
    P.dma(eng, t[:], src_1d.partition_broadcast(128), writes=[t])
    return t


def layer_norm_tile(C, z, out, g_t, b_t, st):
    P = C.P
    junk = C.junk
    P.op("act", lambda e: e.activation(junk[:], z[:], AF.Copy, accum_out=st[:, 0:1]), [z], [junk, st])
    P.op("act", lambda e: e.activation(junk[:], z[:], AF.Square, accum_out=st[:, 1:2]), [z], [junk, st])
    P.op("dve", lambda e: e.tensor_scalar(st[:, 2:3], st[:, 0:1], 1.0 / 1024, None, ALU.mult), [st], [st])
    P.op("dve", lambda e: e.tensor_tensor(st[:, 3:4], st[:, 2:3], st[:, 2:3], ALU.mult), [st], [st])
    P.op("dve", lambda e: e.scalar_tensor_tensor(st[:, 4:5], st[:, 1:2], 1.0 / 1024, st[:, 3:4], ALU.mult, ALU.subtract), [st], [st])
    copy_op(P, "dve", st[:, 5:6], st[:, 4:5], [st], [st])
    rsqrt_op(P, st, 5, 6, LN_EPS, False)
    P.op("dve", lambda e: e.scalar_tensor_tensor(st[:, 6:7], st[:, 2:3], -1.0, st[:, 5:6], ALU.mult, ALU.mult), [st], [st])
    P.op("act", lambda e: e.activation(out[:], z[:], AF.Identity, bias=st[:, 6:7], scale=st[:, 5:6]), [z, st], [out])
    tt(P, "dve", out[:], out[:], g_t[:], ALU.mult, [out, g_t], [out])
    tt(P, "pool", out[:], out[:], b_t[:], ALU.add, [out, b_t], [out])


def load_w_bf16(C, dst, src2d, K, N, stage):
    P = C.P
    kcs = K // 128
    per = max(1, stage.shape[1] // N)
    for k0 in range(0, kcs, per):
        k1 = min(kcs, k0 + per)
        sv = stage[:, 0:(k1 - k0) * N].rearrange("p (k n) -> p k n", n=N)
        P.dma(C.dq(), sv, src2d[k0 * 128:k1 * 128, :].rearrange("(k p) n -> p k n", p=128), writes=[stage])
        copy_op(P, ev_eng(C), dst[:, k0:k1, :], sv, [stage], [C.wres])


def dq_factory(C):
    qs = ["sp", "act", "pool"]
    C.dqi = 0

    def dq():
        C.dqi = (C.dqi + 1) % 2
        return qs[C.dqi]
    C.dq = dq


def rwkv_pass1(P, D, l, S, x_in, scr):
    NCH = S // 64
    with P.scope():
        C = setup_common(P)
        dq_factory(C)
        C.wres = P.res("weights")
        C.junk = Tl(P, "junk", [128, 1024])
        stage = Tl(P, "wstage", [128, 2048])
        wr = Tl(P, "wrkv", [128, 3 * 8, 1024], BF16)
        for j in range(3):
            load_w_bf16(C, wr.t[:, j * 8:(j + 1) * 8, :], D["rwkv_w_rkv"][l, j], 1024, 1024, stage)
        w1 = Tl(P, "w1", [128, 8, 64], BF16)
        load_w_bf16(C, w1.t, D["rwkv_w1"][l], 1024, 64, stage)
        a1 = Tl(P, "a1", [128, 8, 64], BF16)
        load_w_bf16(C, a1.t, D["rwkv_a1"][l], 1024, 64, stage)
        g1 = Tl(P, "g1", [128, 8, 160], BF16)
        load_w_bf16(C, g1.t, D["rwkv_g1"][l], 1024, 160, stage)
        if l > 0:
            v1 = Tl(P, "v1", [128, 8, 32], BF16)
            load_w_bf16(C, v1.t, D["rwkv_v1"][l - 1], 1024, 32, stage)
            v2 = Tl(P, "v2", [65, 1024])
            P.op("dve", lambda e: e.memset(v2[:], 0.0), writes=[C.wres])
            P.dma("sp", v2[0:32, :], D["rwkv_v2"][l - 1], writes=[C.wres])
            P.dma("sp", v2[64:65, :], D["rwkv_v0"][l - 1:l, :], writes=[C.wres])
        w2 = Tl(P, "w2", [65, 1024])
        P.dma("sp", w2[0:64, :], D["rwkv_w2"][l], writes=[C.wres])
        P.dma("sp", w2[64:65, :], D["rwkv_w0"][l:l + 1, :], writes=[C.wres])
        a2 = Tl(P, "a2", [65, 1024])
        P.dma("act", a2[0:64, :], D["rwkv_a2"][l], writes=[C.wres])
        P.dma("act", a2[64:65, :], D["rwkv_a0"][l:l + 1, :], writes=[C.wres])
        g2a = Tl(P, "g2a", [128, 1024])
        g2b = Tl(P, "g2b", [32, 1024])
        P.dma("sp", g2a[:], D["rwkv_g2"][l, 0:128, :], writes=[C.wres])
        P.dma("sp", g2b[:], D["rwkv_g2"][l, 128:160, :], writes=[C.wres])
        mixT = Tl(P, "mixT", [128, 6, 8])
        P.dma("sp", mixT[:], D["rwkv_mix"][l].rearrange("j (k p) -> p j k", p=128), writes=[C.wres],
              allow_slow_non_contiguous=True)
        kk_t = load_bcast(P, "k_k", D["rwkv_k_k"][l], 1024, "act")
        ka_t = load_bcast(P, "k_a", D["rwkv_k_a"][l], 1024, "sp")
        rk_t = load_bcast(P, "r_k", D["rwkv_r_k"][l].rearrange("h n -> (h n)"), 1024, "sp")
        WR = [C.wres, kk_t, ka_t, rk_t]

        xt = Tl(P, "xt", [128, 1024]); xp = Tl(P, "xp", [128, 1024])
        xT = Tl(P, "xT", [128, 8, 128]); xxT = Tl(P, "xxT", [128, 8, 128])
        tmpA = Tl(P, "tmpA", [128, 8, 128]); tmpB = Tl(P, "tmpB", [128, 8, 128])
        xm = [Tl(P, f"xm{j}", [128, 8, 128], BF16) for j in range(6)]
        r_t = Tl(P, "r", [128, 1024]); k_t = Tl(P, "k", [128, 1024]); v_t = Tl(P, "v", [128, 1024])
        sg_t = Tl(P, "sg", [128, 1024]); a_t = Tl(P, "a", [128, 1024]); g_t = Tl(P, "g", [128, 1024])
        kn_t = Tl(P, "kn", [128, 1024]); kp_t = Tl(P, "kp", [128, 1024]); t1 = Tl(P, "t1", [128, 1024])
        t2 = Tl(P, "t2", [128, 1024]); vf_t = Tl(P, "vf", [128, 1024])
        l1w = Tl(P, "l1w", [65, 128]); l1a = Tl(P, "l1a", [65, 128]); l1v = Tl(P, "l1v", [65, 128])
        l1ga = Tl(P, "l1ga", [128, 128]); l1gb = Tl(P, "l1gb", [32, 128])
        s16 = Tl(P, "s16", [128, 16]); s16b = Tl(P, "s16b", [128, 16])
        P.op("dve", lambda e: e.memset(l1w[:], 1.0), writes=[l1w])
        P.op("dve", lambda e: e.memset(l1a[:], 1.0), writes=[l1a])
        P.op("dve", lambda e: e.memset(l1v[:], 0.0), writes=[l1v])
        P.op("dve", lambda e: e.memset(l1v[64:65, :], 1.0), [l1v], [l1v])

        def v3(t):
            return t[:].rearrange("p (h k) -> p h k", k=64)

        for c in range(NCH):
            for b in range(2):
                P.dma("sp", xt[b * 64:(b + 1) * 64, :], x_in[b, c * 64:(c + 1) * 64, :], writes=[xt])
            if c == 0:
                P.op("pool", lambda e: e.memset(xp[:], 0.0), writes=[xp])
                for b in range(2):
                    P.dma("act", xp[b * 64 + 1:(b + 1) * 64, :], x_in[b, 0:63, :], writes=[xp])
            else:
                for b in range(2):
                    P.dma("act", xp[b * 64:(b + 1) * 64, :], x_in[b, c * 64 - 1:c * 64 + 63, :], writes=[xp])
            if l > 0:
                P.dma("sp", vf_t[:], scr["vf"][c], writes=[vf_t])
            bA, bB = bank(C), bank(C)
            for kc in range(8):
                bk = bA if kc < 4 else bB
                P.tr(bk[:, (kc % 4) * 128:(kc % 4 + 1) * 128], xt[:, kc * 128:(kc + 1) * 128], C.ident[:], [xt, C.ident], [bk])
            copy_op(P, "act", xT[:, 0:4, :], bA[:].rearrange("p (k n) -> p k n", n=128), [bA], [xT])
            copy_op(P, "dve", xT[:, 4:8, :], bB[:].rearrange("p (k n) -> p k n", n=128), [bB], [xT])
            bA, bB = bank(C), bank(C)
            for kc in range(8):
                bk = bA if kc < 4 else bB
                P.tr(bk[:, (kc % 4) * 128:(kc % 4 + 1) * 128], xp[:, kc * 128:(kc + 1) * 128], C.ident[:], [xp, C.ident], [bk])
            tt(P, "dve", xxT[:, 0:4, :], bA[:].rearrange("p (k n) -> p k n", n=128), xT[:, 0:4, :], ALU.subtract, [bA, xT], [xxT])
            tt(P, "dve", xxT[:, 4:8, :], bB[:].rearrange("p (k n) -> p k n", n=128), xT[:, 4:8, :], ALU.subtract, [bB, xT], [xxT])
            for j in range(6):
                eng = "dve" if j % 2 == 0 else "pool"
                tmp = tmpA if j % 2 == 0 else tmpB
                mb = mixT[:, j, :].unsqueeze(2).broadcast_to([128, 8, 128])
                tt(P, eng, tmp[:], xxT[:], mb, ALU.mult, [xxT, C.wres], [tmp])
                tt(P, eng, xm[j][:], tmp[:], xT[:], ALU.add, [tmp, xT], [xm[j]])
            for j, dst in enumerate((r_t, k_t, v_t)):
                for hf in range(2):
                    bk = bank(C)
                    for kc in range(8):
                        P.mm(bk[:], xm[j][:, kc, :], wr[:, j * 8 + kc, hf * 512:(hf + 1) * 512], start=(kc == 0), stop=(kc == 7),
                             reads=[xm[j], C.wres], writes=[bk])
                    copy_op(P, ev_eng(C), dst[:, hf * 512:(hf + 1) * 512], bk[:], [bk], [dst])
            bk = bank(C)
            for kc in range(8):
                P.mm(bk[0:64, 0:128], w1[:, kc, :], xm[3][:, kc, :], start=(kc == 0), stop=(kc == 7), reads=[xm[3], C.wres], writes=[bk])
            for kc in range(8):
                P.mm(bk[0:64, 128:256], a1[:, kc, :], xm[4][:, kc, :], start=(kc == 0), stop=(kc == 7), reads=[xm[4], C.wres], writes=[bk])
            if l > 0:
                for kc in range(8):
                    P.mm(bk[0:32, 256:384], v1[:, kc, :], xm[2][:, kc, :], start=(kc == 0), stop=(kc == 7), reads=[xm[2], C.wres], writes=[bk])
            P.op("act", lambda e, bk=bk: e.activation(l1w[0:64, :], bk[0:64, 0:128], AF.Tanh), [bk], [l1w])
            copy_op(P, "act", l1a[0:64, :], bk[0:64, 128:256], [bk], [l1a])
            if l > 0:
                copy_op(P, "act", l1v[0:32, :], bk[0:32, 256:384], [bk], [l1v])
            bk2 = bank(C)
            for kc in range(8):
                P.mm(bk2[:, 0:128], g1[:, kc, 0:128], xm[5][:, kc, :], start=(kc == 0), stop=(kc == 7), reads=[xm[5], C.wres], writes=[bk2])
            for kc in range(8):
                P.mm(bk2[0:32, 128:256], g1[:, kc, 128:160], xm[5][:, kc, :], start=(kc == 0), stop=(kc == 7), reads=[xm[5], C.wres], writes=[bk2])
            P.op("act", lambda e, bk2=bk2: e.activation(l1ga[:], bk2[:, 0:128], AF.Sigmoid), [bk2], [l1ga])
            P.op("act", lambda e, bk2=bk2: e.activation(l1gb[:], bk2[0:32, 128:256], AF.Sigmoid), [bk2], [l1gb])
            for hf in range(2):
                cs = slice(hf * 512, (hf + 1) * 512)
                bk = bank(C)
                P.mm(bk[:], l1w[:], w2[:, cs], reads=[l1w, C.wres], writes=[bk])
                P.op("act", lambda e, bk=bk, cs=cs: e.activation(sg_t[:, cs], bk[:], AF.Sigmoid), [bk], [sg_t])
                bk = bank(C)
                P.mm(bk[:], l1a[:], a2[:, cs], reads=[l1a, C.wres], writes=[bk])
                P.op("act", lambda e, bk=bk, cs=cs: e.activation(a_t[:, cs], bk[:], AF.Sigmoid), [bk], [a_t])
                bk = bank(C)
                P.mm(bk[:], l1ga[:], g2a[:, cs], start=True, stop=False, reads=[l1ga, C.wres], writes=[bk])
                P.mm(bk[:], l1gb[:], g2b[:, cs], start=False, stop=True, reads=[l1gb, C.wres], writes=[bk])
                copy_op(P, "dve", g_t[:, cs], bk[:], [bk], [g_t])
                if l > 0:
                    bk = bank(C)
                    P.mm(bk[:], l1v[:], v2[:, cs], reads=[l1v, C.wres], writes=[bk])
                    P.op("act", lambda e, bk=bk, cs=cs: e.activation(t1[:, cs], bk[:], AF.Sigmoid), [bk], [t1])
            if l > 0:
                tt(P, "dve", t2[:], vf_t[:], v_t[:], ALU.subtract, [vf_t, v_t], [t2])
                tt(P, "dve", t2[:], t2[:], t1[:], ALU.mult, [t2, t1], [t2])
                tt(P, "dve", v_t[:], v_t[:], t2[:], ALU.add, [v_t, t2], [v_t])
            else:
                P.dma("sp", scr["vf"][c], v_t[:], reads=[v_t])
            tt(P, "pool", kn_t[:], k_t[:], kk_t[:], ALU.mult, [k_t, kk_t], [kn_t])
            tt(P, "pool", t2[:], kn_t[:], kn_t[:], ALU.mult, [kn_t], [t2])
            P.op("dve", lambda e: e.tensor_reduce(s16[:], v3(t2), AX.X, ALU.add), [t2], [s16])
            rsqrt_op(P, s16, 0, 16, 1e-24, True)
            tt(P, "dve", v3(kn_t), v3(kn_t), s16[:].unsqueeze(2).broadcast_to([128, 16, 64]), ALU.mult, [kn_t, s16], [kn_t])
            stt(P, "dve", t2[:], a_t[:], -1.0, ka_t[:], ALU.add, ALU.mult, [a_t, ka_t], [t2])
            stt(P, "dve", kp_t[:], t2[:], 1.0, k_t[:], ALU.add, ALU.mult, [t2, k_t], [kp_t])
            tt(P, "pool", a_t[:], kn_t[:], a_t[:], ALU.mult, [kn_t, a_t], [a_t])
            tt(P, "dve", t2[:], r_t[:], kp_t[:], ALU.mult, [r_t, kp_t], [t2])
            tt(P, "pool", t2[:], t2[:], rk_t[:], ALU.mult, [t2, rk_t], [t2])
            P.op("dve", lambda e: e.tensor_reduce(s16b[:], v3(t2), AX.X, ALU.add), [t2], [s16b])
            tt(P, "dve", v3(t2), v3(v_t), s16b[:].unsqueeze(2).broadcast_to([128, 16, 64]), ALU.mult, [v_t, s16b], [t2])
            for nm, tl in (("r", r_t), ("kp", kp_t), ("v", v_t), ("sg", sg_t), ("kn", kn_t), ("kka", a_t), ("bonus", t2), ("g", g_t)):
                P.dma(C.dq(), scr[nm][c], tl[:], reads=[tl])


def rwkv_pass2(P, D, l, S, x_in, x_out, scr):
    NCH = S // 64
    import os as _os
    CUT = float(_os.environ.get('CUT', '99'))
    with P.scope():
        C = setup_common(P)
        dq_factory(C)
        C.wres = P.res("weights")
        stage = Tl(P, "wstage", [128, 1024])
        wo = Tl(P, "wo", [128, 8, 1024], BF16)
        load_w_bf16(C, wo.t, D["rwkv_w_o"][l], 1024, 1024, stage)
        lnxg = load_bcast(P, "lnxg", D["rwkv_lnx_g"][l], 1024, "sp")
        lnxb = load_bcast(P, "lnxb", D["rwkv_lnx_b"][l], 1024, "act")
        lng = load_bcast(P, "lng", D["ln_g"][l, 0], 1024, "sp")
        lnb = load_bcast(P, "lnb", D["ln_b"][l, 0], 1024, "sp")
        iu = Tl(P, "iu", [128, 128]); su = Tl(P, "su", [128, 128]); sl = Tl(P, "sl", [128, 128])
        triI = Tl(P, "triI", [128, 128]); triS = Tl(P, "triS", [128, 128]); bsel = Tl(P, "bsel", [128, 32])
        for m, cmpop, pat, cm in ((iu, ALU.is_ge, 1, -1), (su, ALU.is_gt, 1, -1), (sl, ALU.is_gt, -1, 1)):
            P.op("pool", lambda e, m=m: e.memset(m[:], 1.0), writes=[m])
            P.op("pool", lambda e, m=m, cmpop=cmpop, pat=pat, cm=cm: e.affine_select(
                m[:], m[:], [[pat, 128]], cmpop, 0.0, base=0, channel_multiplier=cm), [m], [m])
        P.op("pool", lambda e: e.memset(iu[0:64, 64:128], 0.0), [iu], [iu])
        P.op("pool", lambda e: e.memset(su[0:64, 64:128], 0.0), [su], [su])
        P.op("pool", lambda e: e.memset(sl[64:128, 0:64], 0.0), [sl], [sl])
        P.op("dve", lambda e: e.tensor_scalar(triI[:], iu[:], -EXPM05, None, ALU.mult), [iu], [triI])
        P.op("dve", lambda e: e.tensor_scalar(triS[:], su[:], -EXPM05, None, ALU.mult), [su], [triS])
        P.op("pool", lambda e: e.memset(bsel[:], 0.0), writes=[bsel])
        P.op("pool", lambda e: e.memset(bsel[0:64, 0:16], -EXPM05), [bsel], [bsel])
        P.op("pool", lambda e: e.memset(bsel[64:128, 16:32], -EXPM05), [bsel], [bsel])
        ST = Tl(P, "ST", [64, 16, 128])
        P.op("dve", lambda e: e.memset(ST[:], 0.0), writes=[ST])
        Vblk = Tl(P, "Vblk", [128, 16, 128]); Ublk = Tl(P, "Ublk", [128, 16, 128])
        P.op("pool", lambda e: e.memset(Vblk[:], 0.0), writes=[Vblk])
        P.op("pool", lambda e: e.memset(Ublk[:], 0.0), writes=[Ublk])
        r_t = Tl(P, "r", [128, 1024]); kp_t = Tl(P, "kp", [128, 1024]); v_t = Tl(P, "v", [128, 1024])
        sg_t = Tl(P, "sg", [128, 1024]); kn_t = Tl(P, "kn", [128, 1024]); kka_t = Tl(P, "kka", [128, 1024])
        Ep = Tl(P, "Ep", [128, 1024]); Em = Tl(P, "Em", [128, 1024]); Epp = Tl(P, "Epp", [128, 1024])
        bon_t = Ep; g_t = Epp; xt = sg_t
        C.junk = Em
        Y = Tl(P, "Y", [128, 1024]); Rh = Tl(P, "Rh", [128, 1024]); t2 = Tl(P, "t2", [128, 1024])
        oT = Tl(P, "oT", [128, 8, 128], BF16)
        gamT = Tl(P, "gamT", [64, 16, 2])
        st = Tl(P, "st", [128, 8]); s16 = Tl(P, "s16", [128, 16]); s16b = Tl(P, "s16b", [128, 16])
        G = []
        for gi in range(4):
            g = Ctx()
            g.qT = Tl(P, f"qT{gi}", [64, 16, 128])
            g.Q = Tl(P, f"Q{gi}", [128, 4, 128]); g.QT = Tl(P, f"QT{gi}", [128, 4, 128])
            g.Aak = Tl(P, f"Aak{gi}", [128, 4, 128]); g.Arb = Tl(P, f"Arb{gi}", [128, 4, 128]); g.Ark = Tl(P, f"Ark{gi}", [128, 4, 128])
            g.X = Tl(P, f"X{gi}", [128, 4, 128])
            g.WT = g.qT
            G.append(g)

        def v3(t):
            return t[:].rearrange("p (h k) -> p h k", k=64)

        def b4(bk):
            return bk[:].rearrange("p (h n) -> p h n", n=128)

        for c in range(NCH):
            for nm, tl in (("r", r_t), ("kp", kp_t), ("v", v_t), ("sg", sg_t), ("kn", kn_t), ("kka", kka_t)):
                P.dma(C.dq(), tl[:], scr[nm][c], writes=[tl])
            for hf in range(2):
                cs = slice(hf * 512, (hf + 1) * 512)
                bk = bank(C)
                P.mm(bk[:], triI[:], sg_t[:, cs], reads=[triI, sg_t], writes=[bk])
                P.op("act", lambda e, bk=bk, cs=cs: e.activation(Ep[:, cs], bk[:], AF.Exp), [bk], [Ep])
                P.op("act", lambda e, bk=bk, cs=cs: e.activation(Em[:, cs], bk[:], AF.Exp, scale=-1.0), [bk], [Em])
                bk = bank(C)
                P.mm(bk[:], triS[:], sg_t[:, cs], reads=[triS, sg_t], writes=[bk])
                P.op("act", lambda e, bk=bk, cs=cs: e.activation(Epp[:, cs], bk[:], AF.Exp), [bk], [Epp])
            bk = bank(C)
            for h in range(16):
                P.mm(bk[0:64, h * 32:(h + 1) * 32], sg_t[:, h * 64:(h + 1) * 64], bsel[:], reads=[sg_t, bsel], writes=[bk])
            P.op("act", lambda e, bk=bk: e.activation(gamT[:], bk[0:64, :].rearrange("p (h b r) -> p h b r", b=2, r=16)[:, :, :, 0], AF.Exp), [bk], [gamT])
            stt(P, "dve", kn_t[:], kn_t[:], -1.0, Epp[:], ALU.mult, ALU.mult, [kn_t, Epp], [kn_t])
            tt(P, "pool", kka_t[:], kka_t[:], Em[:], ALU.mult, [kka_t, Em], [kka_t])
            tt(P, "dve", kp_t[:], kp_t[:], Em[:], ALU.mult, [kp_t, Em], [kp_t])
            tt(P, "pool", r_t[:], r_t[:], Ep[:], ALU.mult, [r_t, Ep], [r_t])
            copy_op(P, "pool", Vblk[0:64, :, 0:64], v3(v_t)[0:64], [v_t], [Vblk])
            copy_op(P, "pool", Vblk[64:128, :, 64:128], v3(v_t)[64:128], [v_t], [Vblk])
            quant = (kn_t, kka_t, kp_t, r_t)
            P.dma("sp", bon_t[:], scr["bonus"][c], writes=[bon_t])
            P.dma("act", g_t[:], scr["g"][c], writes=[g_t])
            for b in range(2):
                P.dma("sp", xt[b * 64:(b + 1) * 64, :], x_in[b, c * 64:(c + 1) * 64, :], writes=[xt])
            if CUT <= 1:
                continue
            for gi, g in enumerate(G):
                for q in range(4):
                    bk = bank(C)
                    for h4 in range(4):
                        h = gi * 4 + h4
                        P.tr(bk[0:64, h4 * 128:(h4 + 1) * 128], quant[q][:, h * 64:(h + 1) * 64], C.ident[:], [quant[q], C.ident], [bk])
                    copy_op(P, ev_eng(C), g.qT[:, q * 4:(q + 1) * 4, :], b4(bk)[0:64], [bk], [g.qT])
            if CUT <= 2:
                continue
            for gi, g in enumerate(G):
                for (dst, lq, rq, msk) in ((g.QT, 1, 0, su), (g.Q, 0, 1, sl), (g.Aak, 2, 0, su), (g.Arb, 1, 3, iu), (g.Ark, 2, 3, iu)):
                    bk = bank(C)
                    for h4 in range(4):
                        P.mm(bk[:, h4 * 128:(h4 + 1) * 128], g.qT[:, lq * 4 + h4, :], g.qT[:, rq * 4 + h4, :], reads=[g.qT], writes=[bk])
                    tt(P, "dve", dst[:], b4(bk), msk[:].unsqueeze(1).broadcast_to([128, 4, 128]), ALU.mult, [bk, msk], [dst])
            if CUT <= 3:
                continue
            for gi, g in enumerate(G):
                bk = bank(C)
                for h4 in range(4):
                    h = gi * 4 + h4
                    P.mm(bk[:, h4 * 128 + 64:(h4 + 1) * 128], g.Aak[:, h4, :], v_t[:, h * 64:(h + 1) * 64], reads=[g.Aak, v_t], writes=[bk])
                copy_op(P, ev_eng(C), g.X[:, :, 64:128], b4(bk)[:, :, 64:128], [bk], [g.X])
                copy_op(P, "pool", g.X[:, :, 0:64], v3(kn_t)[:, gi * 4:(gi + 1) * 4, :], [kn_t], [g.X])
            if CUT <= 4:
                continue
            for j in range(6):
                for gi, g in enumerate(G):
                    bk = bank(C)
                    for h4 in range(4):
                        P.mm(bk[:, h4 * 128:(h4 + 1) * 128], g.QT[:, h4, :], g.X[:, h4, :], reads=[g.QT, g.X], writes=[bk])
                    if j < 5:
                        bq = bank(C); bqt = bank(C)
                        for h4 in range(4):
                            P.mm(bq[:, h4 * 128:(h4 + 1) * 128], g.QT[:, h4, :], g.Q[:, h4, :], reads=[g.QT, g.Q], writes=[bq])
                        for h4 in range(4):
                            P.mm(bqt[:, h4 * 128:(h4 + 1) * 128], g.Q[:, h4, :], g.QT[:, h4, :], reads=[g.QT, g.Q], writes=[bqt])
                    tt(P, "dve", g.X[:], b4(bk), g.X[:], ALU.add, [bk, g.X], [g.X])
                    if j < 5:
                        copy_op(P, "act", g.Q[:], b4(bq), [bq], [g.Q])
                        copy_op(P, ev_eng(C), g.QT[:], b4(bqt), [bqt], [g.QT])
            if CUT <= 5:
                continue
            for gi, g in enumerate(G):
                bk = bank(C)
                for h4 in range(4):
                    h = gi * 4 + h4
                    EV = _os.environ.get("EV", "0")
                    if EV in ("0", "1"):
                        P.mm(bk[:, h4 * 128:h4 * 128 + 64], g.Arb[:, h4, :], g.X[:, h4, 0:64], reads=[g.Arb, g.X], writes=[bk])
                    if EV in ("0", "2"):
                        P.mm(bk[:, h4 * 128 + 64:(h4 + 1) * 128], g.Arb[:, h4, :], g.X[:, h4, 64:128], start=True, stop=False, reads=[g.Arb, g.X], writes=[bk])
                        P.mm(bk[:, h4 * 128 + 64:(h4 + 1) * 128], g.Ark[:, h4, :], v_t[:, h * 64:(h + 1) * 64], start=False, stop=True, reads=[g.Ark, v_t], writes=[bk])
                    if EV == "3":
                        P.mm(bk[:, h4 * 128:(h4 + 1) * 128], g.Arb[:, h4, :], g.X[:, h4, :], reads=[g.Arb, g.X], writes=[bk])
                hs = slice(gi * 4, (gi + 1) * 4)
                EVC = _os.environ.get("EVC", "0")
                if EVC != "1":
                    tt(P, "dve", v3(Rh)[:, hs, :], b4(bk)[:, :, 0:64], v3(r_t)[:, hs, :], ALU.add, [bk, r_t], [Rh])
                if EVC != "2":
                    copy_op(P, "dve", v3(Y)[:, hs, :], b4(bk)[:, :, 64:128], [bk], [Y])
            if CUT <= 5.5:
                continue
            for gi, g in enumerate(G):
                bk = bank(C); bk2 = bank(C)
                for h4 in range(4):
                    h = gi * 4 + h4
                    P.tr(bk[0:64, h4 * 128:(h4 + 1) * 128], g.X[:, h4, 0:64], C.ident[:], [g.X, C.ident], [bk])
                    P.tr(bk2[0:64, h4 * 128:(h4 + 1) * 128], Rh[:, h * 64:(h + 1) * 64], C.ident[:], [Rh, C.ident], [bk2])
                copy_op(P, "act", g.WT[:, 0:4, :], b4(bk)[0:64], [bk], [g.WT])
                copy_op(P, "dve", g.WT[:, 4:8, :], b4(bk2)[0:64], [bk2], [g.WT])
            if CUT <= 6:
                continue
            for gi, g in enumerate(G):
                hs = slice(gi * 4, (gi + 1) * 4)
                bu = bank(C); by = bank(C)
                for h4 in range(4):
                    h = gi * 4 + h4
                    P.mm(bu[:, h4 * 128:(h4 + 1) * 128], g.WT[:, h4, :], ST[:, h, :], reads=[g.WT, ST], writes=[bu])
                for h4 in range(4):
                    h = gi * 4 + h4
                    P.mm(by[:, h4 * 128:(h4 + 1) * 128], g.WT[:, 4 + h4, :], ST[:, h, :], reads=[g.WT, ST], writes=[by])
                for b in range(2):
                    ps_ = slice(b * 64, (b + 1) * 64)
                    tt(P, "dve", Ublk[ps_, hs, b * 64:(b + 1) * 64], b4(bu)[ps_, :, b * 64:(b + 1) * 64], g.X[ps_, :, 64:128], ALU.add,
                       [bu, g.X], [Ublk])
                    tt(P, "dve", v3(Y)[ps_, hs, :], b4(by)[ps_, :, b * 64:(b + 1) * 64], v3(Y)[ps_, hs, :], ALU.add, [by, Y], [Y])
                bs = bank(C)
                for h4 in range(4):
                    h = gi * 4 + h4
                    P.mm(bs[0:64, h4 * 128:(h4 + 1) * 128], kka_t[:, h * 64:(h + 1) * 64], Ublk[:, h, :], start=True, stop=False,
                         reads=[kka_t, Ublk], writes=[bs])
                    P.mm(bs[0:64, h4 * 128:(h4 + 1) * 128], kp_t[:, h * 64:(h + 1) * 64], Vblk[:, h, :], start=False, stop=True,
                         reads=[kp_t, Vblk], writes=[bs])
                tt(P, "dve", ST[:, hs, :], b4(bs)[0:64], ST[:, hs, :], ALU.add, [bs, ST], [ST])
                tt(P, "dve", ST[:, hs, :].rearrange("p h (b v) -> p h b v", b=2),
                   ST[:, hs, :].rearrange("p h (b v) -> p h b v", b=2),
                   gamT[:, hs, :].unsqueeze(3).broadcast_to([64, 4, 2, 64]), ALU.mult, [ST, gamT], [ST])
            if CUT <= 7:
                continue
            P.op("dve", lambda e: e.tensor_reduce(s16[:], v3(Y), AX.X, ALU.add), [Y], [s16])
            tt(P, "pool", t2[:], Y[:], Y[:], ALU.mult, [Y], [t2])
            P.op("dve", lambda e: e.tensor_reduce(s16b[:], v3(t2), AX.X, ALU.add), [t2], [s16b])
            P.op("dve", lambda e: e.tensor_scalar(s16[:], s16[:], 1.0 / 64, None, ALU.mult), [s16], [s16])
            tt(P, "pool", t2[:, 0:16], s16[:], s16[:], ALU.mult, [s16], [t2])
            stt(P, "dve", s16b[:], s16b[:], 1.0 / 64, t2[:, 0:16], ALU.mult, ALU.subtract, [s16b, t2], [s16b])
            rsqrt_op(P, s16b, 0, 16, GN_EPS, False)
            tt(P, "dve", v3(Y), v3(Y), s16[:].unsqueeze(2).broadcast_to([128, 16, 64]), ALU.subtract, [Y, s16], [Y])
            tt(P, "dve", v3(Y), v3(Y), s16b[:].unsqueeze(2).broadcast_to([128, 16, 64]), ALU.mult, [Y, s16b], [Y])
            tt(P, "pool", Y[:], Y[:], lnxg[:], ALU.mult, [Y, lnxg], [Y])
            tt(P, "pool", Y[:], Y[:], lnxb[:], ALU.add, [Y, lnxb], [Y])
            tt(P, "dve", Y[:], Y[:], bon_t[:], ALU.add, [Y, bon_t], [Y])
            tt(P, "dve", Y[:], Y[:], g_t[:], ALU.mult, [Y, g_t], [Y])
            if CUT <= 8:
                continue
            bA, bB = bank(C), bank(C)
            for kc in range(8):
                bk = bA if kc < 4 else bB
                P.tr(bk[:, (kc % 4) * 128:(kc % 4 + 1) * 128], Y[:, kc * 128:(kc + 1) * 128], C.ident[:], [Y, C.ident], [bk])
            copy_op(P, "act", oT[:, 0:4, :], b4(bA), [bA], [oT])
            copy_op(P, "dve", oT[:, 4:8, :], b4(bB), [bB], [oT])
            for hf in range(2):
                cs = slice(hf * 512, (hf + 1) * 512)
                bk = bank(C)
                for kc in range(8):
                    P.mm(bk[:], oT[:, kc, :], wo[:, kc, cs], start=(kc == 0), stop=(kc == 7), reads=[oT, C.wres], writes=[bk])
                copy_op(P, "act", t2[:, cs], bk[:], [bk], [t2])
                stt(P, "dve", t2[:, cs], xt[:, cs], ALPHA, t2[:, cs], ALU.mult, ALU.add, [xt, t2], [t2])
            layer_norm_tile(C, t2, Rh, lng, lnb, st)
            for b in range(2):
                P.dma("act", x_out[b, c * 64:(c + 1) * 64, :], Rh[b * 64:(b + 1) * 64, :], reads=[Rh])


SW_LIMIT = 7.0
SW_ALPHA = 1.702


def moe_stage(P, D, l, S, x_in, x_out, NE=32):
    T = 2 * S
    TQ = min(1024, T)
    NQ = T // TQ
    NT = TQ // 128
    NB = TQ // 512
    xin = x_in.rearrange("b s d -> (b s) d")
    xout = x_out.rearrange("b s d -> (b s) d")
    wgu_d = D["moe_w_gu"][l]
    wdn_d = D["moe_w_dn"][l]
    with P.scope():
        C = setup_common(P)
        dq_factory(C)
        C.wres = P.res("weights")
        wr = Tl(P, "wr", [128, 8, 32])
        P.dma("sp", wr[:], D["moe_router_w"][l].rearrange("(k p) e -> p k e", p=128), writes=[C.wres])
        br = load_bcast(P, "br", D["moe_router_b"][l], 32, "act")
        bdn = Tl(P, "bdn", [32, 1024])
        P.dma("sp", bdn[:], D["moe_b_dn"][l], writes=[C.wres])
        xT32f = Tl(P, "xT32", [128, 1024])
        C.junk = xT32f
        bgu_raw = xT32f
        bguT = Tl(P, "bguT", [128, 16, 32])
        for half in range(2):
            P.dma("act", bgu_raw[0:32, :], D["moe_b_gu"][l][:, half * 1024:(half + 1) * 1024], writes=[bgu_raw])
            for cb in range(2):
                bk = bank(C)
                for c4 in range(4):
                    cc = cb * 4 + c4
                    P.tr(bk[:, c4 * 32:(c4 + 1) * 32], bgu_raw[0:32, cc * 128:(cc + 1) * 128], C.ident[0:32, 0:32], [bgu_raw, C.ident], [bk])
                copy_op(P, "dve", bguT[:, half * 8 + cb * 4:half * 8 + (cb + 1) * 4, :], bk[:, 0:128].rearrange("p (c e) -> p c e", e=32), [bk], [bguT])
        bgu7 = Tl(P, "bgu7", [128, 16, 32])
        P.op("dve", lambda e: e.tensor_scalar(bgu7[:], bguT[:], SW_LIMIT, None, ALU.add), [bguT], [bgu7])
        lng = load_bcast(P, "lng", D["ln_g"][l, 1], 1024, "sp")
        lnb = load_bcast(P, "lnb", D["ln_b"][l, 1], 1024, "act")
        xT = Tl(P, "xT", [128, 8, TQ], BF16)
        acc = Tl(P, "acc", [128, NT, 1024])
        G = Tl(P, "G", [128, NT, 32])
        Wgu = [Tl(P, f"Wgu{i}", [128, 8, 2048], BF16) for i in range(2)]
        Wdn = [Tl(P, f"Wdn{i}", [128, 8, 1024], BF16) for i in range(2)]
        actT = [Tl(P, f"actT{i}", [128, 8, 512], BF16) for i in range(2)]
        xt = Tl(P, "xt", [128, 1024]); z = Tl(P, "z", [128, 1024])
        xT32 = xT32f
        x3 = xT32f[:].rearrange("p (k n) -> p k n", n=128)
        lg = Tl(P, "lg", [128, 32]); t8 = Tl(P, "t8", [128, 8]); msk = Tl(P, "msk", [128, 32]); st = Tl(P, "st", [128, 8])
        GT = Tl(P, "GT", [32, 128])
        tmp = [[Tl(P, f"sw{i}_{j}", [128, 512]) for j in range(3)] for i in range(2)]

        def load_expert(e, buf):
            for k0 in range(0, 8, 2):
                P.dma("pool", Wgu[buf][:, k0:k0 + 2, :], wgu_d[e, k0 * 128:(k0 + 2) * 128, :].rearrange("(k p) n -> p k n", p=128), writes=[Wgu[buf]])
            for k0 in range(0, 8, 4):
                P.dma("pool", Wdn[buf][:, k0:k0 + 4, :], wdn_d[e, k0 * 128:(k0 + 4) * 128, :].rearrange("(k p) n -> p k n", p=128), writes=[Wdn[buf]])

        seq = [(q, e) for q in range(NQ) for e in range(NE)]
        load_expert(0, 0)
        si = 0
        for q in range(NQ):
            t0 = q * TQ
            for t in range(NT):
                rows = slice(t0 + t * 128, t0 + (t + 1) * 128)
                P.dma("sp", xt[:], xin[rows, :], writes=[xt])
                bA, bB = bank(C), bank(C)
                for kc in range(8):
                    bk = bA if kc < 4 else bB
                    P.tr(bk[:, (kc % 4) * 128:(kc % 4 + 1) * 128], xt[:, kc * 128:(kc + 1) * 128], C.ident[:], [xt, C.ident], [bk])
                copy_op(P, "act", x3[:, 0:4, :], bA[:].rearrange("p (k n) -> p k n", n=128), [bA], [xT32])
                copy_op(P, "dve", x3[:, 4:8, :], bB[:].rearrange("p (k n) -> p k n", n=128), [bB], [xT32])
                copy_op(P, "pool", xT[:, :, t * 128:(t + 1) * 128], x3, [xT32], [xT])
                bk = bank(C)
                for kc in range(8):
                    P.mm(bk[:, 0:32], x3[:, kc, :], wr[:, kc, :], start=(kc == 0), stop=(kc == 7), reads=[xT32, C.wres], writes=[bk])
                tt(P, "dve", lg[:], bk[:, 0:32], br[:], ALU.add, [bk, br], [lg])
                P.op("dve", lambda e: e.max(t8[:], lg[:]), [lg], [t8])
                P.op("dve", lambda e: e.tensor_scalar(msk[:], lg[:], t8[:, 3:4], None, ALU.is_ge), [lg, t8], [msk])
                P.op("dve", lambda e: e.tensor_scalar(st[:, 0:1], t8[:, 0:1], -1.0, None, ALU.mult), [t8], [st])
                P.op("act", lambda e: e.activation(lg[:], lg[:], AF.Exp, bias=st[:, 0:1]), [lg, st], [lg])
                tt(P, "dve", lg[:], lg[:], msk[:], ALU.mult, [lg, msk], [lg])
                P.op("dve", lambda e: e.tensor_reduce(st[:, 1:2], lg[:], AX.X, ALU.add), [lg], [st])
                P.op("dve", lambda e: e.reciprocal(st[:, 2:3], st[:, 1:2]), [st], [st])
                P.op("dve", lambda e, t=t: e.tensor_scalar(G[:, t, :], lg[:], st[:, 2:3], None, ALU.mult), [lg, st], [G])
            P.op("pool", lambda e: e.memset(acc[:], 0.0), writes=[acc])
            for e in range(NE):
                buf = si % 2
                import os as _os
                if si + 1 < len(seq) and not (_os.environ.get("MOE_NOLOAD") and si > 0):
                    load_expert(seq[si + 1][1], (si + 1) % 2)
                si += 1
                for blk in range(NB):
                    ts = slice(blk * 512, (blk + 1) * 512)
                    aT = actT[blk % 2]
                    for fc in range(8):
                        tp = tmp[fc % 2]
                        bg, bu = bank(C), bank(C)
                        for kc in range(8):
                            P.mm(bg[:], Wgu[buf][:, kc, fc * 128:(fc + 1) * 128], xT[:, kc, ts], start=(kc == 0), stop=(kc == 7),
                                 reads=[Wgu[buf], xT], writes=[bg])
                        for kc in range(8):
                            P.mm(bu[:], Wgu[buf][:, kc, 1024 + fc * 128:1024 + (fc + 1) * 128], xT[:, kc, ts], start=(kc == 0), stop=(kc == 7),
                                 reads=[Wgu[buf], xT], writes=[bu])
                        gp, sg, u1 = tp
                        P.op("dve", lambda en, bg=bg, gp=gp, fc=fc, e=e: en.tensor_scalar(gp[:], bg[:], bguT[:, fc, e:e + 1], SW_LIMIT, ALU.add, ALU.min),
                             [bg, bguT], [gp])
                        P.op("act", lambda en, gp=gp, sg=sg: en.activation(sg[:], gp[:], AF.Silu, scale=SW_ALPHA), [gp], [sg])
                        P.op("act", lambda en, bu=bu, u1=u1, fc=fc, e=e: en.activation(u1[:], bu[:], AF.Relu, bias=bgu7[:, 8 + fc, e:e + 1]),
                             [bu, bgu7], [u1])
                        P.op("dve", lambda en, u1=u1: en.tensor_scalar(u1[:], u1[:], 2.0 * SW_LIMIT, 1.0 - SW_LIMIT, ALU.min, ALU.add), [u1], [u1])
                        stt(P, "dve", aT[:, fc, :], sg[:], 1.0 / SW_ALPHA, u1[:], ALU.mult, ALU.mult, [sg, u1], [aT])
                    for t4 in range(4):
                        tile_i = blk * 4 + t4
                        for hf in range(2):
                            cs = slice(hf * 512, (hf + 1) * 512)
                            bk = bank(C)
                            for fc in range(8):
                                P.mm(bk[:], aT[:, fc, t4 * 128:(t4 + 1) * 128], Wdn[buf][:, fc, cs], start=(fc == 0), stop=(fc == 7),
                                     reads=[aT, Wdn[buf]], writes=[bk])
                            stt(P, "dve", acc[:, tile_i, cs], bk[:], G[:, tile_i, e:e + 1], acc[:, tile_i, cs], ALU.mult, ALU.add, [bk, G, acc], [acc])
            for t in range(NT):
                rows = slice(t0 + t * 128, t0 + (t + 1) * 128)
                bk = bank(C)
                P.tr(bk[0:32, 0:128], G[:, t, :], C.ident[:], [G, C.ident], [bk])
                copy_op(P, "act", GT[:], bk[0:32, 0:128], [bk], [GT])
                P.dma("sp", xt[:], xin[rows, :], writes=[xt])
                for hf in range(2):
                    cs = slice(hf * 512, (hf + 1) * 512)
                    bk = bank(C)
                    P.mm(bk[:], GT[:], bdn[:, cs], reads=[GT, C.wres], writes=[bk])
                    tt(P, "dve", z[:, cs], bk[:], acc[:, t, cs], ALU.add, [bk, acc], [z])
                stt(P, "dve", z[:], xt[:], ALPHA, z[:], ALU.mult, ALU.add, [xt, z], [z])
                layer_norm_tile(C, z, xt, lng, lnb, st)
                P.dma("act", xout[rows, :], xt[:], reads=[xt])


NEG = -1.0e30
SUBLN_EPS = 1e-5


def xT_block(C, xin, r0, nt, xt, xTb):
    P = C.P
    for t in range(nt):
        P.dma("sp", xt[:], xin[r0 + t * 128:r0 + (t + 1) * 128, :], writes=[xt])
        bA, bB = bank(C), bank(C)
        for kc in range(8):
            bk = bA if kc < 4 else bB
            P.tr(bk[:, (kc % 4) * 128:(kc % 4 + 1) * 128], xt[:, kc * 128:(kc + 1) * 128], C.ident[:], [xt, C.ident], [bk])
        copy_op(P, "act", xTb[:, 0:4, t * 128:(t + 1) * 128], bA[:].rearrange("p (k n) -> p k n", n=128), [bA], [xTb])
        copy_op(P, "dve", xTb[:, 4:8, t * 128:(t + 1) * 128], bB[:].rearrange("p (k n) -> p k n", n=128), [bB], [xTb])


def proj_stage(P, D, S, x_in, w_T_dram, T_scr, w_tok_dram=None, tok_scr=None):
    T = 2 * S
    NBLK = T // 512
    xin = x_in.rearrange("b s d -> (b s) d")
    with P.scope():
        C = setup_common(P)
        dq_factory(C)
        C.wres = P.res("weights")
        wT = Tl(P, "wT", [128, 8, 1024], BF16)
        for k0 in range(0, 8, 2):
            P.dma("pool", wT[:, k0:k0 + 2, :], w_T_dram[k0 * 128:(k0 + 2) * 128, :].rearrange("(k p) n -> p k n", p=128), writes=[C.wres])
        if w_tok_dram is not None:
            wK = Tl(P, "wK", [128, 8, 1024], BF16)
            for k0 in range(0, 8, 2):
                P.dma("pool", wK[:, k0:k0 + 2, :], w_tok_dram[k0 * 128:(k0 + 2) * 128, :].rearrange("(k p) n -> p k n", p=128), writes=[C.wres])
        xt = Tl(P, "xt", [128, 1024])
        xTb = [Tl(P, f"xTb{i}", [128, 8, 512], BF16) for i in range(2)]
        oT = [Tl(P, f"oT{i}", [64, 512]) for i in range(4)]
        ot = [Tl(P, f"ot{i}", [128, 1024]) for i in range(2)]
        for blk in range(NBLK):
            xb = xTb[blk % 2]
            xT_block(C, xin, blk * 512, 4, xt, xb)
            for g in range(16):
                bk = bank(C)
                for kc in range(8):
                    P.mm(bk[0:64, :], wT[:, kc, g * 64:(g + 1) * 64], xb[:, kc, :], start=(kc == 0), stop=(kc == 7), reads=[C.wres, xb], writes=[bk])
                o = oT[g % 4]
                copy_op(P, ev_eng(C), o[:], bk[0:64, :], [bk], [o])
                P.dma(C.dq(), T_scr[g, :, blk * 512:(blk + 1) * 512], o[:], reads=[o])
            if w_tok_dram is not None:
                for t4 in range(4):
                    o = ot[t4 % 2]
                    for hf in range(2):
                        cs = slice(hf * 512, (hf + 1) * 512)
                        bk = bank(C)
                        for kc in range(8):
                            P.mm(bk[:], xb[:, kc, t4 * 128:(t4 + 1) * 128], wK[:, kc, cs], start=(kc == 0), stop=(kc == 7), reads=[C.wres, xb], writes=[bk])
                        copy_op(P, ev_eng(C), o[:, cs], bk[:], [bk], [o])
                    P.dma(C.dq(), tok_scr[blk * 512 + t4 * 128:blk * 512 + (t4 + 1) * 128, :], o[:], reads=[o])


def attn_stage(P, D, j, S, qT_scr, kT_scr, v_scr, o_scr):
    import math
    l = NA + j
    lam_init = 0.8 - 0.6 * math.exp(-0.3 * l)
    NQB = S // 128
    with P.scope():
        C = setup_common(P)
        dq_factory(C)
        lamt = load_bcast(P, "lamt", D["da_lambda"][j].rearrange("a d -> (a d)"), 256, "sp")
        lsc = Tl(P, "lsc", [128, 8]); ljunk = Tl(P, "ljunk", [128, 64])
        tt(P, "dve", ljunk[:], lamt[:, 0:64], lamt[:, 64:128], ALU.mult, [lamt], [ljunk])
        P.op("dve", lambda e: e.tensor_reduce(lsc[:, 0:1], ljunk[:], AX.X, ALU.add), [ljunk], [lsc])
        tt(P, "dve", ljunk[:], lamt[:, 128:192], lamt[:, 192:256], ALU.mult, [lamt, ljunk], [ljunk])
        P.op("dve", lambda e: e.tensor_reduce(lsc[:, 1:2], ljunk[:], AX.X, ALU.add), [ljunk], [lsc])
        P.op("act", lambda e: e.activation(lsc[:, 2:4], lsc[:, 0:2], AF.Exp), [lsc], [lsc])
        tt(P, "dve", lsc[:, 4:5], lsc[:, 3:4], lsc[:, 2:3], ALU.subtract, [lsc], [lsc])
        P.op("dve", lambda e: e.tensor_scalar(lsc[:, 5:6], lsc[:, 4:5], -lam_init, None, ALU.add), [lsc], [lsc])
        gsc = load_bcast(P, "gsc", D["da_subln_g"][j], 128, "act")
        P.op("dve", lambda e: e.tensor_scalar(gsc[:], gsc[:], 1.0 - lam_init, None, ALU.mult), [gsc], [gsc])
        D0i = Tl(P, "D0i", [128, S], I32)
        D0f = Tl(P, "D0f", [128, S])
        P.op("pool", lambda e: e.iota(D0i[:], [[-1, S]], base=S - 128, channel_multiplier=1), writes=[D0i])
        copy_op(P, "dve", D0f[:], D0i[:], [D0i], [D0f])
        Bh = [Tl(P, f"Bh{i}", [128, S]) for i in range(2)]
        kT = [Tl(P, f"kT{i}", [64, 2, S]) for i in range(2)]
        qT = [Tl(P, f"qT{i}", [64, 2, S]) for i in range(2)]
        vv = [Tl(P, f"vv{i}", [128, NQB, 128]) for i in range(2)]
        tmp_all = [[Tl(P, f"tmp{p}_{i}", [128, S]) for i in range(2)] for p in range(2)]
        attnT_all = [Tl(P, f"attnT{p}", [128, NQB, 128]) for p in range(2)]
        sc_all = [Tl(P, f"sc{p}", [128, 16]) for p in range(2)]
        osb_all = [Tl(P, f"osb{p}", [128, 128]) for p in range(2)]
        oo = [Tl(P, f"oo{i}", [128, 128]) for i in range(2)]; ojunk = Tl(P, "ojunk", [128, 128])
        items = [(b, h, i) for b in range(2) for h in range(8) for i in range(NQB)]

        def head_setup(b, h, st_):
            for c in range(2):
                P.dma("sp", kT[st_][:, c, :], kT_scr[h * 2 + c, :, b * S:(b + 1) * S], writes=[kT[st_]])
                P.dma("act", qT[st_][:, c, :], qT_scr[h * 2 + c, :, b * S:(b + 1) * S], writes=[qT[st_]])
            P.dma("sp", vv[st_][:], v_scr[b * S:(b + 1) * S, h * 128:(h + 1) * 128].rearrange("(t p) d -> p t d", p=128), writes=[vv[st_]])
            slope = 2.0 ** (-(h + 1))
            P.op("act", lambda e, slope=slope, st_=st_: e.activation(Bh[st_][:], D0f[:], AF.Copy, scale=-slope), [D0f], [Bh[st_]])
            P.op("pool", lambda e, st_=st_: e.affine_select(Bh[st_][:], Bh[st_][:], [[-1, S]], ALU.is_ge, NEG, base=S - 128, channel_multiplier=1),
                 [Bh[st_]], [Bh[st_]])

        def stage_a(n):
            b, h, i = items[n]
            st_ = (b * 8 + h) % 2
            if i == 0:
                head_setup(b, h, st_)
            nk = (i + 1) * 128
            tmp = tmp_all[n % 2]; sc = sc_all[n % 2]
            for c in range(2):
                tc_ = tmp[c]
                for k0 in range(0, nk, 512):
                    kw = min(512, nk - k0)
                    bk = bank(C)
                    P.mm(bk[:, 0:kw], qT[st_][:, c, i * 128:(i + 1) * 128], kT[st_][:, c, k0:k0 + kw], reads=[qT[st_], kT[st_]], writes=[bk])
                    stt(P, "dve", tc_[:, k0:k0 + kw], bk[:, 0:kw], 0.125, Bh[st_][:, S - nk + k0:S - nk + k0 + kw], ALU.mult, ALU.add, [bk, Bh[st_]], [tc_])
                P.op("dve", lambda e, tc_=tc_, nk=nk, c=c, sc=sc: e.tensor_reduce(sc[:, c:c + 1], tc_[:, 0:nk], AX.X, ALU.max), [tc_], [sc])
                P.op("dve", lambda e, c=c, sc=sc: e.tensor_scalar(sc[:, 2 + c:3 + c], sc[:, c:c + 1], -1.0, None, ALU.mult), [sc], [sc])
                P.op("act", lambda e, tc_=tc_, nk=nk, c=c, sc=sc: e.activation(tc_[:, 0:nk], tc_[:, 0:nk], AF.Exp, bias=sc[:, 2 + c:3 + c],
                                                                            accum_out=sc[:, 4 + c:5 + c]), [tc_, sc], [tc_, sc])
            P.op("dve", lambda e, sc=sc: e.reciprocal(sc[:, 6:8], sc[:, 4:6]), [sc], [sc])
            tt(P, "dve", sc[:, 8:9], sc[:, 7:8], lsc[:, 5:6], ALU.mult, [sc, lsc], [sc])
            P.op("dve", lambda e, nk=nk, tmp=tmp, sc=sc: e.tensor_scalar(tmp[1][:, 0:nk], tmp[1][:, 0:nk], sc[:, 8:9], None, ALU.mult), [tmp[1], sc], [tmp[1]])
            stt(P, "dve", tmp[0][:, 0:nk], tmp[0][:, 0:nk], sc[:, 6:7], tmp[1][:, 0:nk], ALU.mult, ALU.add, [tmp[0], tmp[1], sc], [tmp[0]])

        def stage_b(n):
            b, h, i = items[n]
            st_ = (b * 8 + h) % 2
            tmp = tmp_all[n % 2]; attnT = attnT_all[n % 2]; sc = sc_all[n % 2]; osb = osb_all[n % 2]
            for k0 in range(0, i + 1, 4):
                k1 = min(i + 1, k0 + 4)
                bk = bank(C)
                for kt in range(k0, k1):
                    P.tr(bk[:, (kt - k0) * 128:(kt - k0 + 1) * 128], tmp[0][:, kt * 128:(kt + 1) * 128], C.ident[:], [tmp[0], C.ident], [bk])
                copy_op(P, ev_eng(C), attnT[:, k0:k1, :], bk[:, 0:(k1 - k0) * 128].rearrange("p (k n) -> p k n", n=128), [bk], [attnT])
            bk = bank(C)
            for kt in range(i + 1):
                P.mm(bk[:, 0:128], attnT[:, kt, :], vv[st_][:, kt, :], start=(kt == 0), stop=(kt == i), reads=[attnT, vv[st_]], writes=[bk])
            copy_op(P, "dve", osb[:], bk[:, 0:128], [bk], [osb])
            P.op("act", lambda e, osb=osb, sc=sc: e.activation(ojunk[:], osb[:], AF.Square, accum_out=sc[:, 9:10]), [osb], [ojunk, sc])
            P.op("dve", lambda e, sc=sc: e.tensor_scalar(sc[:, 10:11], sc[:, 9:10], 1.0 / 128, SUBLN_EPS, ALU.mult, ALU.add), [sc], [sc])
            P.op("act", lambda e, sc=sc: e.activation(sc[:, 10:11], sc[:, 10:11], AF.Sqrt), [sc], [sc])
            P.op("dve", lambda e, sc=sc: e.reciprocal(sc[:, 10:11], sc[:, 10:11]), [sc], [sc])
            o = oo[n % 2]
            stt(P, "dve", o[:], osb[:], sc[:, 10:11], gsc[:], ALU.mult, ALU.mult, [osb, sc, gsc], [o])
            P.dma(C.dq(), o_scr[b * S + i * 128:b * S + (i + 1) * 128, h * 128:(h + 1) * 128], o[:], reads=[o])

        for n in range(len(items) + 1):
            if n < len(items):
                stage_a(n)
            if n >= 1:
                stage_b(n - 1)


def outproj_ln_stage(P, D, S, o_scr, w_dram, x_in, x_out, lng_ap, lnb_ap):
    T = 2 * S
    xin = x_in.rearrange("b s d -> (b s) d")
    xout = x_out.rearrange("b s d -> (b s) d")
    with P.scope():
        C = setup_common(P)
        dq_factory(C)
        C.wres = P.res("weights")
        C.junk = Tl(P, "junk", [128, 1024])
        wo = Tl(P, "wo", [128, 8, 1024], BF16)
        for k0 in range(0, 8, 2):
            P.dma("pool", wo[:, k0:k0 + 2, :], w_dram[k0 * 128:(k0 + 2) * 128, :].rearrange("(k p) n -> p k n", p=128), writes=[C.wres])
        lng = load_bcast(P, "lng", lng_ap, 1024, "sp")
        lnb = load_bcast(P, "lnb", lnb_ap, 1024, "act")
        ot = Tl(P, "ot", [128, 1024]); oTb = Tl(P, "oTb", [128, 8, 128], BF16)
        xt = [Tl(P, f"xt{i}", [128, 1024]) for i in range(2)]; z = Tl(P, "z", [128, 1024]); st = Tl(P, "st", [128, 8])
        res = [Tl(P, f"res{i}", [128, 1024]) for i in range(2)]
        for t in range(T // 128):
            rows = slice(t * 128, (t + 1) * 128)
            xx = xt[t % 2]
            P.dma("act", xx[:], xin[rows, :], writes=[xx])
            xT_block(C, o_scr, t * 128, 1, ot, oTb)
            for hf in range(2):
                cs = slice(hf * 512, (hf + 1) * 512)
                bk = bank(C)
                for kc in range(8):
                    P.mm(bk[:], oTb[:, kc, :], wo[:, kc, cs], start=(kc == 0), stop=(kc == 7), reads=[oTb, C.wres], writes=[bk])
                copy_op(P, "act", z[:, cs], bk[:], [bk], [z])
            stt(P, "dve", z[:], xx[:], ALPHA, z[:], ALU.mult, ALU.add, [xx, z], [z])
            r = res[t % 2]
            layer_norm_tile(C, z, r, lng, lnb, st)
            P.dma("sp", xout[rows, :], r[:], reads=[r])


INPUT_SHAPES = {
    "ln_g": [4, 2, 1024], "ln_b": [4, 2, 1024], "rwkv_mix": [2, 6, 1024], "rwkv_w_rkv": [2, 3, 1024, 1024],
    "rwkv_w_o": [2, 1024, 1024], "rwkv_w0": [2, 1024], "rwkv_w1": [2, 1024, 64], "rwkv_w2": [2, 64, 1024],
    "rwkv_a0": [2, 1024], "rwkv_a1": [2, 1024, 64], "rwkv_a2": [2, 64, 1024], "rwkv_g1": [2, 1024, 160],
    "rwkv_g2": [2, 160, 1024], "rwkv_k_k": [2, 1024], "rwkv_k_a": [2, 1024], "rwkv_r_k": [2, 16, 64],
    "rwkv_lnx_g": [2, 1024], "rwkv_lnx_b": [2, 1024], "rwkv_v0": [1, 1024], "rwkv_v1": [1, 1024, 32],
    "rwkv_v2": [1, 32, 1024], "kv_w": [1024, 2048], "da_w_q": [2, 1024, 1024], "da_w_o": [2, 1024, 1024],
    "da_lambda": [2, 4, 64], "da_subln_g": [2, 128], "moe_router_w": [4, 1024, 32], "moe_router_b": [4, 32],
    "moe_w_gu": [4, 32, 1024, 2048], "moe_b_gu": [4, 32, 2048], "moe_w_dn": [4, 32, 1024, 1024], "moe_b_dn": [4, 32, 1024],
}
RWKV_KEYS = [k for k in INPUT_SHAPES if k.startswith("rwkv_")] + ["ln_g", "ln_b"]
SCR_NAMES = ("r", "kp", "v", "sg", "kn", "kka", "bonus", "g", "vf")


def build_program(S, plan, keys, shapes=None, ne=32):
    nc = bass.Bass("TRN2", target_bir_lowering=False)
    shapes = shapes or {}
    D = {k: nc.dram_tensor(k, shapes.get(k, INPUT_SHAPES[k]), F32, kind="ExternalInput").ap() for k in keys}
    x = nc.dram_tensor("x", [2, S, 1024], F32, kind="ExternalInput").ap()
    out = nc.dram_tensor("out", [2, S, 1024], F32, kind="ExternalOutput").ap()
    xa = nc.dram_tensor("xa", [2, S, 1024], F32).ap()
    xb = nc.dram_tensor("xb", [2, S, 1024], F32).ap()
    NCH = S // 64
    scr = {n: nc.dram_tensor("scr_" + n, [NCH, 128, 1024], F32).ap() for n in SCR_NAMES}
    ascr = {"kT": nc.dram_tensor("scr_kT", [16, 64, 2 * S], F32).ap(), "qT": nc.dram_tensor("scr_qT", [16, 64, 2 * S], F32).ap(),
            "v": nc.dram_tensor("scr_vsh", [2 * S, 1024], F32).ap(), "o": nc.dram_tensor("scr_o", [2 * S, 1024], F32).ap()}
    P = Prog(nc)
    bufs = {"x": x, "out": out, "xa": xa, "xb": xb}
    for stg in plan:
        kind = stg[0]
        if kind == "rwkv":
            _, l, src, dst = stg
            import os as _os
            if _os.environ.get("ONLY") != "2":
                rwkv_pass1(P, D, l, S, bufs[src], scr)
            if _os.environ.get("ONLY") != "1":
                rwkv_pass2(P, D, l, S, bufs[src], bufs[dst], scr)
        elif kind == "kvproj":
            _, src_ = stg
            proj_stage(P, D, S, bufs[src_], D["kv_w"][:, 0:1024], ascr["kT"], D["kv_w"][:, 1024:2048], ascr["v"])
        elif kind == "attn":
            _, j, src_, dst = stg
            proj_stage(P, D, S, bufs[src_], D["da_w_q"][j], ascr["qT"])
            attn_stage(P, D, j, S, ascr["qT"], ascr["kT"], ascr["v"], ascr["o"])
            outproj_ln_stage(P, D, S, ascr["o"], D["da_w_o"][j], bufs[src_], bufs[dst], D["ln_g"][NA + j, 0], D["ln_b"][NA + j, 0])
        elif kind == "moe":
            _, l, src_, dst = stg
            moe_stage(P, D, l, S, bufs[src_], bufs[dst], NE=ne)
        else:
            raise ValueError(kind)
    P.finish()
    return nc, P


FULL_PLAN = [("rwkv", 0, "x", "xa"), ("moe", 0, "xa", "xb"), ("rwkv", 1, "xb", "xa"), ("moe", 1, "xa", "xb"),
             ("kvproj", "xb"), ("attn", 0, "xb", "xa"), ("moe", 2, "xa", "xb"), ("attn", 1, "xb", "xa"), ("moe", 3, "xa", "out")]


def kernel(**inputs):
    from concourse.bass_utils import run_bass_kernel_spmd
    n = 8
    S = 2048
    x = np.ascontiguousarray(np.asarray(inputs["x"], dtype=np.float32))
    keys = list(INPUT_SHAPES.keys())
    nc, P = build_program(S, FULL_PLAN, keys)
    shared = {k: np.ascontiguousarray(np.asarray(inputs[k], dtype=np.float32)) for k in keys}
    in_maps = []
    for c in range(n):
        m = dict(shared)
        m["x"] = x[2 * c:2 * c + 2]
        in_maps.append(m)
    res = run_bass_kernel_spmd(nc, in_maps, core_ids=list(range(n)))
    return np.concatenate([r["out"] for r in res.results], axis=0).astype(np.float32)
```

```python
import contextlib
import numpy as np
import concourse.bass as bass
import concourse.mybir as mybir

F32 = mybir.dt.float32
BF16 = mybir.dt.bfloat16
I32 = mybir.dt.int32
ALU = mybir.AluOpType
AF = mybir.ActivationFunctionType
AX = mybir.AxisListType

N_DMA_SEMS = 40


class Res:
    __slots__ = ("name", "last_w", "readers")

    def __init__(self, name=""):
        self.name = name
        self.last_w = None
        self.readers = []


class Op:
    __slots__ = ("eng", "fn", "deps", "marked", "is_dma", "sem", "val", "idx")

    def __init__(self, eng, fn, is_dma):
        self.eng = eng
        self.fn = fn
        self.deps = []
        self.marked = False
        self.is_dma = is_dma
        self.sem = None
        self.val = 0


ENGS = ("pe", "dve", "act", "pool", "sp")


class Prog:
    def __init__(self, nc):
        self.nc = nc
        self.base = contextlib.ExitStack()
        self.csem = {e: self.base.enter_context(nc.semaphore(f"s_{e}")) for e in ENGS}
        self.dsem = [self.base.enter_context(nc.semaphore(f"d_{i}")) for i in range(N_DMA_SEMS)]
        self.ccount = {e: 0 for e in ENGS}
        self.dma_rr = 0
        self.dma_last = [None] * N_DMA_SEMS
        self.dma_cnt = [0] * N_DMA_SEMS
        self.nres = 0
        self.stack = None
        self.total = {e: 0 for e in ENGS}
        self._reset()

    def _reset(self):
        self.ops = {e: [] for e in ENGS}

    @contextlib.contextmanager
    def scope(self, final=False):
        self.stack = contextlib.ExitStack()
        try:
            yield self
            self.flush(final)
        finally:
            self.stack.close()
            self.stack = None

    def sb(self, name, shape, dtype=F32):
        self.nres += 1
        return self.stack.enter_context(self.nc.sbuf_tensor(f"{name}_{self.nres}", list(shape), dtype))

    def ps(self, name, shape, dtype=F32):
        self.nres += 1
        return self.stack.enter_context(self.nc.psum_tensor(f"{name}_{self.nres}", list(shape), dtype))

    def res(self, name=""):
        self.nres += 1
        return Res(name or f"r{self.nres}")

    def _add(self, eng, fn, reads, writes, is_dma):
        op = Op(eng, fn, is_dma)
        deps = []
        for r in reads:
            if r.last_w is not None:
                deps.append(r.last_w)
        for w in writes:
            if w.last_w is not None:
                deps.append(w.last_w)
            deps.extend(w.readers)
        seen = set()
        for d in deps:
            if id(d) in seen or d is op:
                continue
            seen.add(id(d))
            if (not d.is_dma) and (not is_dma) and d.eng == "pe" and eng == "pe":
                continue
            op.deps.append(d)
            d.marked = True
        if is_dma:
            k = self.dma_rr
            self.dma_rr = (self.dma_rr + 1) % N_DMA_SEMS
            prev = self.dma_last[k]
            if prev is not None and all(prev is not x for x in op.deps):
                op.deps.append(prev)
            self.dma_last[k] = op
            self.dma_cnt[k] += 16
            op.sem = k
            op.val = self.dma_cnt[k]
            op.marked = True
        for r in reads:
            if not is_dma:
                r.readers = [o for o in r.readers if o.is_dma or o.eng != eng]
            r.readers.append(op)
        for w in writes:
            w.last_w = op
            w.readers = []
        self.ops[eng].append(op)
        return op

    def op(self, eng, fn, reads=(), writes=()):
        return self._add(eng, fn, _rs(reads), _rs(writes), False)

    def dma(self, eng, out, in_, reads=(), writes=(), **kw):
        return self._add(eng, lambda e: e.dma_start(out=out, in_=in_, **kw),
                         _rs(reads), _rs(writes), True)

    def mm(self, out, lhsT, rhs, start=True, stop=True, reads=(), writes=()):
        return self.op("pe", lambda e: e.matmul(out, lhsT, rhs, start=start, stop=stop), reads, writes)

    def tr(self, out, in_, ident, reads=(), writes=()):
        return self.op("pe", lambda e: e.transpose(out, in_, ident), reads, writes)

    def flush(self, final=False):
        nc = self.nc
        csem, dsem = self.csem, self.dsem
        lastc = []
        for e in ENGS:
            comp = [o for o in self.ops[e] if not o.is_dma and o.fn is not None]
            if comp:
                comp[-1].marked = True
                lastc.append(comp[-1])
        lastd = [o for o in self.dma_last if o is not None]
        for e in ENGS:
            b = Op(e, None, False)
            b.deps = list(lastc) + list(lastd)
            self.ops[e].append(b)
        for e in ENGS:
            c = self.ccount[e]
            for op in self.ops[e]:
                if op.is_dma or op.fn is None:
                    continue
                if op.marked:
                    c += 1
                    op.val = c
            self.ccount[e] = c

        def semof(d):
            return (("d", d.sem), dsem[d.sem]) if d.is_dma else (("c", d.eng), csem[d.eng])

        def replay(ename):
            def body(e):
                known = {}
                for op in self.ops[ename]:
                    for d in op.deps:
                        key, sem = semof(d)
                        if known.get(key, 0) >= d.val:
                            continue
                        e.wait_ge(sem, d.val)
                        known[key] = d.val
                    if op.fn is None:
                        continue
                    ins = op.fn(e)
                    if op.is_dma:
                        ins.then_inc(dsem[op.sem], 16)
                    elif op.marked:
                        ins.then_inc(csem[ename], 1)
            return body

        with nc.Block() as block:
            block.sync(replay("sp"))
            block.scalar(replay("act"))
            block.vector(replay("dve"))
            block.gpsimd(replay("pool"))
            block.tensor(replay("pe"))
        for e in ENGS:
            self.total[e] += len(self.ops[e])
        self._reset()

    def finish(self):
        self.base.close()


class Tl:
    def __init__(self, P, name, shape, dtype=F32, psum=False):
        self.t = (P.ps if psum else P.sb)(name, shape, dtype)
        self.r = P.res(name)
        self.shape = shape

    def __getitem__(self, k):
        return self.t[k]


def _rs(xs):
    return [x.r if isinstance(x, Tl) else x for x in xs]


D_MODEL = 1024
DEPTH = 4
NA = 2
H_R = 16
EXPM05 = float(np.exp(-0.5))
ALPHA = (2 * DEPTH) ** 0.25
LN_EPS = 1e-5
GN_EPS = 64e-5


class Ctx:
    pass


def setup_common(P):
    C = Ctx()
    C.P = P
    C.ident = Tl(P, "ident", [128, 128])
    P.op("pool", lambda e: e.memset(C.ident[:], 0.0), writes=[C.ident])
    P.op("pool", lambda e: e.affine_select(C.ident[:], C.ident[:], [[-1, 128]], ALU.not_equal, 1.0,
                                           base=0, channel_multiplier=1), reads=[C.ident], writes=[C.ident])
    C.banks = [Tl(P, f"bank{i}", [128, 512], F32, psum=True) for i in range(8)]
    C.bi = 0
    C.rr = 0
    return C


def bank(C):
    b = C.banks[C.bi]
    C.bi = (C.bi + 1) % 8
    return b


def ev_eng(C):
    C.rr ^= 1
    return "act" if C.rr else "dve"


def copy_op(P, eng, out, in_, reads, writes):
    if eng == "act":
        P.op("act", lambda e: e.activation(out, in_, AF.Copy), reads, writes)
    else:
        P.op(eng, lambda e: e.tensor_copy(out, in_), reads, writes)


def tt(P, eng, out, in0, in1, op, reads, writes):
    P.op(eng, lambda e: e.tensor_tensor(out, in0, in1, op), reads, writes)


def stt(P, eng, out, in0, scalar, in1, op0, op1, reads, writes):
    P.op(eng, lambda e: e.scalar_tensor_tensor(out, in0, scalar, in1, op0, op1), reads, writes)


def rsqrt_op(P, t, lo, hi, bias, use_max):
    sl = t[:, lo:hi]
    op0 = ALU.max if use_max else ALU.add
    P.op("dve", lambda e: e.tensor_scalar(sl, sl, bias, None, op0), [t], [t])
    P.op("act", lambda e: e.activation(sl, sl, AF.Sqrt), [t], [t])
    P.op("dve", lambda e: e.reciprocal(sl, sl), [t], [t])


def load_bcast(P, name, src_1d, n, eng="sp"):
    t = Tl(P, name, [128, n])
    P.dma(eng, t[:], src_1d.partition_broadcast(128), writes=[t])
    return t


def layer_norm_tile(C, z, out, g_t, b_t, st):
    P = C.P
    junk = C.junk
    P.op("act", lambda e: e.activation(junk[:], z[:], AF.Copy, accum_out=st[:, 0:1]), [z], [junk, st])
    P.op("act", lambda e: e.activation(junk[:], z[:], AF.Square, accum_out=st[:, 1:2]), [z], [junk, st])
    P.op("dve", lambda e: e.tensor_scalar(st[:, 2:3], st[:, 0:1], 1.0 / 1024, None, ALU.mult), [st], [st])
    P.op("dve", lambda e: e.tensor_tensor(st[:, 3:4], st[:, 2:3], st[:, 2:3], ALU.mult), [st], [st])
    P.op("dve", lambda e: e.scalar_tensor_tensor(st[:, 4:5], st[:, 1:2], 1.0 / 1024, st[:, 3:4], ALU.mult, ALU.subtract), [st], [st])
    copy_op(P, "dve", st[:, 5:6], st[:, 4:5], [st], [st])
    rsqrt_op(P, st, 5, 6, LN_EPS, False)
    P.op("dve", lambda e: e.scalar_tensor_tensor(st[:, 6:7], st[:, 2:3], -1.0, st[:, 5:6], ALU.mult, ALU.mult), [st], [st])
    P.op("act", lambda e: e.activation(out[:], z[:], AF.Identity, bias=st[:, 6:7], scale=st[:, 5:6]), [z, st], [out])
    tt(P, "dve", out[:], out[:], g_t[:], ALU.mult, [out, g_t], [out])
    tt(P, "pool", out[:], out[:], b_t[:], ALU.add, [out, b_t], [out])


def load_w_bf16(C, dst, src2d, K, N, stage):
    P = C.P
    kcs = K // 128
    per = max(1, stage.shape[1] // N)
    for k0 in range(0, kcs, per):
        k1 = min(kcs, k0 + per)
        sv = stage[:, 0:(k1 - k0) * N].rearrange("p (k n) -> p k n", n=N)
        P.dma(C.dq(), sv, src2d[k0 * 128:k1 * 128, :].rearrange("(k p) n -> p k n", p=128), writes=[stage])
        copy_op(P, ev_eng(C), dst[:, k0:k1, :], sv, [stage], [C.wres])


def dq_factory(C):
    qs = ["sp", "act", "pool"]
    C.dqi = 0

    def dq():
        C.dqi = (C.dqi + 1) % 2
        return qs[C.dqi]
    C.dq = dq


def rwkv_pass1(P, D, l, S, x_in, scr):
    NCH = S // 64
    with P.scope():
        C = setup_common(P)
        dq_factory(C)
        C.wres = P.res("weights")
        C.junk = Tl(P, "junk", [128, 1024])
        stage = Tl(P, "wstage", [128, 2048])
        wr = Tl(P, "wrkv", [128, 3 * 8, 1024], BF16)
        for j in range(3):
            load_w_bf16(C, wr.t[:, j * 8:(j + 1) * 8, :], D["rwkv_w_rkv"][l, j], 1024, 1024, stage)
        w1 = Tl(P, "w1", [128, 8, 64], BF16)
        load_w_bf16(C, w1.t, D["rwkv_w1"][l], 1024, 64, stage)
        a1 = Tl(P, "a1", [128, 8, 64], BF16)
        load_w_bf16(C, a1.t, D["rwkv_a1"][l], 1024, 64, stage)
        g1 = Tl(P, "g1", [128, 8, 160], BF16)
        load_w_bf16(C, g1.t, D["rwkv_g1"][l], 1024, 160, stage)
        if l > 0:
            v1 = Tl(P, "v1", [128, 8, 32], BF16)
            load_w_bf16(C, v1.t, D["rwkv_v1"][l - 1], 1024, 32, stage)
            v2 = Tl(P, "v2", [65, 1024])
            P.op("dve", lambda e: e.memset(v2[:], 0.0), writes=[C.wres])
            P.dma("sp", v2[0:32, :], D["rwkv_v2"][l - 1], writes=[C.wres])
            P.dma("sp", v2[64:65, :], D["rwkv_v0"][l - 1:l, :], writes=[C.wres])
        w2 = Tl(P, "w2", [65, 1024])
        P.dma("sp", w2[0:64, :], D["rwkv_w2"][l], writes=[C.wres])
        P.dma("sp", w2[64:65, :], D["rwkv_w0"][l:l + 1, :], writes=[C.wres])
        a2 = Tl(P, "a2", [65, 1024])
        P.dma("act", a2[0:64, :], D["rwkv_a2"][l], writes=[C.wres])
        P.dma("act", a2[64:65, :], D["rwkv_a0"][l:l + 1, :], writes=[C.wres])
        g2a = Tl(P, "g2a", [128, 1024])
        g2b = Tl(P, "g2b", [32, 1024])
        P.dma("sp", g2a[:], D["rwkv_g2"][l, 0:128, :], writes=[C.wres])
        P.dma("sp", g2b[:], D["rwkv_g2"][l, 128:160, :], writes=[C.wres])
        mixT = Tl(P, "mixT", [128, 6, 8])
        P.dma("sp", mixT[:], D["rwkv_mix"][l].rearrange("j (k p) -> p j k", p=128), writes=[C.wres],
              allow_slow_non_contiguous=True)
        kk_t = load_bcast(P, "k_k", D["rwkv_k_k"][l], 1024, "act")
        ka_t = load_bcast(P, "k_a", D["rwkv_k_a"][l], 1024, "sp")
        rk_t = load_bcast(P, "r_k", D["rwkv_r_k"][l].rearrange("h n -> (h n)"), 1024, "sp")
        WR = [C.wres, kk_t, ka_t, rk_t]

        xt = Tl(P, "xt", [128, 1024]); xp = Tl(P, "xp", [128, 1024])
        xT = Tl(P, "xT", [128, 8, 128]); xxT = Tl(P, "xxT", [128, 8, 128])
        tmpA = Tl(P, "tmpA", [128, 8, 128]); tmpB = Tl(P, "tmpB", [128, 8, 128])
        xm = [Tl(P, f"xm{j}", [128, 8, 128], BF16) for j in range(6)]
        r_t = Tl(P, "r", [128, 1024]); k_t = Tl(P, "k", [128, 1024]); v_t = Tl(P, "v", [128, 1024])
        sg_t = Tl(P, "sg", [128, 1024]); a_t = Tl(P, "a", [128, 1024]); g_t = Tl(P, "g", [128, 1024])
        kn_t = Tl(P, "kn", [128, 1024]); kp_t = Tl(P, "kp", [128, 1024]); t1 = Tl(P, "t1", [128, 1024])
        t2 = Tl(P, "t2", [128, 1024]); vf_t = Tl(P, "vf", [128, 1024])
        l1w = Tl(P, "l1w", [65, 128]); l1a = Tl(P, "l1a", [65, 128]); l1v = Tl(P, "l1v", [65, 128])
        l1ga = Tl(P, "l1ga", [128, 128]); l1gb = Tl(P, "l1gb", [32, 128])
        s16 = Tl(P, "s16", [128, 16]); s16b = Tl(P, "s16b", [128, 16])
        P.op("dve", lambda e: e.memset(l1w[:], 1.0), writes=[l1w])
        P.op("dve", lambda e: e.memset(l1a[:], 1.0), writes=[l1a])
        P.op("dve", lambda e: e.memset(l1v[:], 0.0), writes=[l1v])
        P.op("dve", lambda e: e.memset(l1v[64:65, :], 1.0), [l1v], [l1v])

        def v3(t):
            return t[:].rearrange("p (h k) -> p h k", k=64)

        for c in range(NCH):
            for b in range(2):
                P.dma("sp", xt[b * 64:(b + 1) * 64, :], x_in[b, c * 64:(c + 1) * 64, :], writes=[xt])
            if c == 0:
                P.op("pool", lambda e: e.memset(xp[:], 0.0), writes=[xp])
                for b in range(2):
                    P.dma("act", xp[b * 64 + 1:(b + 1) * 64, :], x_in[b, 0:63, :], writes=[xp])
            else:
                for b in range(2):
                    P.dma("act", xp[b * 64:(b + 1) * 64, :], x_in[b, c * 64 - 1:c * 64 + 63, :], writes=[xp])
            if l > 0:
                P.dma("sp", vf_t[:], scr["vf"][c], writes=[vf_t])
            bA, bB = bank(C), bank(C)
            for kc in range(8):
                bk = bA if kc < 4 else bB
                P.tr(bk[:, (kc % 4) * 128:(kc % 4 + 1) * 128], xt[:, kc * 128:(kc + 1) * 128], C.ident[:], [xt, C.ident], [bk])
            copy_op(P, "act", xT[:, 0:4, :], bA[:].rearrange("p (k n) -> p k n", n=128), [bA], [xT])
            copy_op(P, "dve", xT[:, 4:8, :], bB[:].rearrange("p (k n) -> p k n", n=128), [bB], [xT])
            bA, bB = bank(C), bank(C)
            for kc in range(8):
                bk = bA if kc < 4 else bB
                P.tr(bk[:, (kc % 4) * 128:(kc % 4 + 1) * 128], xp[:, kc * 128:(kc + 1) * 128], C.ident[:], [xp, C.ident], [bk])
            tt(P, "dve", xxT[:, 0:4, :], bA[:].rearrange("p (k n) -> p k n", n=128), xT[:, 0:4, :], ALU.subtract, [bA, xT], [xxT])
            tt(P, "dve", xxT[:, 4:8, :], bB[:].rearrange("p (k n) -> p k n", n=128), xT[:, 4:8, :], ALU.subtract, [bB, xT], [xxT])
            for j in range(6):
                eng = "dve" if j % 2 == 0 else "pool"
                tmp = tmpA if j % 2 == 0 else tmpB
                mb = mixT[:, j, :].unsqueeze(2).broadcast_to([128, 8, 128])
                tt(P, eng, tmp[:], xxT[:], mb, ALU.mult, [xxT, C.wres], [tmp])
                tt(P, eng, xm[j][:], tmp[:], xT[:], ALU.add, [tmp, xT], [xm[j]])
            for j, dst in enumerate((r_t, k_t, v_t)):
                for hf in range(2):
                    bk = bank(C)
                    for kc in range(8):
                        P.mm(bk[:], xm[j][:, kc, :], wr[:, j * 8 + kc, hf * 512:(hf + 1) * 512], start=(kc == 0), stop=(kc == 7),
                             reads=[xm[j], C.wres], writes=[bk])
                    copy_op(P, ev_eng(C), dst[:, hf * 512:(hf + 1) * 512], bk[:], [bk], [dst])
            bk = bank(C)
            for kc in range(8):
                P.mm(bk[0:64, 0:128], w1[:, kc, :], xm[3][:, kc, :], start=(kc == 0), stop=(kc == 7), reads=[xm[3], C.wres], writes=[bk])
            for kc in range(8):
                P.mm(bk[0:64, 128:256], a1[:, kc, :], xm[4][:, kc, :], start=(kc == 0), stop=(kc == 7), reads=[xm[4], C.wres], writes=[bk])
            if l > 0:
                for kc in range(8):
                    P.mm(bk[0:32, 256:384], v1[:, kc, :], xm[2][:, kc, :], start=(kc == 0), stop=(kc == 7), reads=[xm[2], C.wres], writes=[bk])
            P.op("act", lambda e, bk=bk: e.activation(l1w[0:64, :], bk[0:64, 0:128], AF.Tanh), [bk], [l1w])
            copy_op(P, "act", l1a[0:64, :], bk[0:64, 128:256], [bk], [l1a])
            if l > 0:
                copy_op(P, "act", l1v[0:32, :], bk[0:32, 256:384], [bk], [l1v])
            bk2 = bank(C)
            for kc in range(8):
                P.mm(bk2[:, 0:128], g1[:, kc, 0:128], xm[5][:, kc, :], start=(kc == 0), stop=(kc == 7), reads=[xm[5], C.wres], writes=[bk2])
            for kc in range(8):
                P.mm(bk2[0:32, 128:256], g1[:, kc, 128:160], xm[5][:, kc, :], start=(kc == 0), stop=(kc == 7), reads=[xm[5], C.wres], writes=[bk2])
            P.op("act", lambda e, bk2=bk2: e.activation(l1ga[:], bk2[:, 0:128], AF.Sigmoid), [bk2], [l1ga])
            P.op("act", lambda e, bk2=bk2: e.activation(l1gb[:], bk2[0:32, 128:256], AF.Sigmoid), [bk2], [l1gb])
            for hf in range(2):
                cs = slice(hf * 512, (hf + 1) * 512)
                bk = bank(C)
                P.mm(bk[:], l1w[:], w2[:, cs], reads=[l1w, C.wres], writes=[bk])
                P.op("act", lambda e, bk=bk, cs=cs: e.activation(sg_t[:, cs], bk[:], AF.Sigmoid), [bk], [sg_t])
                bk = bank(C)
                P.mm(bk[:], l1a[:], a2[:, cs], reads=[l1a, C.wres], writes=[bk])
                P.op("act", lambda e, bk=bk, cs=cs: e.activation(a_t[:, cs], bk[:], AF.Sigmoid), [bk], [a_t])
                bk = bank(C)
                P.mm(bk[:], l1ga[:], g2a[:, cs], start=True, stop=False, reads=[l1ga, C.wres], writes=[bk])
                P.mm(bk[:], l1gb[:], g2b[:, cs], start=False, stop=True, reads=[l1gb, C.wres], writes=[bk])
                copy_op(P, "dve", g_t[:, cs], bk[:], [bk], [g_t])
                if l > 0:
                    bk = bank(C)
                    P.mm(bk[:], l1v[:], v2[:, cs], reads=[l1v, C.wres], writes=[bk])
                    P.op("act", lambda e, bk=bk, cs=cs: e.activation(t1[:, cs], bk[:], AF.Sigmoid), [bk], [t1])
            if l > 0:
                tt(P, "dve", t2[:], vf_t[:], v_t[:], ALU.subtract, [vf_t, v_t], [t2])
                tt(P, "dve", t2[:], t2[:], t1[:], ALU.mult, [t2, t1], [t2])
                tt(P, "dve", v_t[:], v_t[:], t2[:], ALU.add, [v_t, t2], [v_t])
            else:
                P.dma("sp", scr["vf"][c], v_t[:], reads=[v_t])
            tt(P, "pool", kn_t[:], k_t[:], kk_t[:], ALU.mult, [k_t, kk_t], [kn_t])
            tt(P, "pool", t2[:], kn_t[:], kn_t[:], ALU.mult, [kn_t], [t2])
            P.op("dve", lambda e: e.tensor_reduce(s16[:], v3(t2), AX.X, ALU.add), [t2], [s16])
            rsqrt_op(P, s16, 0, 16, 1e-24, True)
            tt(P, "dve", v3(kn_t), v3(kn_t), s16[:].unsqueeze(2).broadcast_to([128, 16, 64]), ALU.mult, [kn_t, s16], [kn_t])
            stt(P, "dve", t2[:], a_t[:], -1.0, ka_t[:], ALU.add, ALU.mult, [a_t, ka_t], [t2])
            stt(P, "dve", kp_t[:], t2[:], 1.0, k_t[:], ALU.add, ALU.mult, [t2, k_t], [kp_t])
            tt(P, "pool", a_t[:], kn_t[:], a_t[:], ALU.mult, [kn_t, a_t], [a_t])
            tt(P, "dve", t2[:], r_t[:], kp_t[:], ALU.mult, [r_t, kp_t], [t2])
            tt(P, "pool", t2[:], t2[:], rk_t[:], ALU.mult, [t2, rk_t], [t2])
            P.op("dve", lambda e: e.tensor_reduce(s16b[:], v3(t2), AX.X, ALU.add), [t2], [s16b])
            tt(P, "dve", v3(t2), v3(v_t), s16b[:].unsqueeze(2).broadcast_to([128, 16, 64]), ALU.mult, [v_t, s16b], [t2])
            for nm, tl in (("r", r_t), ("kp", kp_t), ("v", v_t), ("sg", sg_t), ("kn", kn_t), ("kka", a_t), ("bonus", t2), ("g", g_t)):
                P.dma(C.dq(), scr[nm][c], tl[:], reads=[tl])


def rwkv_pass2(P, D, l, S, x_in, x_out, scr):
    NCH = S // 64
    import os as _os
    CUT = float(_os.environ.get('CUT', '99'))
    with P.scope():
        C = setup_common(P)
        dq_factory(C)
        C.wres = P.res("weights")
        stage = Tl(P, "wstage", [128, 1024])
        wo = Tl(P, "wo", [128, 8, 1024], BF16)
        load_w_bf16(C, wo.t, D["rwkv_w_o"][l], 1024, 1024, stage)
        lnxg = load_bcast(P, "lnxg", D["rwkv_lnx_g"][l], 1024, "sp")
        lnxb = load_bcast(P, "lnxb", D["rwkv_lnx_b"][l], 1024, "act")
        lng = load_bcast(P, "lng", D["ln_g"][l, 0], 1024, "sp")
        lnb = load_bcast(P, "lnb", D["ln_b"][l, 0], 1024, "sp")
        iu = Tl(P, "iu", [128, 128]); su = Tl(P, "su", [128, 128]); sl = Tl(P, "sl", [128, 128])
        triI = Tl(P, "triI", [128, 128]); triS = Tl(P, "triS", [128, 128]); bsel = Tl(P, "bsel", [128, 32])
        for m, cmpop, pat, cm in ((iu, ALU.is_ge, 1, -1), (su, ALU.is_gt, 1, -1), (sl, ALU.is_gt, -1, 1)):
            P.op("pool", lambda e, m=m: e.memset(m[:], 1.0), writes=[m])
            P.op("pool", lambda e, m=m, cmpop=cmpop, pat=pat, cm=cm: e.affine_select(
                m[:], m[:], [[pat, 128]], cmpop, 0.0, base=0, channel_multiplier=cm), [m], [m])
        P.op("pool", lambda e: e.memset(iu[0:64, 64:128], 0.0), [iu], [iu])
        P.op("pool", lambda e: e.memset(su[0:64, 64:128], 0.0), [su], [su])
        P.op("pool", lambda e: e.memset(sl[64:128, 0:64], 0.0), [sl], [sl])
        P.op("dve", lambda e: e.tensor_scalar(triI[:], iu[:], -EXPM05, None, ALU.mult), [iu], [triI])
        P.op("dve", lambda e: e.tensor_scalar(triS[:], su[:], -EXPM05, None, ALU.mult), [su], [triS])
        P.op("pool", lambda e: e.memset(bsel[:], 0.0), writes=[bsel])
        P.op("pool", lambda e: e.memset(bsel[0:64, 0:16], -EXPM05), [bsel], [bsel])
        P.op("pool", lambda e: e.memset(bsel[64:128, 16:32], -EXPM05), [bsel], [bsel])
        ST = Tl(P, "ST", [64, 16, 128])
        P.op("dve", lambda e: e.memset(ST[:], 0.0), writes=[ST])
        Vblk = Tl(P, "Vblk", [128, 16, 128]); Ublk = Tl(P, "Ublk", [128, 16, 128])
        P.op("pool", lambda e: e.memset(Vblk[:], 0.0), writes=[Vblk])
        P.op("pool", lambda e: e.memset(Ublk[:], 0.0), writes=[Ublk])
        r_t = Tl(P, "r", [128, 1024]); kp_t = Tl(P, "kp", [128, 1024]); v_t = Tl(P, "v", [128, 1024])
        sg_t = Tl(P, "sg", [128, 1024]); kn_t = Tl(P, "kn", [128, 1024]); kka_t = Tl(P, "kka", [128, 1024])
        Ep = Tl(P, "Ep", [128, 1024]); Em = Tl(P, "Em", [128, 1024]); Epp = Tl(P, "Epp", [128, 1024])
        bon_t = Ep; g_t = Epp; xt = sg_t
        C.junk = Em
        Y = Tl(P, "Y", [128, 1024]); Rh = Tl(P, "Rh", [128, 1024]); t2 = Tl(P, "t2", [128, 1024])
        oT = Tl(P, "oT", [128, 8, 128], BF16)
        gamT = Tl(P, "gamT", [64, 16, 2])
        st = Tl(P, "st", [128, 8]); s16 = Tl(P, "s16", [128, 16]); s16b = Tl(P, "s16b", [128, 16])
        G = []
        for gi in range(4):
            g = Ctx()
            g.qT = Tl(P, f"qT{gi}", [64, 16, 128])
            g.Q = Tl(P, f"Q{gi}", [128, 4, 128]); g.QT = Tl(P, f"QT{gi}", [128, 4, 128])
            g.Aak = Tl(P, f"Aak{gi}", [128, 4, 128]); g.Arb = Tl(P, f"Arb{gi}", [128, 4, 128]); g.Ark = Tl(P, f"Ark{gi}", [128, 4, 128])
            g.X = Tl(P, f"X{gi}", [128, 4, 128])
            g.WT = g.qT
            G.append(g)

        def v3(t):
            return t[:].rearrange("p (h k) -> p h k", k=64)

        def b4(bk):
            return bk[:].rearrange("p (h n) -> p h n", n=128)

        for c in range(NCH):
            for nm, tl in (("r", r_t), ("kp", kp_t), ("v", v_t), ("sg", sg_t), ("kn", kn_t), ("kka", kka_t)):
                P.dma(C.dq(), tl[:], scr[nm][c], writes=[tl])
            for hf in range(2):
                cs = slice(hf * 512, (hf + 1) * 512)
                bk = bank(C)
                P.mm(bk[:], triI[:], sg_t[:, cs], reads=[triI, sg_t], writes=[bk])
                P.op("act", lambda e, bk=bk, cs=cs: e.activation(Ep[:, cs], bk[:], AF.Exp), [bk], [Ep])
                P.op("act", lambda e, bk=bk, cs=cs: e.activation(Em[:, cs], bk[:], AF.Exp, scale=-1.0), [bk], [Em])
                bk = bank(C)
                P.mm(bk[:], triS[:], sg_t[:, cs], reads=[triS, sg_t], writes=[bk])
                P.op("act", lambda e, bk=bk, cs=cs: e.activation(Epp[:, cs], bk[:], AF.Exp), [bk], [Epp])
            bk = bank(C)
            for h in range(16):
                P.mm(bk[0:64, h * 32:(h + 1) * 32], sg_t[:, h * 64:(h + 1) * 64], bsel[:], reads=[sg_t, bsel], writes=[bk])
            P.op("act", lambda e, bk=bk: e.activation(gamT[:], bk[0:64, :].rearrange("p (h b r) -> p h b r", b=2, r=16)[:, :, :, 0], AF.Exp), [bk], [gamT])
            stt(P, "dve", kn_t[:], kn_t[:], -1.0, Epp[:], ALU.mult, ALU.mult, [kn_t, Epp], [kn_t])
            tt(P, "pool", kka_t[:], kka_t[:], Em[:], ALU.mult, [kka_t, Em], [kka_t])
            tt(P, "dve", kp_t[:], kp_t[:], Em[:], ALU.mult, [kp_t, Em], [kp_t])
            tt(P, "pool", r_t[:], r_t[:], Ep[:], ALU.mult, [r_t, Ep], [r_t])
            copy_op(P, "pool", Vblk[0:64, :, 0:64], v3(v_t)[0:64], [v_t], [Vblk])
            copy_op(P, "pool", Vblk[64:128, :, 64:128], v3(v_t)[64:128], [v_t], [Vblk])
            quant = (kn_t, kka_t, kp_t, r_t)
            P.dma("sp", bon_t[:], scr["bonus"][c], writes=[bon_t])
            P.dma("act", g_t[:], scr["g"][c], writes=[g_t])
            for b in range(2):
                P.dma("sp", xt[b * 64:(b + 1) * 64, :], x_in[b, c * 64:(c + 1) * 64, :], writes=[xt])
            if CUT <= 1:
                continue
            for gi, g in enumerate(G):
                for q in range(4):
                    bk = bank(C)
                    for h4 in range(4):
                        h = gi * 4 + h4
                        P.tr(bk[0:64, h4 * 128:(h4 + 1) * 128], quant[q][:, h * 64:(h + 1) * 64], C.ident[:], [quant[q], C.ident], [bk])
                    copy_op(P, ev_eng(C), g.qT[:, q * 4:(q + 1) * 4, :], b4(bk)[0:64], [bk], [g.qT])
            if CUT <= 2:
                continue
            for gi, g in enumerate(G):
                for (dst, lq, rq, msk) in ((g.QT, 1, 0, su), (g.Q, 0, 1, sl), (g.Aak, 2, 0, su), (g.Arb, 1, 3, iu), (g.Ark, 2, 3, iu)):
                    bk = bank(C)
                    for h4 in range(4):
                        P.mm(bk[:, h4 * 128:(h4 + 1) * 128], g.qT[:, lq * 4 + h4, :], g.qT[:, rq * 4 + h4, :], reads=[g.qT], writes=[bk])
                    tt(P, "dve", dst[:], b4(bk), msk[:].unsqueeze(1).broadcast_to([128, 4, 128]), ALU.mult, [bk, msk], [dst])
            if CUT <= 3:
                continue
            for gi, g in enumerate(G):
                bk = bank(C)
                for h4 in range(4):
                    h = gi * 4 + h4
                    P.mm(bk[:, h4 * 128 + 64:(h4 + 1) * 128], g.Aak[:, h4, :], v_t[:, h * 64:(h + 1) * 64], reads=[g.Aak, v_t], writes=[bk])
                copy_op(P, ev_eng(C), g.X[:, :, 64:128], b4(bk)[:, :, 64:128], [bk], [g.X])
                copy_op(P, "pool", g.X[:, :, 0:64], v3(kn_t)[:, gi * 4:(gi + 1) * 4, :], [kn_t], [g.X])
            if CUT <= 4:
                continue
            for j in range(6):
                for gi, g in enumerate(G):
                    bk = bank(C)
                    for h4 in range(4):
                        P.mm(bk[:, h4 * 128:(h4 + 1) * 128], g.QT[:, h4, :], g.X[:, h4, :], reads=[g.QT, g.X], writes=[bk])
                    if j < 5:
                        bq = bank(C); bqt = bank(C)
                        for h4 in range(4):
                            P.mm(bq[:, h4 * 128:(h4 + 1) * 128], g.QT[:, h4, :], g.Q[:, h4, :], reads=[g.QT, g.Q], writes=[bq])
                        for h4 in range(4):
                            P.mm(bqt[:, h4 * 128:(h4 + 1) * 128], g.Q[:, h4, :], g.QT[:, h4, :], reads=[g.QT, g.Q], writes=[bqt])
                    tt(P, "dve", g.X[:], b4(bk), g.X[:], ALU.add, [bk, g.X], [g.X])
                    if j < 5:
                        copy_op(P, "act", g.Q[:], b4(bq), [bq], [g.Q])
                        copy_op(P, ev_eng(C), g.QT[:], b4(bqt), [bqt], [g.QT])
            if CUT <= 5:
                continue
            for gi, g in enumerate(G):
                bk = bank(C)
                for h4 in range(4):
                    h = gi * 4 + h4
                    EV = _os.environ.get("EV", "0")
                    if EV in ("0", "1"):
                        P.mm(bk[:, h4 * 128:h4 * 128 + 64], g.Arb[:, h4, :], g.X[:, h4, 0:64], reads=[g.Arb, g.X], writes=[bk])
                    if EV in ("0", "2"):
                        P.mm(bk[:, h4 * 128 + 64:(h4 + 1) * 128], g.Arb[:, h4, :], g.X[:, h4, 64:128], start=True, stop=False, reads=[g.Arb, g.X], writes=[bk])
                        P.mm(bk[:, h4 * 128 + 64:(h4 + 1) * 128], g.Ark[:, h4, :], v_t[:, h * 64:(h + 1) * 64], start=False, stop=True, reads=[g.Ark, v_t], writes=[bk])
                    if EV == "3":
                        P.mm(bk[:, h4 * 128:(h4 + 1) * 128], g.Arb[:, h4, :], g.X[:, h4, :], reads=[g.Arb, g.X], writes=[bk])
                hs = slice(gi * 4, (gi + 1) * 4)
                EVC = _os.environ.get("EVC", "0")
                if EVC != "1":
                    tt(P, "dve", v3(Rh)[:, hs, :], b4(bk)[:, :, 0:64], v3(r_t)[:, hs, :], ALU.add, [bk, r_t], [Rh])
                if EVC != "2":
                    copy_op(P, "dve", v3(Y)[:, hs, :], b4(bk)[:, :, 64:128], [bk], [Y])
            if CUT <= 5.5:
                continue
            for gi, g in enumerate(G):
                bk = bank(C); bk2 = bank(C)
                for h4 in range(4):
                    h = gi * 4 + h4
                    P.tr(bk[0:64, h4 * 128:(h4 + 1) * 128], g.X[:, h4, 0:64], C.ident[:], [g.X, C.ident], [bk])
                    P.tr(bk2[0:64, h4 * 128:(h4 + 1) * 128], Rh[:, h * 64:(h + 1) * 64], C.ident[:], [Rh, C.ident], [bk2])
                copy_op(P, "act", g.WT[:, 0:4, :], b4(bk)[0:64], [bk], [g.WT])
                copy_op(P, "dve", g.WT[:, 4:8, :], b4(bk2)[0:64], [bk2], [g.WT])
            if CUT <= 6:
                continue
            for gi, g in enumerate(G):
                hs = slice(gi * 4, (gi + 1) * 4)
                bu = bank(C); by = bank(C)
                for h4 in range(4):
                    h = gi * 4 + h4
                    P.mm(bu[:, h4 * 128:(h4 + 1) * 128], g.WT[:, h4, :], ST[:, h, :], reads=[g.WT, ST], writes=[bu])
                for h4 in range(4):
                    h = gi * 4 + h4
                    P.mm(by[:, h4 * 128:(h4 + 1) * 128], g.WT[:, 4 + h4, :], ST[:, h, :], reads=[g.WT, ST], writes=[by])
                for b in range(2):
                    ps_ = slice(b * 64, (b + 1) * 64)
                    tt(P, "dve", Ublk[ps_, hs, b * 64:(b + 1) * 64], b4(bu)[ps_, :, b * 64:(b + 1) * 64], g.X[ps_, :, 64:128], ALU.add,
                       [bu, g.X], [Ublk])
                    tt(P, "dve", v3(Y)[ps_, hs, :], b4(by)[ps_, :, b * 64:(b + 1) * 64], v3(Y)[ps_, hs, :], ALU.add, [by, Y], [Y])
                bs = bank(C)
                for h4 in range(4):
                    h = gi * 4 + h4
                    P.mm(bs[0:64, h4 * 128:(h4 + 1) * 128], kka_t[:, h * 64:(h + 1) * 64], Ublk[:, h, :], start=True, stop=False,
                         reads=[kka_t, Ublk], writes=[bs])
                    P.mm(bs[0:64, h4 * 128:(h4 + 1) * 128], kp_t[:, h * 64:(h + 1) * 64], Vblk[:, h, :], start=False, stop=True,
                         reads=[kp_t, Vblk], writes=[bs])
                tt(P, "dve", ST[:, hs, :], b4(bs)[0:64], ST[:, hs, :], ALU.add, [bs, ST], [ST])
                tt(P, "dve", ST[:, hs, :].rearrange("p h (b v) -> p h b v", b=2),
                   ST[:, hs, :].rearrange("p h (b v) -> p h b v", b=2),
                   gamT[:, hs, :].unsqueeze(3).broadcast_to([64, 4, 2, 64]), ALU.mult, [ST, gamT], [ST])
            if CUT <= 7:
                continue
            P.op("dve", lambda e: e.tensor_reduce(s16[:], v3(Y), AX.X, ALU.add), [Y], [s16])
            tt(P, "pool", t2[:], Y[:], Y[:], ALU.mult, [Y], [t2])
            P.op("dve", lambda e: e.tensor_reduce(s16b[:], v3(t2), AX.X, ALU.add), [t2], [s16b])
            P.op("dve", lambda e: e.tensor_scalar(s16[:], s16[:], 1.0 / 64, None, ALU.mult), [s16], [s16])
            tt(P, "pool", t2[:, 0:16], s16[:], s16[:], ALU.mult, [s16], [t2])
            stt(P, "dve", s16b[:], s16b[:], 1.0 / 64, t2[:, 0:16], ALU.mult, ALU.subtract, [s16b, t2], [s16b])
            rsqrt_op(P, s16b, 0, 16, GN_EPS, False)
            tt(P, "dve", v3(Y), v3(Y), s16[:].unsqueeze(2).broadcast_to([128, 16, 64]), ALU.subtract, [Y, s16], [Y])
            tt(P, "dve", v3(Y), v3(Y), s16b[:].unsqueeze(2).broadcast_to([128, 16, 64]), ALU.mult, [Y, s16b], [Y])
            tt(P, "pool", Y[:], Y[:], lnxg[:], ALU.mult, [Y, lnxg], [Y])
            tt(P, "pool", Y[:], Y[:], lnxb[:], ALU.add, [Y, lnxb], [Y])
            tt(P, "dve", Y[:], Y[:], bon_t[:], ALU.add, [Y, bon_t], [Y])
            tt(P, "dve", Y[:], Y[:], g_t[:], ALU.mult, [Y, g_t], [Y])
            if CUT <= 8:
                continue
            bA, bB = bank(C), bank(C)
            for kc in range(8):
                bk = bA if kc < 4 else bB
                P.tr(bk[:, (kc % 4) * 128:(kc % 4 + 1) * 128], Y[:, kc * 128:(kc + 1) * 128], C.ident[:], [Y, C.ident], [bk])
            copy_op(P, "act", oT[:, 0:4, :], b4(bA), [bA], [oT])
            copy_op(P, "dve", oT[:, 4:8, :], b4(bB), [bB], [oT])
            for hf in range(2):
                cs = slice(hf * 512, (hf + 1) * 512)
                bk = bank(C)
                for kc in range(8):
                    P.mm(bk[:], oT[:, kc, :], wo[:, kc, cs], start=(kc == 0), stop=(kc == 7), reads=[oT, C.wres], writes=[bk])
                copy_op(P, "act", t2[:, cs], bk[:], [bk], [t2])
                stt(P, "dve", t2[:, cs], xt[:, cs], ALPHA, t2[:, cs], ALU.mult, ALU.add, [xt, t2], [t2])
            layer_norm_tile(C, t2, Rh, lng, lnb, st)
            for b in range(2):
                P.dma("act", x_out[b, c * 64:(c + 1) * 64, :], Rh[b * 64:(b + 1) * 64, :], reads=[Rh])


SW_LIMIT = 7.0
SW_ALPHA = 1.702


def moe_stage(P, D, l, S, x_in, x_out, NE=32):
    T = 2 * S
    TQ = min(1024, T)
    NQ = T // TQ
    NT = TQ // 128
    NB = TQ // 512
    xin = x_in.rearrange("b s d -> (b s) d")
    xout = x_out.rearrange("b s d -> (b s) d")
    wgu_d = D["moe_w_gu"][l]
    wdn_d = D["moe_w_dn"][l]
    with P.scope():
        C = setup_common(P)
        dq_factory(C)
        C.wres = P.res("weights")
        wr = Tl(P, "wr", [128, 8, 32])
        P.dma("sp", wr[:], D["moe_router_w"][l].rearrange("(k p) e -> p k e", p=128), writes=[C.wres])
        br = load_bcast(P, "br", D["moe_router_b"][l], 32, "act")
        bdn = Tl(P, "bdn", [32, 1024])
        P.dma("sp", bdn[:], D["moe_b_dn"][l], writes=[C.wres])
        xT32f = Tl(P, "xT32", [128, 1024])
        C.junk = xT32f
        bgu_raw = xT32f
        bguT = Tl(P, "bguT", [128, 16, 32])
        for half in range(2):
            P.dma("act", bgu_raw[0:32, :], D["moe_b_gu"][l][:, half * 1024:(half + 1) * 1024], writes=[bgu_raw])
            for cb in range(2):
                bk = bank(C)
                for c4 in range(4):
                    cc = cb * 4 + c4
                    P.tr(bk[:, c4 * 32:(c4 + 1) * 32], bgu_raw[0:32, cc * 128:(cc + 1) * 128], C.ident[0:32, 0:32], [bgu_raw, C.ident], [bk])
                copy_op(P, "dve", bguT[:, half * 8 + cb * 4:half * 8 + (cb + 1) * 4, :], bk[:, 0:128].rearrange("p (c e) -> p c e", e=32), [bk], [bguT])
        bgu7 = Tl(P, "bgu7", [128, 16, 32])
        P.op("dve", lambda e: e.tensor_scalar(bgu7[:], bguT[:], SW_LIMIT, None, ALU.add), [bguT], [bgu7])
        lng = load_bcast(P, "lng", D["ln_g"][l, 1], 1024, "sp")
        lnb = load_bcast(P, "lnb", D["ln_b"][l, 1], 1024, "act")
        xT = Tl(P, "xT", [128, 8, TQ], BF16)
        acc = Tl(P, "acc", [128, NT, 1024])
        G = Tl(P, "G", [128, NT, 32])
        Wgu = [Tl(P, f"Wgu{i}", [128, 8, 2048], BF16) for i in range(2)]
        Wdn = [Tl(P, f"Wdn{i}", [128, 8, 1024], BF16) for i in range(2)]
        actT = [Tl(P, f"actT{i}", [128, 8, 512], BF16) for i in range(2)]
        xt = Tl(P, "xt", [128, 1024]); z = Tl(P, "z", [128, 1024])
        xT32 = xT32f
        x3 = xT32f[:].rearrange("p (k n) -> p k n", n=128)
        lg = Tl(P, "lg", [128, 32]); t8 = Tl(P, "t8", [128, 8]); msk = Tl(P, "msk", [128, 32]); st = Tl(P, "st", [128, 8])
        GT = Tl(P, "GT", [32, 128])
        tmp = [[Tl(P, f"sw{i}_{j}", [128, 512]) for j in range(3)] for i in range(2)]

        def load_expert(e, buf):
            for k0 in range(0, 8, 2):
                P.dma("pool", Wgu[buf][:, k0:k0 + 2, :], wgu_d[e, k0 * 128:(k0 + 2) * 128, :].rearrange("(k p) n -> p k n", p=128), writes=[Wgu[buf]])
            for k0 in range(0, 8, 4):
                P.dma("pool", Wdn[buf][:, k0:k0 + 4, :], wdn_d[e, k0 * 128:(k0 + 4) * 128, :].rearrange("(k p) n -> p k n", p=128), writes=[Wdn[buf]])

        seq = [(q, e) for q in range(NQ) for e in range(NE)]
        load_expert(0, 0)
        si = 0
        for q in range(NQ):
            t0 = q * TQ
            for t in range(NT):
                rows = slice(t0 + t * 128, t0 + (t + 1) * 128)
                P.dma("sp", xt[:], xin[rows, :], writes=[xt])
                bA, bB = bank(C), bank(C)
                for kc in range(8):
                    bk = bA if kc < 4 else bB
                    P.tr(bk[:, (kc % 4) * 128:(kc % 4 + 1) * 128], xt[:, kc * 128:(kc + 1) * 128], C.ident[:], [xt, C.ident], [bk])
                copy_op(P, "act", x3[:, 0:4, :], bA[:].rearrange("p (k n) -> p k n", n=128), [bA], [xT32])
                copy_op(P, "dve", x3[:, 4:8, :], bB[:].rearrange("p (k n) -> p k n", n=128), [bB], [xT32])
                copy_op(P, "pool", xT[:, :, t * 128:(t + 1) * 128], x3, [xT32], [xT])
                bk = bank(C)
                for kc in range(8):
                    P.mm(bk[:, 0:32], x3[:, kc, :], wr[:, kc, :], start=(kc == 0), stop=(kc == 7), reads=[xT32, C.wres], writes=[bk])
                tt(P, "dve", lg[:], bk[:, 0:32], br[:], ALU.add, [bk, br], [lg])
                P.op("dve", lambda e: e.max(t8[:], lg[:]), [lg], [t8])
                P.op("dve", lambda e: e.tensor_scalar(msk[:], lg[:], t8[:, 3:4], None, ALU.is_ge), [lg, t8], [msk])
                P.op("dve", lambda e: e.tensor_scalar(st[:, 0:1], t8[:, 0:1], -1.0, None, ALU.mult), [t8], [st])
                P.op("act", lambda e: e.activation(lg[:], lg[:], AF.Exp, bias=st[:, 0:1]), [lg, st], [lg])
                tt(P, "dve", lg[:], lg[:], msk[:], ALU.mult, [lg, msk], [lg])
                P.op("dve", lambda e: e.tensor_reduce(st[:, 1:2], lg[:], AX.X, ALU.add), [lg], [st])
                P.op("dve", lambda e: e.reciprocal(st[:, 2:3], st[:, 1:2]), [st], [st])
                P.op("dve", lambda e, t=t: e.tensor_scalar(G[:, t, :], lg[:], st[:, 2:3], None, ALU.mult), [lg, st], [G])
            P.op("pool", lambda e: e.memset(acc[:], 0.0), writes=[acc])
            for e in range(NE):
                buf = si % 2
                import os as _os
                if si + 1 < len(seq) and not (_os.environ.get("MOE_NOLOAD") and si > 0):
                    load_expert(seq[si + 1][1], (si + 1) % 2)
                si += 1
                for blk in range(NB):
                    ts = slice(blk * 512, (blk + 1) * 512)
                    aT = actT[blk % 2]
                    for fc in range(8):
                        tp = tmp[fc % 2]
                        bg, bu = bank(C), bank(C)
                        for kc in range(8):
                            P.mm(bg[:], Wgu[buf][:, kc, fc * 128:(fc + 1) * 128], xT[:, kc, ts], start=(kc == 0), stop=(kc == 7),
                                 reads=[Wgu[buf], xT], writes=[bg])
                        for kc in range(8):
                            P.mm(bu[:], Wgu[buf][:, kc, 1024 + fc * 128:1024 + (fc + 1) * 128], xT[:, kc, ts], start=(kc == 0), stop=(kc == 7),
                                 reads=[Wgu[buf], xT], writes=[bu])
                        gp, sg, u1 = tp
                        P.op("dve", lambda en, bg=bg, gp=gp, fc=fc, e=e: en.tensor_scalar(gp[:], bg[:], bguT[:, fc, e:e + 1], SW_LIMIT, ALU.add, ALU.min),
                             [bg, bguT], [gp])
                        P.op("act", lambda en, gp=gp, sg=sg: en.activation(sg[:], gp[:], AF.Silu, scale=SW_ALPHA), [gp], [sg])
                        P.op("act", lambda en, bu=bu, u1=u1, fc=fc, e=e: en.activation(u1[:], bu[:], AF.Relu, bias=bgu7[:, 8 + fc, e:e + 1]),
                             [bu, bgu7], [u1])
                        P.op("dve", lambda en, u1=u1: en.tensor_scalar(u1[:], u1[:], 2.0 * SW_LIMIT, 1.0 - SW_LIMIT, ALU.min, ALU.add), [u1], [u1])
                        stt(P, "dve", aT[:, fc, :], sg[:], 1.0 / SW_ALPHA, u1[:], ALU.mult, ALU.mult, [sg, u1], [aT])
                for blk in range(NB):
                    aT = actT[blk % 2]
                    for t4 in range(4):
                        tile_i = blk * 4 + t4
                        for hf in range(2):
                            cs = slice(hf * 512, (hf + 1) * 512)
                            bk = bank(C)
                            for fc in range(8):
                                P.mm(bk[:], aT[:, fc, t4 * 128:(t4 + 1) * 128], Wdn[buf][:, fc, cs], start=(fc == 0), stop=(fc == 7),
                                     reads=[aT, Wdn[buf]], writes=[bk])
                            stt(P, "dve", acc[:, tile_i, cs], bk[:], G[:, tile_i, e:e + 1], acc[:, tile_i, cs], ALU.mult, ALU.add, [bk, G, acc], [acc])
            for t in range(NT):
                rows = slice(t0 + t * 128, t0 + (t + 1) * 128)
                bk = bank(C)
                P.tr(bk[0:32, 0:128], G[:, t, :], C.ident[:], [G, C.ident], [bk])
                copy_op(P, "act", GT[:], bk[0:32, 0:128], [bk], [GT])
                P.dma("sp", xt[:], xin[rows, :], writes=[xt])
                for hf in range(2):
                    cs = slice(hf * 512, (hf + 1) * 512)
                    bk = bank(C)
                    P.mm(bk[:], GT[:], bdn[:, cs], reads=[GT, C.wres], writes=[bk])
                    tt(P, "dve", z[:, cs], bk[:], acc[:, t, cs], ALU.add, [bk, acc], [z])
                stt(P, "dve", z[:], xt[:], ALPHA, z[:], ALU.mult, ALU.add, [xt, z], [z])
                layer_norm_tile(C, z, xt, lng, lnb, st)
                P.dma("act", xout[rows, :], xt[:], reads=[xt])


NEG = -1.0e30
SUBLN_EPS = 1e-5


def xT_block(C, xin, r0, nt, xt, xTb):
    P = C.P
    for t in range(nt):
        P.dma("sp", xt[:], xin[r0 + t * 128:r0 + (t + 1) * 128, :], writes=[xt])
        bA, bB = bank(C), bank(C)
        for kc in range(8):
            bk = bA if kc < 4 else bB
            P.tr(bk[:, (kc % 4) * 128:(kc % 4 + 1) * 128], xt[:, kc * 128:(kc + 1) * 128], C.ident[:], [xt, C.ident], [bk])
        copy_op(P, "act", xTb[:, 0:4, t * 128:(t + 1) * 128], bA[:].rearrange("p (k n) -> p k n", n=128), [bA], [xTb])
        copy_op(P, "dve", xTb[:, 4:8, t * 128:(t + 1) * 128], bB[:].rearrange("p (k n) -> p k n", n=128), [bB], [xTb])


def proj_stage(P, D, S, x_in, w_T_dram, T_scr, w_tok_dram=None, tok_scr=None):
    T = 2 * S
    NBLK = T // 512
    xin = x_in.rearrange("b s d -> (b s) d")
    with P.scope():
        C = setup_common(P)
        dq_factory(C)
        C.wres = P.res("weights")
        wT = Tl(P, "wT", [128, 8, 1024], BF16)
        for k0 in range(0, 8, 2):
            P.dma("pool", wT[:, k0:k0 + 2, :], w_T_dram[k0 * 128:(k0 + 2) * 128, :].rearrange("(k p) n -> p k n", p=128), writes=[C.wres])
        if w_tok_dram is not None:
            wK = Tl(P, "wK", [128, 8, 1024], BF16)
            for k0 in range(0, 8, 2):
                P.dma("pool", wK[:, k0:k0 + 2, :], w_tok_dram[k0 * 128:(k0 + 2) * 128, :].rearrange("(k p) n -> p k n", p=128), writes=[C.wres])
        xt = Tl(P, "xt", [128, 1024])
        xTb = [Tl(P, f"xTb{i}", [128, 8, 512], BF16) for i in range(2)]
        oT = [Tl(P, f"oT{i}", [64, 512]) for i in range(4)]
        ot = [Tl(P, f"ot{i}", [128, 1024]) for i in range(2)]
        for blk in range(NBLK):
            xb = xTb[blk % 2]
            xT_block(C, xin, blk * 512, 4, xt, xb)
            for g in range(16):
                bk = bank(C)
                for kc in range(8):
                    P.mm(bk[0:64, :], wT[:, kc, g * 64:(g + 1) * 64], xb[:, kc, :], start=(kc == 0), stop=(kc == 7), reads=[C.wres, xb], writes=[bk])
                o = oT[g % 4]
                copy_op(P, ev_eng(C), o[:], bk[0:64, :], [bk], [o])
                P.dma(C.dq(), T_scr[g, :, blk * 512:(blk + 1) * 512], o[:], reads=[o])
            if w_tok_dram is not None:
                for t4 in range(4):
                    o = ot[t4 % 2]
                    for hf in range(2):
                        cs = slice(hf * 512, (hf + 1) * 512)
                        bk = bank(C)
                        for kc in range(8):
                            P.mm(bk[:], xb[:, kc, t4 * 128:(t4 + 1) * 128], wK[:, kc, cs], start=(kc == 0), stop=(kc == 7), reads=[C.wres, xb], writes=[bk])
                        copy_op(P, ev_eng(C), o[:, cs], bk[:], [bk], [o])
                    P.dma(C.dq(), tok_scr[blk * 512 + t4 * 128:blk * 512 + (t4 + 1) * 128, :], o[:], reads=[o])


def attn_stage(P, D, j, S, qT_scr, kT_scr, v_scr, o_scr):
    import math
    l = NA + j
    lam_init = 0.8 - 0.6 * math.exp(-0.3 * l)
    NQB = S // 128
    with P.scope():
        C = setup_common(P)
        dq_factory(C)
        lamt = load_bcast(P, "lamt", D["da_lambda"][j].rearrange("a d -> (a d)"), 256, "sp")
        lsc = Tl(P, "lsc", [128, 8]); ljunk = Tl(P, "ljunk", [128, 64])
        tt(P, "dve", ljunk[:], lamt[:, 0:64], lamt[:, 64:128], ALU.mult, [lamt], [ljunk])
        P.op("dve", lambda e: e.tensor_reduce(lsc[:, 0:1], ljunk[:], AX.X, ALU.add), [ljunk], [lsc])
        tt(P, "dve", ljunk[:], lamt[:, 128:192], lamt[:, 192:256], ALU.mult, [lamt, ljunk], [ljunk])
        P.op("dve", lambda e: e.tensor_reduce(lsc[:, 1:2], ljunk[:], AX.X, ALU.add), [ljunk], [lsc])
        P.op("act", lambda e: e.activation(lsc[:, 2:4], lsc[:, 0:2], AF.Exp), [lsc], [lsc])
        tt(P, "dve", lsc[:, 4:5], lsc[:, 3:4], lsc[:, 2:3], ALU.subtract, [lsc], [lsc])
        P.op("dve", lambda e: e.tensor_scalar(lsc[:, 5:6], lsc[:, 4:5], -lam_init, None, ALU.add), [lsc], [lsc])
        gsc = load_bcast(P, "gsc", D["da_subln_g"][j], 128, "act")
        P.op("dve", lambda e: e.tensor_scalar(gsc[:], gsc[:], 1.0 - lam_init, None, ALU.mult), [gsc], [gsc])
        D0i = Tl(P, "D0i", [128, S], I32)
        D0f = Tl(P, "D0f", [128, S])
        P.op("pool", lambda e: e.iota(D0i[:], [[-1, S]], base=S - 128, channel_multiplier=1), writes=[D0i])
        copy_op(P, "dve", D0f[:], D0i[:], [D0i], [D0f])
        Bh = [Tl(P, f"Bh{i}", [128, S]) for i in range(2)]
        kT = [Tl(P, f"kT{i}", [64, 2, S]) for i in range(2)]
        qT = [Tl(P, f"qT{i}", [64, 2, S]) for i in range(2)]
        vv = [Tl(P, f"vv{i}", [128, NQB, 128]) for i in range(2)]
        tmp_all = [[Tl(P, f"tmp{p}_{i}", [128, S]) for i in range(2)] for p in range(2)]
        attnT_all = [Tl(P, f"attnT{p}", [128, NQB, 128]) for p in range(2)]
        sc_all = [Tl(P, f"sc{p}", [128, 16]) for p in range(2)]
        osb_all = [Tl(P, f"osb{p}", [128, 128]) for p in range(2)]
        oo = [Tl(P, f"oo{i}", [128, 128]) for i in range(2)]; ojunk = Tl(P, "ojunk", [128, 128])
        items = [(b, h, i) for b in range(2) for h in range(8) for i in range(NQB)]

        def head_setup(b, h, st_):
            for c in range(2):
                P.dma("sp", kT[st_][:, c, :], kT_scr[h * 2 + c, :, b * S:(b + 1) * S], writes=[kT[st_]])
                P.dma("act", qT[st_][:, c, :], qT_scr[h * 2 + c, :, b * S:(b + 1) * S], writes=[qT[st_]])
            P.dma("sp", vv[st_][:], v_scr[b * S:(b + 1) * S, h * 128:(h + 1) * 128].rearrange("(t p) d -> p t d", p=128), writes=[vv[st_]])
            slope = 2.0 ** (-(h + 1))
            P.op("act", lambda e, slope=slope, st_=st_: e.activation(Bh[st_][:], D0f[:], AF.Copy, scale=-slope), [D0f], [Bh[st_]])
            P.op("pool", lambda e, st_=st_: e.affine_select(Bh[st_][:], Bh[st_][:], [[-1, S]], ALU.is_ge, NEG, base=S - 128, channel_multiplier=1),
                 [Bh[st_]], [Bh[st_]])

        def stage_a(n):
            b, h, i = items[n]
            st_ = (b * 8 + h) % 2
            if i == 0:
                head_setup(b, h, st_)
            nk = (i + 1) * 128
            tmp = tmp_all[n % 2]; sc = sc_all[n % 2]
            for c in range(2):
                tc_ = tmp[c]
                for k0 in range(0, nk, 512):
                    kw = min(512, nk - k0)
                    bk = bank(C)
                    P.mm(bk[:, 0:kw], qT[st_][:, c, i * 128:(i + 1) * 128], kT[st_][:, c, k0:k0 + kw], reads=[qT[st_], kT[st_]], writes=[bk])
                    stt(P, "dve", tc_[:, k0:k0 + kw], bk[:, 0:kw], 0.125, Bh[st_][:, S - nk + k0:S - nk + k0 + kw], ALU.mult, ALU.add, [bk, Bh[st_]], [tc_])
                P.op("dve", lambda e, tc_=tc_, nk=nk, c=c, sc=sc: e.tensor_reduce(sc[:, c:c + 1], tc_[:, 0:nk], AX.X, ALU.max), [tc_], [sc])
                P.op("dve", lambda e, c=c, sc=sc: e.tensor_scalar(sc[:, 2 + c:3 + c], sc[:, c:c + 1], -1.0, None, ALU.mult), [sc], [sc])
                P.op("act", lambda e, tc_=tc_, nk=nk, c=c, sc=sc: e.activation(tc_[:, 0:nk], tc_[:, 0:nk], AF.Exp, bias=sc[:, 2 + c:3 + c],
                                                                            accum_out=sc[:, 4 + c:5 + c]), [tc_, sc], [tc_, sc])
            P.op("dve", lambda e, sc=sc: e.reciprocal(sc[:, 6:8], sc[:, 4:6]), [sc], [sc])
            tt(P, "dve", sc[:, 8:9], sc[:, 7:8], lsc[:, 5:6], ALU.mult, [sc, lsc], [sc])
            P.op("dve", lambda e, nk=nk, tmp=tmp, sc=sc: e.tensor_scalar(tmp[1][:, 0:nk], tmp[1][:, 0:nk], sc[:, 8:9], None, ALU.mult), [tmp[1], sc], [tmp[1]])
            stt(P, "dve", tmp[0][:, 0:nk], tmp[0][:, 0:nk], sc[:, 6:7], tmp[1][:, 0:nk], ALU.mult, ALU.add, [tmp[0], tmp[1], sc], [tmp[0]])

        def stage_b(n):
            b, h, i = items[n]
            st_ = (b * 8 + h) % 2
            tmp = tmp_all[n % 2]; attnT = attnT_all[n % 2]; sc = sc_all[n % 2]; osb = osb_all[n % 2]
            for k0 in range(0, i + 1, 4):
                k1 = min(i + 1, k0 + 4)
                bk = bank(C)
                for kt in range(k0, k1):
                    P.tr(bk[:, (kt - k0) * 128:(kt - k0 + 1) * 128], tmp[0][:, kt * 128:(kt + 1) * 128], C.ident[:], [tmp[0], C.ident], [bk])
                copy_op(P, ev_eng(C), attnT[:, k0:k1, :], bk[:, 0:(k1 - k0) * 128].rearrange("p (k n) -> p k n", n=128), [bk], [attnT])
            bk = bank(C)
            for kt in range(i + 1):
                P.mm(bk[:, 0:128], attnT[:, kt, :], vv[st_][:, kt, :], start=(kt == 0), stop=(kt == i), reads=[attnT, vv[st_]], writes=[bk])
            copy_op(P, "dve", osb[:], bk[:, 0:128], [bk], [osb])
            P.op("act", lambda e, osb=osb, sc=sc: e.activation(ojunk[:], osb[:], AF.Square, accum_out=sc[:, 9:10]), [osb], [ojunk, sc])
            P.op("dve", lambda e, sc=sc: e.tensor_scalar(sc[:, 10:11], sc[:, 9:10], 1.0 / 128, SUBLN_EPS, ALU.mult, ALU.add), [sc], [sc])
            P.op("act", lambda e, sc=sc: e.activation(sc[:, 10:11], sc[:, 10:11], AF.Sqrt), [sc], [sc])
            P.op("dve", lambda e, sc=sc: e.reciprocal(sc[:, 10:11], sc[:, 10:11]), [sc], [sc])
            o = oo[n % 2]
            stt(P, "dve", o[:], osb[:], sc[:, 10:11], gsc[:], ALU.mult, ALU.mult, [osb, sc, gsc], [o])
            P.dma(C.dq(), o_scr[b * S + i * 128:b * S + (i + 1) * 128, h * 128:(h + 1) * 128], o[:], reads=[o])

        for n in range(len(items) + 1):
            if n < len(items):
                stage_a(n)
            if n >= 1:
                stage_b(n - 1)


def outproj_ln_stage(P, D, S, o_scr, w_dram, x_in, x_out, lng_ap, lnb_ap):
    T = 2 * S
    xin = x_in.rearrange("b s d -> (b s) d")
    xout = x_out.rearrange("b s d -> (b s) d")
    with P.scope():
        C = setup_common(P)
        dq_factory(C)
        C.wres = P.res("weights")
        C.junk = Tl(P, "junk", [128, 1024])
        wo = Tl(P, "wo", [128, 8, 1024], BF16)
        for k0 in range(0, 8, 2):
            P.dma("pool", wo[:, k0:k0 + 2, :], w_dram[k0 * 128:(k0 + 2) * 128, :].rearrange("(k p) n -> p k n", p=128), writes=[C.wres])
        lng = load_bcast(P, "lng", lng_ap, 1024, "sp")
        lnb = load_bcast(P, "lnb", lnb_ap, 1024, "act")
        ot = Tl(P, "ot", [128, 1024]); oTb = Tl(P, "oTb", [128, 8, 128], BF16)
        xt = [Tl(P, f"xt{i}", [128, 1024]) for i in range(2)]; z = Tl(P, "z", [128, 1024]); st = Tl(P, "st", [128, 8])
        res = [Tl(P, f"res{i}", [128, 1024]) for i in range(2)]
        for t in range(T // 128):
            rows = slice(t * 128, (t + 1) * 128)
            xx = xt[t % 2]
            P.dma("act", xx[:], xin[rows, :], writes=[xx])
            xT_block(C, o_scr, t * 128, 1, ot, oTb)
            for hf in range(2):
                cs = slice(hf * 512, (hf + 1) * 512)
                bk = bank(C)
                for kc in range(8):
                    P.mm(bk[:], oTb[:, kc, :], wo[:, kc, cs], start=(kc == 0), stop=(kc == 7), reads=[oTb, C.wres], writes=[bk])
                copy_op(P, "act", z[:, cs], bk[:], [bk], [z])
            stt(P, "dve", z[:], xx[:], ALPHA, z[:], ALU.mult, ALU.add, [xx, z], [z])
            r = res[t % 2]
            layer_norm_tile(C, z, r, lng, lnb, st)
            P.dma("sp", xout[rows, :], r[:], reads=[r])


INPUT_SHAPES = {
    "ln_g": [4, 2, 1024], "ln_b": [4, 2, 1024], "rwkv_mix": [2, 6, 1024], "rwkv_w_rkv": [2, 3, 1024, 1024],
    "rwkv_w_o": [2, 1024, 1024], "rwkv_w0": [2, 1024], "rwkv_w1": [2, 1024, 64], "rwkv_w2": [2, 64, 1024],
    "rwkv_a0": [2, 1024], "rwkv_a1": [2, 1024, 64], "rwkv_a2": [2, 64, 1024], "rwkv_g1": [2, 1024, 160],
    "rwkv_g2": [2, 160, 1024], "rwkv_k_k": [2, 1024], "rwkv_k_a": [2, 1024], "rwkv_r_k": [2, 16, 64],
    "rwkv_lnx_g": [2, 1024], "rwkv_lnx_b": [2, 1024], "rwkv_v0": [1, 1024], "rwkv_v1": [1, 1024, 32],
    "rwkv_v2": [1, 32, 1024], "kv_w": [1024, 2048], "da_w_q": [2, 1024, 1024], "da_w_o": [2, 1024, 1024],
    "da_lambda": [2, 4, 64], "da_subln_g": [2, 128], "moe_router_w": [4, 1024, 32], "moe_router_b": [4, 32],
    "moe_w_gu": [4, 32, 1024, 2048], "moe_b_gu": [4, 32, 2048], "moe_w_dn": [4, 32, 1024, 1024], "moe_b_dn": [4, 32, 1024],
}
RWKV_KEYS = [k for k in INPUT_SHAPES if k.startswith("rwkv_")] + ["ln_g", "ln_b"]
SCR_NAMES = ("r", "kp", "v", "sg", "kn", "kka", "bonus", "g", "vf")


def build_program(S, plan, keys, shapes=None, ne=32):
    nc = bass.Bass("TRN2", target_bir_lowering=False)
    shapes = shapes or {}
    D = {k: nc.dram_tensor(k, shapes.get(k, INPUT_SHAPES[k]), F32, kind="ExternalInput").ap() for k in keys}
    x = nc.dram_tensor("x", [2, S, 1024], F32, kind="ExternalInput").ap()
    out = nc.dram_tensor("out", [2, S, 1024], F32, kind="ExternalOutput").ap()
    xa = nc.dram_tensor("xa", [2, S, 1024], F32).ap()
    xb = nc.dram_tensor("xb", [2, S, 1024], F32).ap()
    NCH = S // 64
    scr = {n: nc.dram_tensor("scr_" + n, [NCH, 128, 1024], F32).ap() for n in SCR_NAMES}
    ascr = {"kT": nc.dram_tensor("scr_kT", [16, 64, 2 * S], F32).ap(), "qT": nc.dram_tensor("scr_qT", [16, 64, 2 * S], F32).ap(),
            "v": nc.dram_tensor("scr_vsh", [2 * S, 1024], F32).ap(), "o": nc.dram_tensor("scr_o", [2 * S, 1024], F32).ap()}
    P = Prog(nc)
    bufs = {"x": x, "out": out, "xa": xa, "xb": xb}
    for stg in plan:
        kind = stg[0]
        if kind == "rwkv":
            _, l, src, dst = stg
            import os as _os
            if _os.environ.get("ONLY") != "2":
                rwkv_pass1(P, D, l, S, bufs[src], scr)
            if _os.environ.get("ONLY") != "1":
                rwkv_pass2(P, D, l, S, bufs[src], bufs[dst], scr)
        elif kind == "kvproj":
            _, src_ = stg
            proj_stage(P, D, S, bufs[src_], D["kv_w"][:, 0:1024], ascr["kT"], D["kv_w"][:, 1024:2048], ascr["v"])
        elif kind == "attn":
            _, j, src_, dst = stg
            proj_stage(P, D, S, bufs[src_], D["da_w_q"][j], ascr["qT"])
            attn_stage(P, D, j, S, ascr["qT"], ascr["kT"], ascr["v"], ascr["o"])
            outproj_ln_stage(P, D, S, ascr["o"], D["da_w_o"][j], bufs[src_], bufs[dst], D["ln_g"][NA + j, 0], D["ln_b"][NA + j, 0])
        elif kind == "moe":
            _, l, src_, dst = stg
            moe_stage(P, D, l, S, bufs[src_], bufs[dst], NE=ne)
        else:
            raise ValueError(kind)
    P.finish()
    return nc, P


FULL_PLAN = [("rwkv", 0, "x", "xa"), ("moe", 0, "xa", "xb"), ("rwkv", 1, "xb", "xa"), ("moe", 1, "xa", "xb"),
             ("kvproj", "xb"), ("attn", 0, "xb", "xa"), ("moe", 2, "xa", "xb"), ("attn", 1, "xb", "xa"), ("moe", 3, "xa", "out")]


def kernel(**inputs):
    from concourse.bass_utils import run_bass_kernel_spmd
    n = 8
    S = 2048
    x = np.ascontiguousarray(np.asarray(inputs["x"], dtype=np.float32))
    keys = list(INPUT_SHAPES.keys())
    nc, P = build_program(S, FULL_PLAN, keys)
    shared = {k: np.ascontiguousarray(np.asarray(inputs[k], dtype=np.float32)) for k in keys}
    in_maps = []
    for c in range(n):
        m = dict(shared)
        m["x"] = x[2 * c:2 * c + 2]
        in_maps.append(m)
    res = run_bass_kernel_spmd(nc, in_maps, core_ids=list(range(n)))
    return np.concatenate([r["out"] for r in res.results], axis=0).astype(np.float32)
```

```python
import contextlib
import numpy as np
import concourse.bass as bass
import concourse.mybir as mybir

F32 = mybir.dt.float32
BF16 = mybir.dt.bfloat16
I32 = mybir.dt.int32
ALU = mybir.AluOpType
AF = mybir.ActivationFunctionType
AX = mybir.AxisListType

N_DMA_SEMS = 40


class Res:
    __slots__ = ("name", "last_w", "readers")

    def __init__(self, name=""):
        self.name = name
        self.last_w = None
        self.readers = []


class Op:
    __slots__ = ("eng", "fn", "deps", "marked", "is_dma", "sem", "val", "idx")

    def __init__(self, eng, fn, is_dma):
        self.eng = eng
        self.fn = fn
        self.deps = []
        self.marked = False
        self.is_dma = is_dma
        self.sem = None
        self.val = 0


ENGS = ("pe", "dve", "act", "pool", "sp")


class Prog:
    def __init__(self, nc):
        self.nc = nc
        self.base = contextlib.ExitStack()
        self.csem = {e: self.base.enter_context(nc.semaphore(f"s_{e}")) for e in ENGS}
        self.dsem = [self.base.enter_context(nc.semaphore(f"d_{i}")) for i in range(N_DMA_SEMS)]
        self.ccount = {e: 0 for e in ENGS}
        self.dma_rr = 0
        self.dma_last = [None] * N_DMA_SEMS
        self.dma_cnt = [0] * N_DMA_SEMS
        self.nres = 0
        self.stack = None
        self.total = {e: 0 for e in ENGS}
        self._reset()

    def _reset(self):
        self.ops = {e: [] for e in ENGS}

    @contextlib.contextmanager
    def scope(self, final=False):
        self.stack = contextlib.ExitStack()
        try:
            yield self
            self.flush(final)
        finally:
            self.stack.close()
            self.stack = None

    def sb(self, name, shape, dtype=F32):
        self.nres += 1
        return self.stack.enter_context(self.nc.sbuf_tensor(f"{name}_{self.nres}", list(shape), dtype))

    def ps(self, name, shape, dtype=F32):
        self.nres += 1
        return self.stack.enter_context(self.nc.psum_tensor(f"{name}_{self.nres}", list(shape), dtype))

    def res(self, name=""):
        self.nres += 1
        return Res(name or f"r{self.nres}")

    def _add(self, eng, fn, reads, writes, is_dma):
        op = Op(eng, fn, is_dma)
        deps = []
        for r in reads:
            if r.last_w is not None:
                deps.append(r.last_w)
        for w in writes:
            if w.last_w is not None:
                deps.append(w.last_w)
            deps.extend(w.readers)
        seen = set()
        for d in deps:
            if id(d) in seen or d is op:
                continue
            seen.add(id(d))
            if (not d.is_dma) and (not is_dma) and d.eng == "pe" and eng == "pe":
                continue
            op.deps.append(d)
            d.marked = True
        if is_dma:
            k = self.dma_rr
            self.dma_rr = (self.dma_rr + 1) % N_DMA_SEMS
            prev = self.dma_last[k]
            if prev is not None and all(prev is not x for x in op.deps):
                op.deps.append(prev)
            self.dma_last[k] = op
            self.dma_cnt[k] += 16
            op.sem = k
            op.val = self.dma_cnt[k]
            op.marked = True
        for r in reads:
            if not is_dma:
                r.readers = [o for o in r.readers if o.is_dma or o.eng != eng]
            r.readers.append(op)
        for w in writes:
            w.last_w = op
            w.readers = []
        self.ops[eng].append(op)
        return op

    def op(self, eng, fn, reads=(), writes=()):
        return self._add(eng, fn, _rs(reads), _rs(writes), False)

    def dma(self, eng, out, in_, reads=(), writes=(), **kw):
        return self._add(eng, lambda e: e.dma_start(out=out, in_=in_, **kw),
                         _rs(reads), _rs(writes), True)

    def mm(self, out, lhsT, rhs, start=True, stop=True, reads=(), writes=()):
        return self.op("pe", lambda e: e.matmul(out, lhsT, rhs, start=start, stop=stop), reads, writes)

    def tr(self, out, in_, ident, reads=(), writes=()):
        return self.op("pe", lambda e: e.transpose(out, in_, ident), reads, writes)

    def flush(self, final=False):
        nc = self.nc
        csem, dsem = self.csem, self.dsem
        lastc = []
        for e in ENGS:
            comp = [o for o in self.ops[e] if not o.is_dma and o.fn is not None]
            if comp:
                comp[-1].marked = True
                lastc.append(comp[-1])
        lastd = [o for o in self.dma_last if o is not None]
        for e in ENGS:
            b = Op(e, None, False)
            b.deps = list(lastc) + list(lastd)
            self.ops[e].append(b)
        for e in ENGS:
            c = self.ccount[e]
            for op in self.ops[e]:
                if op.is_dma or op.fn is None:
                    continue
                if op.marked:
                    c += 1
                    op.val = c
            self.ccount[e] = c

        def semof(d):
            return (("d", d.sem), dsem[d.sem]) if d.is_dma else (("c", d.eng), csem[d.eng])

        def replay(ename):
            def body(e):
                known = {}
                for op in self.ops[ename]:
                    for d in op.deps:
                        key, sem = semof(d)
                        if known.get(key, 0) >= d.val:
                            continue
                        e.wait_ge(sem, d.val)
                        known[key] = d.val
                    if op.fn is None:
                        continue
                    ins = op.fn(e)
                    if op.is_dma:
                        ins.then_inc(dsem[op.sem], 16)
                    elif op.marked:
                        ins.then_inc(csem[ename], 1)
            return body

        with nc.Block() as block:
            block.sync(replay("sp"))
            block.scalar(replay("act"))
            block.vector(replay("dve"))
            block.gpsimd(replay("pool"))
            block.tensor(replay("pe"))
        for e in ENGS:
            self.total[e] += len(self.ops[e])
        self._reset()

    def finish(self):
        self.base.close()


class Tl:
    def __init__(self, P, name, shape, dtype=F32, psum=False):
        self.t = (P.ps if psum else P.sb)(name, shape, dtype)
        self.r = P.res(name)
        self.shape = shape

    def __getitem__(self, k):
        return self.t[k]


def _rs(xs):
    return [x.r if isinstance(x, Tl) else x for x in xs]


D_MODEL = 1024
DEPTH = 4
NA = 2
H_R = 16
EXPM05 = float(np.exp(-0.5))
ALPHA = (2 * DEPTH) ** 0.25
LN_EPS = 1e-5
GN_EPS = 64e-5


class Ctx:
    pass


def setup_common(P):
    C = Ctx()
    C.P = P
    C.ident = Tl(P, "ident", [128, 128])
    P.op("pool", lambda e: e.memset(C.ident[:], 0.0), writes=[C.ident])
    P.op("pool", lambda e: e.affine_select(C.ident[:], C.ident[:], [[-1, 128]], ALU.not_equal, 1.0,
                                           base=0, channel_multiplier=1), reads=[C.ident], writes=[C.ident])
    C.banks = [Tl(P, f"bank{i}", [128, 512], F32, psum=True) for i in range(8)]
    C.bi = 0
    C.rr = 0
    return C


def bank(C):
    b = C.banks[C.bi]
    C.bi = (C.bi + 1) % 8
    return b


def ev_eng(C):
    C.rr ^= 1
    return "act" if C.rr else "dve"


def copy_op(P, eng, out, in_, reads, writes):
    if eng == "act":
        P.op("act", lambda e: e.activation(out, in_, AF.Copy), reads, writes)
    else:
        P.op(eng, lambda e: e.tensor_copy(out, in_), reads, writes)


def tt(P, eng, out, in0, in1, op, reads, writes):
    P.op(eng, lambda e: e.tensor_tensor(out, in0, in1, op), reads, writes)


def stt(P, eng, out, in0, scalar, in1, op0, op1, reads, writes):
    P.op(eng, lambda e: e.scalar_tensor_tensor(out, in0, scalar, in1, op0, op1), reads, writes)


def rsqrt_op(P, t, lo, hi, bias, use_max):
    sl = t[:, lo:hi]
    op0 = ALU.max if use_max else ALU.add
    P.op("dve", lambda e: e.tensor_scalar(sl, sl, bias, None, op0), [t], [t])
    P.op("act", lambda e: e.activation(sl, sl, AF.Sqrt), [t], [t])
    P.op("dve", lambda e: e.reciprocal(sl, sl), [t], [t])


def load_bcast(P, name, src_1d, n, eng="sp"):
    t = Tl(P, name, [128, n])
    P.dma(eng, t[:], src_1d.partition_broadcast(128), writes=[t])
    return t


def layer_norm_tile(C, z, out, g_t, b_t, st):
    P = C.P
    junk = C.junk
    P.op("act", lambda e: e.activation(junk[:], z[:], AF.Copy, accum_out=st[:, 0:1]), [z], [junk, st])
    P.op("act", lambda e: e.activation(junk[:], z[:], AF.Square, accum_out=st[:, 1:2]), [z], [junk, st])
    P.op("dve", lambda e: e.tensor_scalar(st[:, 2:3], st[:, 0:1], 1.0 / 1024, None, ALU.mult), [st], [st])
    P.op("dve", lambda e: e.tensor_tensor(st[:, 3:4], st[:, 2:3], st[:, 2:3], ALU.mult), [st], [st])
    P.op("dve", lambda e: e.scalar_tensor_tensor(st[:, 4:5], st[:, 1:2], 1.0 / 1024, st[:, 3:4], ALU.mult, ALU.subtract), [st], [st])
    copy_op(P, "dve", st[:, 5:6], st[:, 4:5], [st], [st])
    rsqrt_op(P, st, 5, 6, LN_EPS, False)
    P.op("dve", lambda e: e.scalar_tensor_tensor(st[:, 6:7], st[:, 2:3], -1.0, st[:, 5:6], ALU.mult, ALU.mult), [st], [st])
    P.op("act", lambda e: e.activation(out[:], z[:], AF.Identity, bias=st[:, 6:7], scale=st[:, 5:6]), [z, st], [out])
    tt(P, "dve", out[:], out[:], g_t[:], ALU.mult, [out, g_t], [out])
    tt(P, "pool", out[:], out[:], b_t[:], ALU.add, [out, b_t], [out])


def load_w_bf16(C, dst, src2d, K, N, stage):
    P = C.P
    kcs = K // 128
    per = max(1, stage.shape[1] // N)
    for k0 in range(0, kcs, per):
        k1 = min(kcs, k0 + per)
        sv = stage[:, 0:(k1 - k0) * N].rearrange("p (k n) -> p k n", n=N)
        P.dma(C.dq(), sv, src2d[k0 * 128:k1 * 128, :].rearrange("(k p) n -> p k n", p=128), writes=[stage])
        copy_op(P, ev_eng(C), dst[:, k0:k1, :], sv, [stage], [C.wres])


def dq_factory(C):
    qs = ["sp", "act", "pool"]
    C.dqi = 0

    def dq():
        C.dqi = (C.dqi + 1) % 2
        return qs[C.dqi]
    C.dq = dq


def rwkv_pass1(P, D, l, S, x_in, scr):
    NCH = S // 64
    with P.scope():
        C = setup_common(P)
        dq_factory(C)
        C.wres = P.res("weights")
        C.junk = Tl(P, "junk", [128, 1024])
        stage = Tl(P, "wstage", [128, 2048])
        wr = Tl(P, "wrkv", [128, 3 * 8, 1024], BF16)
        for j in range(3):
            load_w_bf16(C, wr.t[:, j * 8:(j + 1) * 8, :], D["rwkv_w_rkv"][l, j], 1024, 1024, stage)
        w1 = Tl(P, "w1", [128, 8, 64], BF16)
        load_w_bf16(C, w1.t, D["rwkv_w1"][l], 1024, 64, stage)
        a1 = Tl(P, "a1", [128, 8, 64], BF16)
        load_w_bf16(C, a1.t, D["rwkv_a1"][l], 1024, 64, stage)
        g1 = Tl(P, "g1", [128, 8, 160], BF16)
        load_w_bf16(C, g1.t, D["rwkv_g1"][l], 1024, 160, stage)
        if l > 0:
            v1 = Tl(P, "v1", [128, 8, 32], BF16)
            load_w_bf16(C, v1.t, D["rwkv_v1"][l - 1], 1024, 32, stage)
            v2 = Tl(P, "v2", [65, 1024])
            P.op("dve", lambda e: e.memset(v2[:], 0.0), writes=[C.wres])
            P.dma("sp", v2[0:32, :], D["rwkv_v2"][l - 1], writes=[C.wres])
            P.dma("sp", v2[64:65, :], D["rwkv_v0"][l - 1:l, :], writes=[C.wres])
        w2 = Tl(P, "w2", [65, 1024])
        P.dma("sp", w2[0:64, :], D["rwkv_w2"][l], writes=[C.wres])
        P.dma("sp", w2[64:65, :], D["rwkv_w0"][l:l + 1, :], writes=[C.wres])
        a2 = Tl(P, "a2", [65, 1024])
        P.dma("act", a2[0:64, :], D["rwkv_a2"][l], writes=[C.wres])
        P.dma("act", a2[64:65, :], D["rwkv_a0"][l:l + 1, :], writes=[C.wres])
        g2a = Tl(P, "g2a", [128, 1024])
        g2b = Tl(P, "g2b", [32, 1024])
        P.dma("sp", g2a[:], D["rwkv_g2"][l, 0:128, :], writes=[C.wres])
        P.dma("sp", g2b[:], D["rwkv_g2"][l, 128:160, :], writes=[C.wres])
        mixT = Tl(P, "mixT", [128, 6, 8])
        P.dma("sp", mixT[:], D["rwkv_mix"][l].rearrange("j (k p) -> p j k", p=128), writes=[C.wres],
              allow_slow_non_contiguous=True)
        kk_t = load_bcast(P, "k_k", D["rwkv_k_k"][l], 1024, "act")
        ka_t = load_bcast(P, "k_a", D["rwkv_k_a"][l], 1024, "sp")
        rk_t = load_bcast(P, "r_k", D["rwkv_r_k"][l].rearrange("h n -> (h n)"), 1024, "sp")
        WR = [C.wres, kk_t, ka_t, rk_t]

        xt = Tl(P, "xt", [128, 1024]); xp = Tl(P, "xp", [128, 1024])
        xT = Tl(P, "xT", [128, 8, 128]); xxT = Tl(P, "xxT", [128, 8, 128])
        tmpA = Tl(P, "tmpA", [128, 8, 128]); tmpB = Tl(P, "tmpB", [128, 8, 128])
        xm = [Tl(P, f"xm{j}", [128, 8, 128], BF16) for j in range(6)]
        r_t = Tl(P, "r", [128, 1024]); k_t = Tl(P, "k", [128, 1024]); v_t = Tl(P, "v", [128, 1024])
        sg_t = Tl(P, "sg", [128, 1024]); a_t = Tl(P, "a", [128, 1024]); g_t = Tl(P, "g", [128, 1024])
        kn_t = Tl(P, "kn", [128, 1024]); kp_t = Tl(P, "kp", [128, 1024]); t1 = Tl(P, "t1", [128, 1024])
        t2 = Tl(P, "t2", [128, 1024]); vf_t = Tl(P, "vf", [128, 1024])
        l1w = Tl(P, "l1w", [65, 128]); l1a = Tl(P, "l1a", [65, 128]); l1v = Tl(P, "l1v", [65, 128])
        l1ga = Tl(P, "l1ga", [128, 128]); l1gb = Tl(P, "l1gb", [32, 128])
        s16 = Tl(P, "s16", [128, 16]); s16b = Tl(P, "s16b", [128, 16])
        P.op("dve", lambda e: e.memset(l1w[:], 1.0), writes=[l1w])
        P.op("dve", lambda e: e.memset(l1a[:], 1.0), writes=[l1a])
        P.op("dve", lambda e: e.memset(l1v[:], 0.0), writes=[l1v])
        P.op("dve", lambda e: e.memset(l1v[64:65, :], 1.0), [l1v], [l1v])

        def v3(t):
            return t[:].rearrange("p (h k) -> p h k", k=64)

        for c in range(NCH):
            for b in range(2):
                P.dma("sp", xt[b * 64:(b + 1) * 64, :], x_in[b, c * 64:(c + 1) * 64, :], writes=[xt])
            if c == 0:
                P.op("pool", lambda e: e.memset(xp[:], 0.0), writes=[xp])
                for b in range(2):
                    P.dma("act", xp[b * 64 + 1:(b + 1) * 64, :], x_in[b, 0:63, :], writes=[xp])
            else:
                for b in range(2):
                    P.dma("act", xp[b * 64:(b + 1) * 64, :], x_in[b, c * 64 - 1:c * 64 + 63, :], writes=[xp])
            if l > 0:
                P.dma("sp", vf_t[:], scr["vf"][c], writes=[vf_t])
            bA, bB = bank(C), bank(C)
            for kc in range(8):
                bk = bA if kc < 4 else bB
                P.tr(bk[:, (kc % 4) * 128:(kc % 4 + 1) * 128], xt[:, kc * 128:(kc + 1) * 128], C.ident[:], [xt, C.ident], [bk])
            copy_op(P, "act", xT[:, 0:4, :], bA[:].rearrange("p (k n) -> p k n", n=128), [bA], [xT])
            copy_op(P, "dve", xT[:, 4:8, :], bB[:].rearrange("p (k n) -> p k n", n=128), [bB], [xT])
            bA, bB = bank(C), bank(C)
            for kc in range(8):
                bk = bA if kc < 4 else bB
                P.tr(bk[:, (kc % 4) * 128:(kc % 4 + 1) * 128], xp[:, kc * 128:(kc + 1) * 128], C.ident[:], [xp, C.ident], [bk])
            tt(P, "dve", xxT[:, 0:4, :], bA[:].rearrange("p (k n) -> p k n", n=128), xT[:, 0:4, :], ALU.subtract, [bA, xT], [xxT])
            tt(P, "dve", xxT[:, 4:8, :], bB[:].rearrange("p (k n) -> p k n", n=128), xT[:, 4:8, :], ALU.subtract, [bB, xT], [xxT])
            for j in range(6):
                eng = "dve" if j % 2 == 0 else "pool"
                tmp = tmpA if j % 2 == 0 else tmpB
                mb = mixT[:, j, :].unsqueeze(2).broadcast_to([128, 8, 128])
                tt(P, eng, tmp[:], xxT[:], mb, ALU.mult, [xxT, C.wres], [tmp])
                tt(P, eng, xm[j][:], tmp[:], xT[:], ALU.add, [tmp, xT], [xm[j]])
            for j, dst in enumerate((r_t, k_t, v_t)):
                for hf in range(2):
                    bk = bank(C)
                    for kc in range(8):
                        P.mm(bk[:], xm[j][:, kc, :], wr[:, j * 8 + kc, hf * 512:(hf + 1) * 512], start=(kc == 0), stop=(kc == 7),
                             reads=[xm[j], C.wres], writes=[bk])
                    copy_op(P, ev_eng(C), dst[:, hf * 512:(hf + 1) * 512], bk[:], [bk], [dst])
            bk = bank(C)
            for kc in range(8):
                P.mm(bk[0:64, 0:128], w1[:, kc, :], xm[3][:, kc, :], start=(kc == 0), stop=(kc == 7), reads=[xm[3], C.wres], writes=[bk])
            for kc in range(8):
                P.mm(bk[0:64, 128:256], a1[:, kc, :], xm[4][:, kc, :], start=(kc == 0), stop=(kc == 7), reads=[xm[4], C.wres], writes=[bk])
            if l > 0:
                for kc in range(8):
                    P.mm(bk[0:32, 256:384], v1[:, kc, :], xm[2][:, kc, :], start=(kc == 0), stop=(kc == 7), reads=[xm[2], C.wres], writes=[bk])
            P.op("act", lambda e, bk=bk: e.activation(l1w[0:64, :], bk[0:64, 0:128], AF.Tanh), [bk], [l1w])
            copy_op(P, "act", l1a[0:64, :], bk[0:64, 128:256], [bk], [l1a])
            if l > 0:
                copy_op(P, "act", l1v[0:32, :], bk[0:32, 256:384], [bk], [l1v])
            bk2 = bank(C)
            for kc in range(8):
                P.mm(bk2[:, 0:128], g1[:, kc, 0:128], xm[5][:, kc, :], start=(kc == 0), stop=(kc == 7), reads=[xm[5], C.wres], writes=[bk2])
            for kc in range(8):
                P.mm(bk2[0:32, 128:256], g1[:, kc, 128:160], xm[5][:, kc, :], start=(kc == 0), stop=(kc == 7), reads=[xm[5], C.wres], writes=[bk2])
            P.op("act", lambda e, bk2=bk2: e.activation(l1ga[:], bk2[:, 0:128], AF.Sigmoid), [bk2], [l1ga])
            P.op("act", lambda e, bk2=bk2: e.activation(l1gb[:], bk2[0:32, 128:256], AF.Sigmoid), [bk2], [l1gb])
            for hf in range(2):
                cs = slice(hf * 512, (hf + 1) * 512)
                bk = bank(C)
                P.mm(bk[:], l1w[:], w2[:, cs], reads=[l1w, C.wres], writes=[bk])
                P.op("act", lambda e, bk=bk, cs=cs: e.activation(sg_t[:, cs], bk[:], AF.Sigmoid), [bk], [sg_t])
                bk = bank(C)
                P.mm(bk[:], l1a[:], a2[:, cs], reads=[l1a, C.wres], writes=[bk])
                P.op("act", lambda e, bk=bk, cs=cs: e.activation(a_t[:, cs], bk[:], AF.Sigmoid), [bk], [a_t])
                bk = bank(C)
                P.mm(bk[:], l1ga[:], g2a[:, cs], start=True, stop=False, reads=[l1ga, C.wres], writes=[bk])
                P.mm(bk[:], l1gb[:], g2b[:, cs], start=False, stop=True, reads=[l1gb, C.wres], writes=[bk])
                copy_op(P, "dve", g_t[:, cs], bk[:], [bk], [g_t])
                if l > 0:
                    bk = bank(C)
                    P.mm(bk[:], l1v[:], v2[:, cs], reads=[l1v, C.wres], writes=[bk])
                    P.op("act", lambda e, bk=bk, cs=cs: e.activation(t1[:, cs], bk[:], AF.Sigmoid), [bk], [t1])
            if l > 0:
                tt(P, "dve", t2[:], vf_t[:], v_t[:], ALU.subtract, [vf_t, v_t], [t2])
                tt(P, "dve", t2[:], t2[:], t1[:], ALU.mult, [t2, t1], [t2])
                tt(P, "dve", v_t[:], v_t[:], t2[:], ALU.add, [v_t, t2], [v_t])
            else:
                P.dma("sp", scr["vf"][c], v_t[:], reads=[v_t])
            tt(P, "pool", kn_t[:], k_t[:], kk_t[:], ALU.mult, [k_t, kk_t], [kn_t])
            tt(P, "pool", t2[:], kn_t[:], kn_t[:], ALU.mult, [kn_t], [t2])
            P.op("dve", lambda e: e.tensor_reduce(s16[:], v3(t2), AX.X, ALU.add), [t2], [s16])
            rsqrt_op(P, s16, 0, 16, 1e-24, True)
            tt(P, "dve", v3(kn_t), v3(kn_t), s16[:].unsqueeze(2).broadcast_to([128, 16, 64]), ALU.mult, [kn_t, s16], [kn_t])
            stt(P, "dve", t2[:], a_t[:], -1.0, ka_t[:], ALU.add, ALU.mult, [a_t, ka_t], [t2])
            stt(P, "dve", kp_t[:], t2[:], 1.0, k_t[:], ALU.add, ALU.mult, [t2, k_t], [kp_t])
            tt(P, "pool", a_t[:], kn_t[:], a_t[:], ALU.mult, [kn_t, a_t], [a_t])
            tt(P, "dve", t2[:], r_t[:], kp_t[:], ALU.mult, [r_t, kp_t], [t2])
            tt(P, "pool", t2[:], t2[:], rk_t[:], ALU.mult, [t2, rk_t], [t2])
            P.op("dve", lambda e: e.tensor_reduce(s16b[:], v3(t2), AX.X, ALU.add), [t2], [s16b])
            tt(P, "dve", v3(t2), v3(v_t), s16b[:].unsqueeze(2).broadcast_to([128, 16, 64]), ALU.mult, [v_t, s16b], [t2])
            for nm, tl in (("r", r_t), ("kp", kp_t), ("v", v_t), ("sg", sg_t), ("kn", kn_t), ("kka", a_t), ("bonus", t2), ("g", g_t)):
                P.dma(C.dq(), scr[nm][c], tl[:], reads=[tl])


def rwkv_pass2(P, D, l, S, x_in, x_out, scr):
    NCH = S // 64
    import os as _os
    CUT = float(_os.environ.get('CUT', '99'))
    with P.scope():
        C = setup_common(P)
        dq_factory(C)
        C.wres = P.res("weights")
        stage = Tl(P, "wstage", [128, 1024])
        wo = Tl(P, "wo", [128, 8, 1024], BF16)
        load_w_bf16(C, wo.t, D["rwkv_w_o"][l], 1024, 1024, stage)
        lnxg = load_bcast(P, "lnxg", D["rwkv_lnx_g"][l], 1024, "sp")
        lnxb = load_bcast(P, "lnxb", D["rwkv_lnx_b"][l], 1024, "act")
        lng = load_bcast(P, "lng", D["ln_g"][l, 0], 1024, "sp")
        lnb = load_bcast(P, "lnb", D["ln_b"][l, 0], 1024, "sp")
        iu = Tl(P, "iu", [128, 128]); su = Tl(P, "su", [128, 128]); sl = Tl(P, "sl", [128, 128])
        triI = Tl(P, "triI", [128, 128]); triS = Tl(P, "triS", [128, 128]); bsel = Tl(P, "bsel", [128, 32])
        for m, cmpop, pat, cm in ((iu, ALU.is_ge, 1, -1), (su, ALU.is_gt, 1, -1), (sl, ALU.is_gt, -1, 1)):
            P.op("pool", lambda e, m=m: e.memset(m[:], 1.0), writes=[m])
            P.op("pool", lambda e, m=m, cmpop=cmpop, pat=pat, cm=cm: e.affine_select(
                m[:], m[:], [[pat, 128]], cmpop, 0.0, base=0, channel_multiplier=cm), [m], [m])
        P.op("pool", lambda e: e.memset(iu[0:64, 64:128], 0.0), [iu], [iu])
        P.op("pool", lambda e: e.memset(su[0:64, 64:128], 0.0), [su], [su])
        P.op("pool", lambda e: e.memset(sl[64:128, 0:64], 0.0), [sl], [sl])
        P.op("dve", lambda e: e.tensor_scalar(triI[:], iu[:], -EXPM05, None, ALU.mult), [iu], [triI])
        P.op("dve", lambda e: e.tensor_scalar(triS[:], su[:], -EXPM05, None, ALU.mult), [su], [triS])
        P.op("pool", lambda e: e.memset(bsel[:], 0.0), writes=[bsel])
        P.op("pool", lambda e: e.memset(bsel[0:64, 0:16], -EXPM05), [bsel], [bsel])
        P.op("pool", lambda e: e.memset(bsel[64:128, 16:32], -EXPM05), [bsel], [bsel])
        ST = Tl(P, "ST", [64, 16, 128])
        P.op("dve", lambda e: e.memset(ST[:], 0.0), writes=[ST])
        Vblk = Tl(P, "Vblk", [128, 16, 128]); Ublk = Tl(P, "Ublk", [128, 16, 128])
        P.op("pool", lambda e: e.memset(Vblk[:], 0.0), writes=[Vblk])
        P.op("pool", lambda e: e.memset(Ublk[:], 0.0), writes=[Ublk])
        r_t = Tl(P, "r", [128, 1024]); kp_t = Tl(P, "kp", [128, 1024]); v_t = Tl(P, "v", [128, 1024])
        sg_t = Tl(P, "sg", [128, 1024]); kn_t = Tl(P, "kn", [128, 1024]); kka_t = Tl(P, "kka", [128, 1024])
        Ep = Tl(P, "Ep", [128, 1024]); Em = Tl(P, "Em", [128, 1024]); Epp = Tl(P, "Epp", [128, 1024])
        bon_t = Ep; g_t = Epp; xt = sg_t
        C.junk = Em
        Y = Tl(P, "Y", [128, 1024]); Rh = Tl(P, "Rh", [128, 1024]); t2 = Tl(P, "t2", [128, 1024])
        oT = Tl(P, "oT", [128, 8, 128], BF16)
        gamT = Tl(P, "gamT", [64, 16, 2])
        st = Tl(P, "st", [128, 8]); s16 = Tl(P, "s16", [128, 16]); s16b = Tl(P, "s16b", [128, 16])
        G = []
        for gi in range(4):
            g = Ctx()
            g.qT = Tl(P, f"qT{gi}", [64, 16, 128])
            g.Q = Tl(P, f"Q{gi}", [128, 4, 128]); g.QT = Tl(P, f"QT{gi}", [128, 4, 128])
            g.Aak = Tl(P, f"Aak{gi}", [128, 4, 128]); g.Arb = Tl(P, f"Arb{gi}", [128, 4, 128]); g.Ark = Tl(P, f"Ark{gi}", [128, 4, 128])
            g.X = Tl(P, f"X{gi}", [128, 4, 128])
            g.WT = g.qT
            G.append(g)

        def v3(t):
            return t[:].rearrange("p (h k) -> p h k", k=64)

        def b4(bk):
            return bk[:].rearrange("p (h n) -> p h n", n=128)

        for c in range(NCH):
            for nm, tl in (("r", r_t), ("kp", kp_t), ("v", v_t), ("sg", sg_t), ("kn", kn_t), ("kka", kka_t)):
                P.dma(C.dq(), tl[:], scr[nm][c], writes=[tl])
            for hf in range(2):
                cs = slice(hf * 512, (hf + 1) * 512)
                bk = bank(C)
                P.mm(bk[:], triI[:], sg_t[:, cs], reads=[triI, sg_t], writes=[bk])
                P.op("act", lambda e, bk=bk, cs=cs: e.activation(Ep[:, cs], bk[:], AF.Exp), [bk], [Ep])
                P.op("act", lambda e, bk=bk, cs=cs: e.activation(Em[:, cs], bk[:], AF.Exp, scale=-1.0), [bk], [Em])
                bk = bank(C)
                P.mm(bk[:], triS[:], sg_t[:, cs], reads=[triS, sg_t], writes=[bk])
                P.op("act", lambda e, bk=bk, cs=cs: e.activation(Epp[:, cs], bk[:], AF.Exp), [bk], [Epp])
            bk = bank(C)
            for h in range(16):
                P.mm(bk[0:64, h * 32:(h + 1) * 32], sg_t[:, h * 64:(h + 1) * 64], bsel[:], reads=[sg_t, bsel], writes=[bk])
            P.op("act", lambda e, bk=bk: e.activation(gamT[:], bk[0:64, :].rearrange("p (h b r) -> p h b r", b=2, r=16)[:, :, :, 0], AF.Exp), [bk], [gamT])
            stt(P, "dve", kn_t[:], kn_t[:], -1.0, Epp[:], ALU.mult, ALU.mult, [kn_t, Epp], [kn_t])
            tt(P, "pool", kka_t[:], kka_t[:], Em[:], ALU.mult, [kka_t, Em], [kka_t])
            tt(P, "dve", kp_t[:], kp_t[:], Em[:], ALU.mult, [kp_t, Em], [kp_t])
            tt(P, "pool", r_t[:], r_t[:], Ep[:], ALU.mult, [r_t, Ep], [r_t])
            copy_op(P, "pool", Vblk[0:64, :, 0:64], v3(v_t)[0:64], [v_t], [Vblk])
            copy_op(P, "pool", Vblk[64:128, :, 64:128], v3(v_t)[64:128], [v_t], [Vblk])
            quant = (kn_t, kka_t, kp_t, r_t)
            P.dma("sp", bon_t[:], scr["bonus"][c], writes=[bon_t])
            P.dma("act", g_t[:], scr["g"][c], writes=[g_t])
            for b in range(2):
                P.dma("sp", xt[b * 64:(b + 1) * 64, :], x_in[b, c * 64:(c + 1) * 64, :], writes=[xt])
            if CUT <= 1:
                continue
            for gi, g in enumerate(G):
                for q in range(4):
                    bk = bank(C)
                    for h4 in range(4):
                        h = gi * 4 + h4
                        P.tr(bk[0:64, h4 * 128:(h4 + 1) * 128], quant[q][:, h * 64:(h + 1) * 64], C.ident[:], [quant[q], C.ident], [bk])
                    copy_op(P, ev_eng(C), g.qT[:, q * 4:(q + 1) * 4, :], b4(bk)[0:64], [bk], [g.qT])
            if CUT <= 2:
                continue
            for gi, g in enumerate(G):
                for (dst, lq, rq, msk) in ((g.QT, 1, 0, su), (g.Q, 0, 1, sl), (g.Aak, 2, 0, su), (g.Arb, 1, 3, iu), (g.Ark, 2, 3, iu)):
                    bk = bank(C)
                    for h4 in range(4):
                        P.mm(bk[:, h4 * 128:(h4 + 1) * 128], g.qT[:, lq * 4 + h4, :], g.qT[:, rq * 4 + h4, :], reads=[g.qT], writes=[bk])
                    tt(P, "dve", dst[:], b4(bk), msk[:].unsqueeze(1).broadcast_to([128, 4, 128]), ALU.mult, [bk, msk], [dst])
            if CUT <= 3:
                continue
            for gi, g in enumerate(G):
                bk = bank(C)
                for h4 in range(4):
                    h = gi * 4 + h4
                    P.mm(bk[:, h4 * 128 + 64:(h4 + 1) * 128], g.Aak[:, h4, :], v_t[:, h * 64:(h + 1) * 64], reads=[g.Aak, v_t], writes=[bk])
                copy_op(P, ev_eng(C), g.X[:, :, 64:128], b4(bk)[:, :, 64:128], [bk], [g.X])
                copy_op(P, "pool", g.X[:, :, 0:64], v3(kn_t)[:, gi * 4:(gi + 1) * 4, :], [kn_t], [g.X])
            if CUT <= 4:
                continue
            for j in range(6):
                for gi, g in enumerate(G):
                    bk = bank(C)
                    for h4 in range(4):
                        P.mm(bk[:, h4 * 128:(h4 + 1) * 128], g.QT[:, h4, :], g.X[:, h4, :], reads=[g.QT, g.X], writes=[bk])
                    if j < 5:
                        bq = bank(C); bqt = bank(C)
                        for h4 in range(4):
                            P.mm(bq[:, h4 * 128:(h4 + 1) * 128], g.QT[:, h4, :], g.Q[:, h4, :], reads=[g.QT, g.Q], writes=[bq])
                        for h4 in range(4):
                            P.mm(bqt[:, h4 * 128:(h4 + 1) * 128], g.Q[:, h4, :], g.QT[:, h4, :], reads=[g.QT, g.Q], writes=[bqt])
                    tt(P, "dve", g.X[:], b4(bk), g.X[:], ALU.add, [bk, g.X], [g.X])
                    if j < 5:
                        copy_op(P, "act", g.Q[:], b4(bq), [bq], [g.Q])
                        copy_op(P, ev_eng(C), g.QT[:], b4(bqt), [bqt], [g.QT])
            if CUT <= 5:
                continue
            for gi, g in enumerate(G):
                bk = bank(C)
                for h4 in range(4):
                    h = gi * 4 + h4
                    EV = _os.environ.get("EV", "0")
                    if EV in ("0", "1"):
                        P.mm(bk[:, h4 * 128:h4 * 128 + 64], g.Arb[:, h4, :], g.X[:, h4, 0:64], reads=[g.Arb, g.X], writes=[bk])
                    if EV in ("0", "2"):
                        P.mm(bk[:, h4 * 128 + 64:(h4 + 1) * 128], g.Arb[:, h4, :], g.X[:, h4, 64:128], start=True, stop=False, reads=[g.Arb, g.X], writes=[bk])
                        P.mm(bk[:, h4 * 128 + 64:(h4 + 1) * 128], g.Ark[:, h4, :], v_t[:, h * 64:(h + 1) * 64], start=False, stop=True, reads=[g.Ark, v_t], writes=[bk])
                    if EV == "3":
                        P.mm(bk[:, h4 * 128:(h4 + 1) * 128], g.Arb[:, h4, :], g.X[:, h4, :], reads=[g.Arb, g.X], writes=[bk])
                hs = slice(gi * 4, (gi + 1) * 4)
                EVC = _os.environ.get("EVC", "0")
                if EVC != "1":
                    tt(P, "dve", v3(Rh)[:, hs, :], b4(bk)[:, :, 0:64], v3(r_t)[:, hs, :], ALU.add, [bk, r_t], [Rh])
                if EVC != "2":
                    copy_op(P, "dve", v3(Y)[:, hs, :], b4(bk)[:, :, 64:128], [bk], [Y])
            if CUT <= 5.5:
                continue
            for gi, g in enumerate(G):
                bk = bank(C); bk2 = bank(C)
                for h4 in range(4):
                    h = gi * 4 + h4
                    P.tr(bk[0:64, h4 * 128:(h4 + 1) * 128], g.X[:, h4, 0:64], C.ident[:], [g.X, C.ident], [bk])
                    P.tr(bk2[0:64, h4 * 128:(h4 + 1) * 128], Rh[:, h * 64:(h + 1) * 64], C.ident[:], [Rh, C.ident], [bk2])
                copy_op(P, "act", g.WT[:, 0:4, :], b4(bk)[0:64], [bk], [g.WT])
                copy_op(P, "dve", g.WT[:, 4:8, :], b4(bk2)[0:64], [bk2], [g.WT])
            if CUT <= 6:
                continue
            for gi, g in enumerate(G):
                hs = slice(gi * 4, (gi + 1) * 4)
                bu = bank(C); by = bank(C)
                for h4 in range(4):
                    h = gi * 4 + h4
                    P.mm(bu[:, h4 * 128:(h4 + 1) * 128], g.WT[:, h4, :], ST[:, h, :], reads=[g.WT, ST], writes=[bu])
                for h4 in range(4):
                    h = gi * 4 + h4
                    P.mm(by[:, h4 * 128:(h4 + 1) * 128], g.WT[:, 4 + h4, :], ST[:, h, :], reads=[g.WT, ST], writes=[by])
                for b in range(2):
                    ps_ = slice(b * 64, (b + 1) * 64)
                    tt(P, "dve", Ublk[ps_, hs, b * 64:(b + 1) * 64], b4(bu)[ps_, :, b * 64:(b + 1) * 64], g.X[ps_, :, 64:128], ALU.add,
                       [bu, g.X], [Ublk])
                    tt(P, "dve", v3(Y)[ps_, hs, :], b4(by)[ps_, :, b * 64:(b + 1) * 64], v3(Y)[ps_, hs, :], ALU.add, [by, Y], [Y])
                bs = bank(C)
                for h4 in range(4):
                    h = gi * 4 + h4
                    P.mm(bs[0:64, h4 * 128:(h4 + 1) * 128], kka_t[:, h * 64:(h + 1) * 64], Ublk[:, h, :], start=True, stop=False,
                         reads=[kka_t, Ublk], writes=[bs])
                    P.mm(bs[0:64, h4 * 128:(h4 + 1) * 128], kp_t[:, h * 64:(h + 1) * 64], Vblk[:, h, :], start=False, stop=True,
                         reads=[kp_t, Vblk], writes=[bs])
                tt(P, "dve", ST[:, hs, :], b4(bs)[0:64], ST[:, hs, :], ALU.add, [bs, ST], [ST])
                tt(P, "dve", ST[:, hs, :].rearrange("p h (b v) -> p h b v", b=2),
                   ST[:, hs, :].rearrange("p h (b v) -> p h b v", b=2),
                   gamT[:, hs, :].unsqueeze(3).broadcast_to([64, 4, 2, 64]), ALU.mult, [ST, gamT], [ST])
            if CUT <= 7:
                continue
            P.op("dve", lambda e: e.tensor_reduce(s16[:], v3(Y), AX.X, ALU.add), [Y], [s16])
            tt(P, "pool", t2[:], Y[:], Y[:], ALU.mult, [Y], [t2])
            P.op("dve", lambda e: e.tensor_reduce(s16b[:], v3(t2), AX.X, ALU.add), [t2], [s16b])
            P.op("dve", lambda e: e.tensor_scalar(s16[:], s16[:], 1.0 / 64, None, ALU.mult), [s16], [s16])
            tt(P, "pool", t2[:, 0:16], s16[:], s16[:], ALU.mult, [s16], [t2])
            stt(P, "dve", s16b[:], s16b[:], 1.0 / 64, t2[:, 0:16], ALU.mult, ALU.subtract, [s16b, t2], [s16b])
            rsqrt_op(P, s16b, 0, 16, GN_EPS, False)
            tt(P, "dve", v3(Y), v3(Y), s16[:].unsqueeze(2).broadcast_to([128, 16, 64]), ALU.subtract, [Y, s16], [Y])
            tt(P, "dve", v3(Y), v3(Y), s16b[:].unsqueeze(2).broadcast_to([128, 16, 64]), ALU.mult, [Y, s16b], [Y])
            tt(P, "pool", Y[:], Y[:], lnxg[:], ALU.mult, [Y, lnxg], [Y])
            tt(P, "pool", Y[:], Y[:], lnxb[:], ALU.add, [Y, lnxb], [Y])
            tt(P, "dve", Y[:], Y[:], bon_t[:], ALU.add, [Y, bon_t], [Y])
            tt(P, "dve", Y[:], Y[:], g_t[:], ALU.mult, [Y, g_t], [Y])
            if CUT <= 8:
                continue
            bA, bB = bank(C), bank(C)
            for kc in range(8):
                bk = bA if kc < 4 else bB
                P.tr(bk[:, (kc % 4) * 128:(kc % 4 + 1) * 128], Y[:, kc * 128:(kc + 1) * 128], C.ident[:], [Y, C.ident], [bk])
            copy_op(P, "act", oT[:, 0:4, :], b4(bA), [bA], [oT])
            copy_op(P, "dve", oT[:, 4:8, :], b4(bB), [bB], [oT])
            for hf in range(2):
                cs = slice(hf * 512, (hf + 1) * 512)
                bk = bank(C)
                for kc in range(8):
                    P.mm(bk[:], oT[:, kc, :], wo[:, kc, cs], start=(kc == 0), stop=(kc == 7), reads=[oT, C.wres], writes=[bk])
                copy_op(P, "act", t2[:, cs], bk[:], [bk], [t2])
                stt(P, "dve", t2[:, cs], xt[:, cs], ALPHA, t2[:, cs], ALU.mult, ALU.add, [xt, t2], [t2])
            layer_norm_tile(C, t2, Rh, lng, lnb, st)
            for b in range(2):
                P.dma("act", x_out[b, c * 64:(c + 1) * 64, :], Rh[b * 64:(b + 1) * 64, :], reads=[Rh])


SW_LIMIT = 7.0
SW_ALPHA = 1.702


def moe_stage(P, D, l, S, x_in, x_out, NE=32):
    T = 2 * S
    TQ = min(1024, T)
    NQ = T // TQ
    NT = TQ // 128
    NB = TQ // 512
    xin = x_in.rearrange("b s d -> (b s) d")
    xout = x_out.rearrange("b s d -> (b s) d")
    wgu_d = D["moe_w_gu"][l]
    wdn_d = D["moe_w_dn"][l]
    with P.scope():
        C = setup_common(P)
        dq_factory(C)
        C.wres = P.res("weights")
        wr = Tl(P, "wr", [128, 8, 32])
        P.dma("sp", wr[:], D["moe_router_w"][l].rearrange("(k p) e -> p k e", p=128), writes=[C.wres])
        br = load_bcast(P, "br", D["moe_router_b"][l], 32, "act")
        bdn = Tl(P, "bdn", [32, 1024])
        P.dma("sp", bdn[:], D["moe_b_dn"][l], writes=[C.wres])
        xT32f = Tl(P, "xT32", [128, 1024])
        C.junk = xT32f
        bgu_raw = xT32f
        bguT = Tl(P, "bguT", [128, 16, 32])
        for half in range(2):
            P.dma("act", bgu_raw[0:32, :], D["moe_b_gu"][l][:, half * 1024:(half + 1) * 1024], writes=[bgu_raw])
            for cb in range(2):
                bk = bank(C)
                for c4 in range(4):
                    cc = cb * 4 + c4
                    P.tr(bk[:, c4 * 32:(c4 + 1) * 32], bgu_raw[0:32, cc * 128:(cc + 1) * 128], C.ident[0:32, 0:32], [bgu_raw, C.ident], [bk])
                copy_op(P, "dve", bguT[:, half * 8 + cb * 4:half * 8 + (cb + 1) * 4, :], bk[:, 0:128].rearrange("p (c e) -> p c e", e=32), [bk], [bguT])
        bgu7 = Tl(P, "bgu7", [128, 16, 32])
        P.op("dve", lambda e: e.tensor_scalar(bgu7[:], bguT[:], SW_LIMIT, None, ALU.add), [bguT], [bgu7])
        lng = load_bcast(P, "lng", D["ln_g"][l, 1], 1024, "sp")
        lnb = load_bcast(P, "lnb", D["ln_b"][l, 1], 1024, "act")
        xT = Tl(P, "xT", [128, 8, TQ], BF16)
        acc = Tl(P, "acc", [128, NT, 1024])
        G = Tl(P, "G", [128, NT, 32])
        Wgu = [Tl(P, f"Wgu{i}", [128, 8, 2048], BF16) for i in range(2)]
        Wdn = [Tl(P, f"Wdn{i}", [128, 8, 1024], BF16) for i in range(2)]
        actT = [Tl(P, f"actT{i}", [128, 8, 512], BF16) for i in range(2)]
        xt = Tl(P, "xt", [128, 1024]); z = Tl(P, "z", [128, 1024])
        xT32 = xT32f
        x3 = xT32f[:].rearrange("p (k n) -> p k n", n=128)
        lg = Tl(P, "lg", [128, 32]); t8 = Tl(P, "t8", [128, 8]); msk = Tl(P, "msk", [128, 32]); st = Tl(P, "st", [128, 8])
        GT = Tl(P, "GT", [32, 128])
        tmp = [[Tl(P, f"sw{i}_{j}", [128, 512]) for j in range(3)] for i in range(2)]

        def load_expert(e, buf):
            for k0 in range(0, 8, 2):
                P.dma("pool", Wgu[buf][:, k0:k0 + 2, :], wgu_d[e, k0 * 128:(k0 + 2) * 128, :].rearrange("(k p) n -> p k n", p=128), writes=[Wgu[buf]])
            for k0 in range(0, 8, 4):
                P.dma("pool", Wdn[buf][:, k0:k0 + 4, :], wdn_d[e, k0 * 128:(k0 + 4) * 128, :].rearrange("(k p) n -> p k n", p=128), writes=[Wdn[buf]])

        seq = [(q, e) for q in range(NQ) for e in range(NE)]
        load_expert(0, 0)
        si = 0
        for q in range(NQ):
            t0 = q * TQ
            for t in range(NT):
                rows = slice(t0 + t * 128, t0 + (t + 1) * 128)
                P.dma("sp", xt[:], xin[rows, :], writes=[xt])
                bA, bB = bank(C), bank(C)
                for kc in range(8):
                    bk = bA if kc < 4 else bB
                    P.tr(bk[:, (kc % 4) * 128:(kc % 4 + 1) * 128], xt[:, kc * 128:(kc + 1) * 128], C.ident[:], [xt, C.ident], [bk])
                copy_op(P, "act", x3[:, 0:4, :], bA[:].rearrange("p (k n) -> p k n", n=128), [bA], [xT32])
                copy_op(P, "dve", x3[:, 4:8, :], bB[:].rearrange("p (k n) -> p k n", n=128), [bB], [xT32])
                copy_op(P, "act", xT[:, :, t * 128:(t + 1) * 128], x3, [xT32], [xT])
                bk = bank(C)
                for kc in range(8):
                    P.mm(bk[:, 0:32], x3[:, kc, :], wr[:, kc, :], start=(kc == 0), stop=(kc == 7), reads=[xT32, C.wres], writes=[bk])
                tt(P, "dve", lg[:], bk[:, 0:32], br[:], ALU.add, [bk, br], [lg])
                P.op("dve", lambda e: e.max(t8[:], lg[:]), [lg], [t8])
                P.op("dve", lambda e: e.tensor_scalar(msk[:], lg[:], t8[:, 3:4], None, ALU.is_ge), [lg, t8], [msk])
                P.op("dve", lambda e: e.tensor_scalar(st[:, 0:1], t8[:, 0:1], -1.0, None, ALU.mult), [t8], [st])
                P.op("act", lambda e: e.activation(lg[:], lg[:], AF.Exp, bias=st[:, 0:1]), [lg, st], [lg])
                tt(P, "dve", lg[:], lg[:], msk[:], ALU.mult, [lg, msk], [lg])
                P.op("dve", lambda e: e.tensor_reduce(st[:, 1:2], lg[:], AX.X, ALU.add), [lg], [st])
                P.op("dve", lambda e: e.reciprocal(st[:, 2:3], st[:, 1:2]), [st], [st])
                P.op("dve", lambda e, t=t: e.tensor_scalar(G[:, t, :], lg[:], st[:, 2:3], None, ALU.mult), [lg, st], [G])
            P.op("dve", lambda e: e.memset(acc[:], 0.0), writes=[acc])
            for e in range(NE):
                buf = si % 2
                import os as _os
                if si + 1 < len(seq) and not (_os.environ.get("MOE_NOLOAD") and si > 0):
                    load_expert(seq[si + 1][1], (si + 1) % 2)
                si += 1
                for blk in range(NB):
                    ts = slice(blk * 512, (blk + 1) * 512)
                    aT = actT[blk % 2]
                    for fc in range(8):
                        tp = tmp[fc % 2]
                        bg, bu = bank(C), bank(C)
                        for kc in range(8):
                            P.mm(bg[:], Wgu[buf][:, kc, fc * 128:(fc + 1) * 128], xT[:, kc, ts], start=(kc == 0), stop=(kc == 7),
                                 reads=[Wgu[buf], xT], writes=[bg])
                        for kc in range(8):
                            P.mm(bu[:], Wgu[buf][:, kc, 1024 + fc * 128:1024 + (fc + 1) * 128], xT[:, kc, ts], start=(kc == 0), stop=(kc == 7),
                                 reads=[Wgu[buf], xT], writes=[bu])
                        gp, sg, u1 = tp
                        P.op("dve", lambda en, bg=bg, gp=gp, fc=fc, e=e: en.tensor_scalar(gp[:], bg[:], bguT[:, fc, e:e + 1], SW_LIMIT, ALU.add, ALU.min),
                             [bg, bguT], [gp])
                        P.op("act", lambda en, gp=gp, sg=sg: en.activation(sg[:], gp[:], AF.Silu, scale=SW_ALPHA), [gp], [sg])
                        P.op("act", lambda en, bu=bu, u1=u1, fc=fc, e=e: en.activation(u1[:], bu[:], AF.Relu, bias=bgu7[:, 8 + fc, e:e + 1]),
                             [bu, bgu7], [u1])
                        P.op("dve", lambda en, u1=u1: en.tensor_scalar(u1[:], u1[:], 2.0 * SW_LIMIT, 1.0 - SW_LIMIT, ALU.min, ALU.add), [u1], [u1])
                        stt(P, "dve", aT[:, fc, :], sg[:], 1.0 / SW_ALPHA, u1[:], ALU.mult, ALU.mult, [sg, u1], [aT])
                for blk in range(NB):
                    aT = actT[blk % 2]
                    for t4 in range(4):
                        tile_i = blk * 4 + t4
                        for hf in range(2):
                            cs = slice(hf * 512, (hf + 1) * 512)
                            bk = bank(C)
                            for fc in range(8):
                                P.mm(bk[:], aT[:, fc, t4 * 128:(t4 + 1) * 128], Wdn[buf][:, fc, cs], start=(fc == 0), stop=(fc == 7),
                                     reads=[aT, Wdn[buf]], writes=[bk])
                            stt(P, "dve", acc[:, tile_i, cs], bk[:], G[:, tile_i, e:e + 1], acc[:, tile_i, cs], ALU.mult, ALU.add, [bk, G, acc], [acc])
            for t in range(NT):
                rows = slice(t0 + t * 128, t0 + (t + 1) * 128)
                bk = bank(C)
                P.tr(bk[0:32, 0:128], G[:, t, :], C.ident[:], [G, C.ident], [bk])
                copy_op(P, "act", GT[:], bk[0:32, 0:128], [bk], [GT])
                P.dma("sp", xt[:], xin[rows, :], writes=[xt])
                for hf in range(2):
                    cs = slice(hf * 512, (hf + 1) * 512)
                    bk = bank(C)
                    P.mm(bk[:], GT[:], bdn[:, cs], reads=[GT, C.wres], writes=[bk])
                    tt(P, "dve", z[:, cs], bk[:], acc[:, t, cs], ALU.add, [bk, acc], [z])
                stt(P, "dve", z[:], xt[:], ALPHA, z[:], ALU.mult, ALU.add, [xt, z], [z])
                layer_norm_tile(C, z, xt, lng, lnb, st)
                P.dma("act", xout[rows, :], xt[:], reads=[xt])


NEG = -1.0e30
SUBLN_EPS = 1e-5


def xT_block(C, xin, r0, nt, xt, xTb):
    P = C.P
    for t in range(nt):
        P.dma("sp", xt[:], xin[r0 + t * 128:r0 + (t + 1) * 128, :], writes=[xt])
        bA, bB = bank(C), bank(C)
        for kc in range(8):
            bk = bA if kc < 4 else bB
            P.tr(bk[:, (kc % 4) * 128:(kc % 4 + 1) * 128], xt[:, kc * 128:(kc + 1) * 128], C.ident[:], [xt, C.ident], [bk])
        copy_op(P, "act", xTb[:, 0:4, t * 128:(t + 1) * 128], bA[:].rearrange("p (k n) -> p k n", n=128), [bA], [xTb])
        copy_op(P, "dve", xTb[:, 4:8, t * 128:(t + 1) * 128], bB[:].rearrange("p (k n) -> p k n", n=128), [bB], [xTb])


def proj_stage(P, D, S, x_in, w_T_dram, T_scr, w_tok_dram=None, tok_scr=None):
    T = 2 * S
    NBLK = T // 512
    xin = x_in.rearrange("b s d -> (b s) d")
    with P.scope():
        C = setup_common(P)
        dq_factory(C)
        C.wres = P.res("weights")
        wT = Tl(P, "wT", [128, 8, 1024], BF16)
        for k0 in range(0, 8, 2):
            P.dma("pool", wT[:, k0:k0 + 2, :], w_T_dram[k0 * 128:(k0 + 2) * 128, :].rearrange("(k p) n -> p k n", p=128), writes=[C.wres])
        if w_tok_dram is not None:
            wK = Tl(P, "wK", [128, 8, 1024], BF16)
            for k0 in range(0, 8, 2):
                P.dma("pool", wK[:, k0:k0 + 2, :], w_tok_dram[k0 * 128:(k0 + 2) * 128, :].rearrange("(k p) n -> p k n", p=128), writes=[C.wres])
        xt = Tl(P, "xt", [128, 1024])
        xTb = [Tl(P, f"xTb{i}", [128, 8, 512], BF16) for i in range(2)]
        oT = [Tl(P, f"oT{i}", [64, 512]) for i in range(4)]
        ot = [Tl(P, f"ot{i}", [128, 1024]) for i in range(2)]
        for blk in range(NBLK):
            xb = xTb[blk % 2]
            xT_block(C, xin, blk * 512, 4, xt, xb)
            for g in range(16):
                bk = bank(C)
                for kc in range(8):
                    P.mm(bk[0:64, :], wT[:, kc, g * 64:(g + 1) * 64], xb[:, kc, :], start=(kc == 0), stop=(kc == 7), reads=[C.wres, xb], writes=[bk])
                o = oT[g % 4]
                copy_op(P, ev_eng(C), o[:], bk[0:64, :], [bk], [o])
                P.dma(C.dq(), T_scr[g, :, blk * 512:(blk + 1) * 512], o[:], reads=[o])
            if w_tok_dram is not None:
                for t4 in range(4):
                    o = ot[t4 % 2]
                    for hf in range(2):
                        cs = slice(hf * 512, (hf + 1) * 512)
                        bk = bank(C)
                        for kc in range(8):
                            P.mm(bk[:], xb[:, kc, t4 * 128:(t4 + 1) * 128], wK[:, kc, cs], start=(kc == 0), stop=(kc == 7), reads=[C.wres, xb], writes=[bk])
                        copy_op(P, ev_eng(C), o[:, cs], bk[:], [bk], [o])
                    P.dma(C.dq(), tok_scr[blk * 512 + t4 * 128:blk * 512 + (t4 + 1) * 128, :], o[:], reads=[o])


def attn_stage(P, D, j, S, qT_scr, kT_scr, v_scr, o_scr):
    import math
    l = NA + j
    lam_init = 0.8 - 0.6 * math.exp(-0.3 * l)
    NQB = S // 128
    with P.scope():
        C = setup_common(P)
        dq_factory(C)
        lamt = load_bcast(P, "lamt", D["da_lambda"][j].rearrange("a d -> (a d)"), 256, "sp")
        lsc = Tl(P, "lsc", [128, 8]); ljunk = Tl(P, "ljunk", [128, 64])
        tt(P, "dve", ljunk[:], lamt[:, 0:64], lamt[:, 64:128], ALU.mult, [lamt], [ljunk])
        P.op("dve", lambda e: e.tensor_reduce(lsc[:, 0:1], ljunk[:], AX.X, ALU.add), [ljunk], [lsc])
        tt(P, "dve", ljunk[:], lamt[:, 128:192], lamt[:, 192:256], ALU.mult, [lamt, ljunk], [ljunk])
        P.op("dve", lambda e: e.tensor_reduce(lsc[:, 1:2], ljunk[:], AX.X, ALU.add), [ljunk], [lsc])
        P.op("act", lambda e: e.activation(lsc[:, 2:4], lsc[:, 0:2], AF.Exp), [lsc], [lsc])
        tt(P, "dve", lsc[:, 4:5], lsc[:, 3:4], lsc[:, 2:3], ALU.subtract, [lsc], [lsc])
        P.op("dve", lambda e: e.tensor_scalar(lsc[:, 5:6], lsc[:, 4:5], -lam_init, None, ALU.add), [lsc], [lsc])
        gsc = load_bcast(P, "gsc", D["da_subln_g"][j], 128, "act")
        P.op("dve", lambda e: e.tensor_scalar(gsc[:], gsc[:], 1.0 - lam_init, None, ALU.mult), [gsc], [gsc])
        D0i = Tl(P, "D0i", [128, S], I32)
        D0f = Tl(P, "D0f", [128, S])
        P.op("pool", lambda e: e.iota(D0i[:], [[-1, S]], base=S - 128, channel_multiplier=1), writes=[D0i])
        copy_op(P, "dve", D0f[:], D0i[:], [D0i], [D0f])
        Bh = [Tl(P, f"Bh{i}", [128, S]) for i in range(2)]
        kT = [Tl(P, f"kT{i}", [64, 2, S]) for i in range(2)]
        qT = [Tl(P, f"qT{i}", [64, 2, S]) for i in range(2)]
        vv = [Tl(P, f"vv{i}", [128, NQB, 128]) for i in range(2)]
        tmp_all = [[Tl(P, f"tmp{p}_{i}", [128, S]) for i in range(2)] for p in range(2)]
        attnT_all = [Tl(P, f"attnT{p}", [128, NQB, 128]) for p in range(2)]
        sc_all = [Tl(P, f"sc{p}", [128, 16]) for p in range(2)]
        osb_all = [Tl(P, f"osb{p}", [128, 128]) for p in range(2)]
        oo = [Tl(P, f"oo{i}", [128, 128]) for i in range(2)]; ojunk = Tl(P, "ojunk", [128, 128])
        items = [(b, h, i) for b in range(2) for h in range(8) for i in range(NQB)]

        def head_setup(b, h, st_):
            for c in range(2):
                P.dma("sp", kT[st_][:, c, :], kT_scr[h * 2 + c, :, b * S:(b + 1) * S], writes=[kT[st_]])
                P.dma("act", qT[st_][:, c, :], qT_scr[h * 2 + c, :, b * S:(b + 1) * S], writes=[qT[st_]])
            P.dma("sp", vv[st_][:], v_scr[b * S:(b + 1) * S, h * 128:(h + 1) * 128].rearrange("(t p) d -> p t d", p=128), writes=[vv[st_]])
            slope = 2.0 ** (-(h + 1))
            P.op("act", lambda e, slope=slope, st_=st_: e.activation(Bh[st_][:], D0f[:], AF.Copy, scale=-slope), [D0f], [Bh[st_]])
            P.op("pool", lambda e, st_=st_: e.affine_select(Bh[st_][:], Bh[st_][:], [[-1, S]], ALU.is_ge, NEG, base=S - 128, channel_multiplier=1),
                 [Bh[st_]], [Bh[st_]])

        def stage_a(n):
            b, h, i = items[n]
            st_ = (b * 8 + h) % 2
            if i == 0:
                head_setup(b, h, st_)
            nk = (i + 1) * 128
            tmp = tmp_all[n % 2]; sc = sc_all[n % 2]
            for c in range(2):
                tc_ = tmp[c]
                for k0 in range(0, nk, 512):
                    kw = min(512, nk - k0)
                    bk = bank(C)
                    P.mm(bk[:, 0:kw], qT[st_][:, c, i * 128:(i + 1) * 128], kT[st_][:, c, k0:k0 + kw], reads=[qT[st_], kT[st_]], writes=[bk])
                    stt(P, "dve", tc_[:, k0:k0 + kw], bk[:, 0:kw], 0.125, Bh[st_][:, S - nk + k0:S - nk + k0 + kw], ALU.mult, ALU.add, [bk, Bh[st_]], [tc_])
                P.op("dve", lambda e, tc_=tc_, nk=nk, c=c, sc=sc: e.tensor_reduce(sc[:, c:c + 1], tc_[:, 0:nk], AX.X, ALU.max), [tc_], [sc])
                P.op("dve", lambda e, c=c, sc=sc: e.tensor_scalar(sc[:, 2 + c:3 + c], sc[:, c:c + 1], -1.0, None, ALU.mult), [sc], [sc])
                P.op("act", lambda e, tc_=tc_, nk=nk, c=c, sc=sc: e.activation(tc_[:, 0:nk], tc_[:, 0:nk], AF.Exp, bias=sc[:, 2 + c:3 + c],
                                                                            accum_out=sc[:, 4 + c:5 + c]), [tc_, sc], [tc_, sc])
            P.op("dve", lambda e, sc=sc: e.reciprocal(sc[:, 6:8], sc[:, 4:6]), [sc], [sc])
            tt(P, "dve", sc[:, 8:9], sc[:, 7:8], lsc[:, 5:6], ALU.mult, [sc, lsc], [sc])
            P.op("dve", lambda e, nk=nk, tmp=tmp, sc=sc: e.tensor_scalar(tmp[1][:, 0:nk], tmp[1][:, 0:nk], sc[:, 8:9], None, ALU.mult), [tmp[1], sc], [tmp[1]])
            stt(P, "dve", tmp[0][:, 0:nk], tmp[0][:, 0:nk], sc[:, 6:7], tmp[1][:, 0:nk], ALU.mult, ALU.add, [tmp[0], tmp[1], sc], [tmp[0]])

        def stage_b(n):
            b, h, i = items[n]
            st_ = (b * 8 + h) % 2
            tmp = tmp_all[n % 2]; attnT = attnT_all[n % 2]; sc = sc_all[n % 2]; osb = osb_all[n % 2]
            for k0 in range(0, i + 1, 4):
                k1 = min(i + 1, k0 + 4)
                bk = bank(C)
                for kt in range(k0, k1):
                    P.tr(bk[:, (kt - k0) * 128:(kt - k0 + 1) * 128], tmp[0][:, kt * 128:(kt + 1) * 128], C.ident[:], [tmp[0], C.ident], [bk])
                copy_op(P, ev_eng(C), attnT[:, k0:k1, :], bk[:, 0:(k1 - k0) * 128].rearrange("p (k n) -> p k n", n=128), [bk], [attnT])
            bk = bank(C)
            for kt in range(i + 1):
                P.mm(bk[:, 0:128], attnT[:, kt, :], vv[st_][:, kt, :], start=(kt == 0), stop=(kt == i), reads=[attnT, vv[st_]], writes=[bk])
            copy_op(P, "dve", osb[:], bk[:, 0:128], [bk], [osb])
            P.op("act", lambda e, osb=osb, sc=sc: e.activation(ojunk[:], osb[:], AF.Square, accum_out=sc[:, 9:10]), [osb], [ojunk, sc])
            P.op("dve", lambda e, sc=sc: e.tensor_scalar(sc[:, 10:11], sc[:, 9:10], 1.0 / 128, SUBLN_EPS, ALU.mult, ALU.add), [sc], [sc])
            P.op("act", lambda e, sc=sc: e.activation(sc[:, 10:11], sc[:, 10:11], AF.Sqrt), [sc], [sc])
            P.op("dve", lambda e, sc=sc: e.reciprocal(sc[:, 10:11], sc[:, 10:11]), [sc], [sc])
            o = oo[n % 2]
            stt(P, "dve", o[:], osb[:], sc[:, 10:11], gsc[:], ALU.mult, ALU.mult, [osb, sc, gsc], [o])
            P.dma(C.dq(), o_scr[b * S + i * 128:b * S + (i + 1) * 128, h * 128:(h + 1) * 128], o[:], reads=[o])

        for n in range(len(items) + 1):
            if n < len(items):
                stage_a(n)
            if n >= 1:
                stage_b(n - 1)


def outproj_ln_stage(P, D, S, o_scr, w_dram, x_in, x_out, lng_ap, lnb_ap):
    T = 2 * S
    xin = x_in.rearrange("b s d -> (b s) d")
    xout = x_out.rearrange("b s d -> (b s) d")
    with P.scope():
        C = setup_common(P)
        dq_factory(C)
        C.wres = P.res("weights")
        C.junk = Tl(P, "junk", [128, 1024])
        wo = Tl(P, "wo", [128, 8, 1024], BF16)
        for k0 in range(0, 8, 2):
            P.dma("pool", wo[:, k0:k0 + 2, :], w_dram[k0 * 128:(k0 + 2) * 128, :].rearrange("(k p) n -> p k n", p=128), writes=[C.wres])
        lng = load_bcast(P, "lng", lng_ap, 1024, "sp")
        lnb = load_bcast(P, "lnb", lnb_ap, 1024, "act")
        ot = Tl(P, "ot", [128, 1024]); oTb = Tl(P, "oTb", [128, 8, 128], BF16)
        xt = [Tl(P, f"xt{i}", [128, 1024]) for i in range(2)]; z = Tl(P, "z", [128, 1024]); st = Tl(P, "st", [128, 8])
        res = [Tl(P, f"res{i}", [128, 1024]) for i in range(2)]
        for t in range(T // 128):
            rows = slice(t * 128, (t + 1) * 128)
            xx = xt[t % 2]
            P.dma("act", xx[:], xin[rows, :], writes=[xx])
            xT_block(C, o_scr, t * 128, 1, ot, oTb)
            for hf in range(2):
                cs = slice(hf * 512, (hf + 1) * 512)
                bk = bank(C)
                for kc in range(8):
                    P.mm(bk[:], oTb[:, kc, :], wo[:, kc, cs], start=(kc == 0), stop=(kc == 7), reads=[oTb, C.wres], writes=[bk])
                copy_op(P, "act", z[:, cs], bk[:], [bk], [z])
            stt(P, "dve", z[:], xx[:], ALPHA, z[:], ALU.mult, ALU.add, [xx, z], [z])
            r = res[t % 2]
            layer_norm_tile(C, z, r, lng, lnb, st)
            P.dma("sp", xout[rows, :], r[:], reads=[r])


INPUT_SHAPES = {
    "ln_g": [4, 2, 1024], "ln_b": [4, 2, 1024], "rwkv_mix": [2, 6, 1024], "rwkv_w_rkv": [2, 3, 1024, 1024],
    "rwkv_w_o": [2, 1024, 1024], "rwkv_w0": [2, 1024], "rwkv_w1": [2, 1024, 64], "rwkv_w2": [2, 64, 1024],
    "rwkv_a0": [2, 1024], "rwkv_a1": [2, 1024, 64], "rwkv_a2": [2, 64, 1024], "rwkv_g1": [2, 1024, 160],
    "rwkv_g2": [2, 160, 1024], "rwkv_k_k": [2, 1024], "rwkv_k_a": [2, 1024], "rwkv_r_k": [2, 16, 64],
    "rwkv_lnx_g": [2, 1024], "rwkv_lnx_b": [2, 1024], "rwkv_v0": [1, 1024], "rwkv_v1": [1, 1024, 32],
    "rwkv_v2": [1, 32, 1024], "kv_w": [1024, 2048], "da_w_q": [2, 1024, 1024], "da_w_o": [2, 1024, 1024],
    "da_lambda": [2, 4, 64], "da_subln_g": [2, 128], "moe_router_w": [4, 1024, 32], "moe_router_b": [4, 32],
    "moe_w_gu": [4, 32, 1024, 2048], "moe_b_gu": [4, 32, 2048], "moe_w_dn": [4, 32, 1024, 1024], "moe_b_dn": [4, 32, 1024],
}
RWKV_KEYS = [k for k in INPUT_SHAPES if k.startswith("rwkv_")] + ["ln_g", "ln_b"]
SCR_NAMES = ("r", "kp", "v", "sg", "kn", "kka", "bonus", "g", "vf")


def build_program(S, plan, keys, shapes=None, ne=32):
    nc = bass.Bass("TRN2", target_bir_lowering=False)
    shapes = shapes or {}
    D = {k: nc.dram_tensor(k, shapes.get(k, INPUT_SHAPES[k]), F32, kind="ExternalInput").ap() for k in keys}
    x = nc.dram_tensor("x", [2, S, 1024], F32, kind="ExternalInput").ap()
    out = nc.dram_tensor("out", [2, S, 1024], F32, kind="ExternalOutput").ap()
    xa = nc.dram_tensor("xa", [2, S, 1024], F32).ap()
    xb = nc.dram_tensor("xb", [2, S, 1024], F32).ap()
    NCH = S // 64
    scr = {n: nc.dram_tensor("scr_" + n, [NCH, 128, 1024], F32).ap() for n in SCR_NAMES}
    ascr = {"kT": nc.dram_tensor("scr_kT", [16, 64, 2 * S], F32).ap(), "qT": nc.dram_tensor("scr_qT", [16, 64, 2 * S], F32).ap(),
            "v": nc.dram_tensor("scr_vsh", [2 * S, 1024], F32).ap(), "o": nc.dram_tensor("scr_o", [2 * S, 1024], F32).ap()}
    P = Prog(nc)
    bufs = {"x": x, "out": out, "xa": xa, "xb": xb}
    for stg in plan:
        kind = stg[0]
        if kind == "rwkv":
            _, l, src, dst = stg
            import os as _os
            if _os.environ.get("ONLY") != "2":
                rwkv_pass1(P, D, l, S, bufs[src], scr)
            if _os.environ.get("ONLY") != "1":
                rwkv_pass2(P, D, l, S, bufs[src], bufs[dst], scr)
        elif kind == "kvproj":
            _, src_ = stg
            proj_stage(P, D, S, bufs[src_], D["kv_w"][:, 0:1024], ascr["kT"], D["kv_w"][:, 1024:2048], ascr["v"])
        elif kind == "attn":
            _, j, src_, dst = stg
            proj_stage(P, D, S, bufs[src_], D["da_w_q"][j], ascr["qT"])
            attn_stage(P, D, j, S, ascr["qT"], ascr["kT"], ascr["v"], ascr["o"])
            outproj_ln_stage(P, D, S, ascr["o"], D["da_w_o"][j], bufs[src_], bufs[dst], D["ln_g"][NA + j, 0], D["ln_b"][NA + j, 0])
        elif kind == "moe":
            _, l, src_, dst = stg
            moe_stage(P, D, l, S, bufs[src_], bufs[dst], NE=ne)
        else:
            raise ValueError(kind)
    P.finish()
    return nc, P


FULL_PLAN = [("rwkv", 0, "x", "xa"), ("moe", 0, "xa", "xb"), ("rwkv", 1, "xb", "xa"), ("moe", 1, "xa", "xb"),
             ("kvproj", "xb"), ("attn", 0, "xb", "xa"), ("moe", 2, "xa", "xb"), ("attn", 1, "xb", "xa"), ("moe", 3, "xa", "out")]


def kernel(**inputs):
    from concourse.bass_utils import run_bass_kernel_spmd
    n = 8
    S = 2048
    x = np.ascontiguousarray(np.asarray(inputs["x"], dtype=np.float32))
    keys = list(INPUT_SHAPES.keys())
    nc, P = build_program(S, FULL_PLAN, keys)
    shared = {k: np.ascontiguousarray(np.asarray(inputs[k], dtype=np.float32)) for k in keys}
    in_maps = []
    for c in range(n):
        m = dict(shared)
        m["x"] = x[2 * c:2 * c + 2]
        in_maps.append(m)
    res = run_bass_kernel_spmd(nc, in_maps, core_ids=list(range(n)))
    return np.concatenate([r["out"] for r in res.results], axis=0).astype(np.float32)
```
